# Optimizing a Trainium2 kernel written in Bass

```python
import math
import jax, jax.numpy as jnp
from jax import lax
import numpy as np

D_MODEL = 1024
BATCH = 8
SEQ = 8192
DEPTH = 1

MIX_WIDTH = D_MODEL
DN_HEADS = 4
DN_HEAD_DIM = MIX_WIDTH // 2 // DN_HEADS
DN_WIDTH = DN_HEADS * DN_HEAD_DIM
CONV_WIDTH = 4
CHUNK = 64
POOL_WINDOWS = (2, 4, 8, 16)
POOL_GROUPS = len(POOL_WINDOWS)
POOL_WIDTH = MIX_WIDTH - DN_WIDTH
POOL_GROUP_DIM = POOL_WIDTH // POOL_GROUPS
IN_WIDTH = 4 * DN_WIDTH + 2 * DN_HEADS + POOL_WIDTH
PEER_HEADS = 8
N_KEYS = 128
N_EXPERTS = N_KEYS * N_KEYS
PEER_QUERY_DIM = 256
PEER_HALF = PEER_QUERY_DIM // 2
PEER_TOPK = 16
PEER_TOKEN_BLOCK = 128
EPS = 1e-6

kernel_name = "hymba_deltanet_pool_peer_block"


def _rmsnorm(x, w):
    xf = x.astype(jnp.float32)
    y = xf * lax.rsqrt(jnp.mean(xf * xf, axis=-1, keepdims=True) + EPS)
    return (y * w.astype(jnp.float32)).astype(x.dtype)


def _l2norm(x):
    return x * lax.rsqrt(jnp.sum(x * x, axis=-1, keepdims=True) + EPS)


def _causal_conv(x, w):
    c = x.shape[-1]
    return lax.conv_general_dilated(
        x, w[:, None, :].astype(x.dtype), window_strides=(1,),
        padding=((CONV_WIDTH - 1, 0),),
        dimension_numbers=("NWC", "WIO", "NWC"), feature_group_count=c)


def _chunk_gated_delta_rule(q, k, v, g, beta):
    b, s, h, dk = q.shape
    dv = v.shape[-1]
    n = s // CHUNK
    q = q * (dk ** -0.5)

    def to_chunks(t):
        return t.reshape(b, n, CHUNK, h, -1).transpose(0, 3, 1, 2, 4)

    q, k, v = to_chunks(q), to_chunks(k), to_chunks(v)
    g = g.reshape(b, n, CHUNK, h).transpose(0, 3, 1, 2)
    beta = beta.reshape(b, n, CHUNK, h).transpose(0, 3, 1, 2)
    g = jnp.cumsum(g, axis=-1)
    k_beta = k * beta[..., None]
    v_beta = v * beta[..., None]
    causal = jnp.tril(jnp.ones((CHUNK, CHUNK), dtype=bool))
    strict = jnp.tril(jnp.ones((CHUNK, CHUNK), dtype=bool), -1)
    decay = jnp.exp(jnp.where(causal, g[..., :, None] - g[..., None, :], -jnp.inf))
    a_mat = jnp.where(strict, jnp.einsum("bhnid,bhnjd->bhnij", k_beta, k) * decay, 0.0)
    eye = jnp.eye(CHUNK, dtype=q.dtype)
    t_mat = lax.linalg.triangular_solve(eye + a_mat, jnp.broadcast_to(eye, a_mat.shape),
                                        left_side=True, lower=True)
    w_vals = jnp.einsum("bhnij,bhnjd->bhnid", t_mat, v_beta)
    k_cumdecay = jnp.einsum("bhnij,bhnjd->bhnid", t_mat, k_beta * jnp.exp(g)[..., None])
    attn_intra = jnp.where(causal, jnp.einsum("bhnid,bhnjd->bhnij", q, k) * decay, 0.0)
    g_last = g[..., -1]
    k_decay = k * jnp.exp(g_last[..., None] - g)[..., None]
    q_decay = q * jnp.exp(g)[..., None]
    xs = (jnp.moveaxis(q_decay, 2, 0), jnp.moveaxis(k_cumdecay, 2, 0), jnp.moveaxis(w_vals, 2, 0),
          jnp.moveaxis(attn_intra, 2, 0), jnp.moveaxis(k_decay, 2, 0), jnp.moveaxis(g_last, 2, 0))

    def step(state, inp):
        qd, kcd, wv, attn_c, kd, gl = inp
        v_new = wv - jnp.einsum("bhcd,bhde->bhce", kcd, state)
        o = jnp.einsum("bhcd,bhde->bhce", qd, state) + jnp.einsum("bhij,bhje->bhie", attn_c, v_new)
        state = state * jnp.exp(gl)[..., None, None] + jnp.einsum("bhcd,bhce->bhde", kd, v_new)
        return state, o

    state0 = jnp.zeros((b, h, dk, dv), dtype=q.dtype)
    _, o = lax.scan(step, state0, xs)
    return o.transpose(1, 0, 3, 2, 4).reshape(b, s, h, dv)


def _multiscale_pool(p, w_pool, pool_scale):
    b, s, _ = p.shape
    pf = p.astype(jnp.float32).reshape(b, s, POOL_GROUPS, POOL_GROUP_DIM)
    cs = jnp.cumsum(pf, axis=1)
    pos = jnp.arange(1, s + 1, dtype=jnp.int32)
    outs = []
    for gi, w in enumerate(POOL_WINDOWS):
        c = cs[:, :, gi]
        lower = jnp.pad(c, ((0, 0), (w, 0), (0, 0)))[:, :s]
        count = jnp.minimum(pos, w).astype(jnp.float32)[None, :, None]
        outs.append((c - lower) / count - pf[:, :, gi])
    pooled = jnp.stack(outs, axis=2)
    mixed = jnp.einsum("bsgc,gcd->bsgd", pooled, w_pool.astype(jnp.float32))
    return mixed.reshape(b, s, POOL_WIDTH) * pool_scale.astype(jnp.float32)


def _peer(xn, w_query, sub_keys, expert_down, expert_up):
    b, s, d = xn.shape
    blocks = xn.reshape(-1, PEER_TOKEN_BLOCK, d)
    sk = sub_keys.astype(jnp.float32)

    def block(xb):
        t = xb.shape[0]
        q = (xb @ w_query).astype(jnp.float32).reshape(t, PEER_HEADS, 2, PEER_HALF)
        scores = jnp.einsum("thpc,hpkc->thpk", q, sk)
        s1, i1 = lax.top_k(scores[:, :, 0], PEER_TOPK)
        s2, i2 = lax.top_k(scores[:, :, 1], PEER_TOPK)
        cand = (s1[..., :, None] + s2[..., None, :]).reshape(t, PEER_HEADS, PEER_TOPK * PEER_TOPK)
        cand_idx = (i1[..., :, None] * N_KEYS + i2[..., None, :]).reshape(t, PEER_HEADS, PEER_TOPK * PEER_TOPK)
        top_s, top_pos = lax.top_k(cand, PEER_TOPK)
        idx = jnp.take_along_axis(cand_idx, top_pos, axis=-1)
        gate = jax.nn.softmax(top_s, axis=-1)
        u = expert_down[idx]
        v = expert_up[idx]
        act = jax.nn.gelu(jnp.einsum("td,thkd->thk", xb, u).astype(jnp.float32), approximate=False)
        return jnp.einsum("thk,thkd->td", (gate * act).astype(v.dtype), v)

    return lax.map(block, blocks).reshape(b, s, d).astype(xn.dtype)


def setup_inputs(seed: int = 0) -> dict:
    key = jax.random.key(seed)
    ks = jax.random.split(key, 16)
    f32 = jnp.float32
    L = DEPTH

    def gain(k, shape):
        return 1.0 + 0.02 * jax.random.normal(k, shape, f32)

    x = jax.random.normal(ks[0], (BATCH, SEQ, D_MODEL), f32)
    norm_mix_w = gain(ks[1], (L, D_MODEL))
    w_in = jax.random.normal(ks[2], (L, D_MODEL, IN_WIDTH), f32) * D_MODEL ** -0.5
    conv_w = jax.random.normal(ks[3], (L, CONV_WIDTH, 3 * DN_WIDTH), f32) * CONV_WIDTH ** -0.5
    a_log = jnp.log(jax.random.uniform(ks[4], (L, DN_HEADS), f32, 1.0, 16.0))
    dt = jnp.exp(jax.random.uniform(ks[5], (L, DN_HEADS), f32, math.log(1e-3), math.log(1e-1)))
    dt_bias = dt + jnp.log(-jnp.expm1(-dt))
    dn_norm_w = gain(ks[6], (L, DN_HEAD_DIM))
    w_pool = jax.random.normal(ks[7], (L, POOL_GROUPS, POOL_GROUP_DIM, POOL_GROUP_DIM), f32) * POOL_GROUP_DIM ** -0.5
    pool_scale = gain(ks[8], (L, POOL_WIDTH))
    w_out = jax.random.normal(ks[9], (L, MIX_WIDTH, D_MODEL), f32) * MIX_WIDTH ** -0.5
    norm_ffn_w = gain(ks[10], (L, D_MODEL))
    w_query = jax.random.normal(ks[11], (L, D_MODEL, PEER_HEADS * PEER_QUERY_DIM), f32) * D_MODEL ** -0.5
    sub_keys = jax.random.normal(ks[12], (L, PEER_HEADS, 2, N_KEYS, PEER_HALF), f32) * PEER_HALF ** -0.5
    expert_down = jax.random.normal(ks[13], (L, N_EXPERTS, D_MODEL), f32) * D_MODEL ** -0.5
    expert_up = jax.random.normal(ks[14], (L, N_EXPERTS, D_MODEL), f32) * PEER_HEADS ** -0.5
    norm_final_w = gain(ks[15], (D_MODEL,))
    return {"x": x, "norm_mix_w": norm_mix_w, "w_in": w_in, "conv_w": conv_w, "a_log": a_log,
            "dt_bias": dt_bias, "dn_norm_w": dn_norm_w, "w_pool": w_pool, "pool_scale": pool_scale,
            "w_out": w_out, "norm_ffn_w": norm_ffn_w, "w_query": w_query, "sub_keys": sub_keys,
            "expert_down": expert_down, "expert_up": expert_up, "norm_final_w": norm_final_w}


def reference(x, norm_mix_w, w_in, conv_w, a_log, dt_bias, dn_norm_w, w_pool, pool_scale,
              w_out, norm_ffn_w, w_query, sub_keys, expert_down, expert_up, norm_final_w):
    b, s, _ = x.shape
    for l in range(DEPTH):
        h = _rmsnorm(x, norm_mix_w[l])
        proj = h @ w_in[l]
        qkv = proj[..., :3 * DN_WIDTH]
        z = proj[..., 3 * DN_WIDTH:4 * DN_WIDTH]
        b_gate = proj[..., 4 * DN_WIDTH:4 * DN_WIDTH + DN_HEADS]
        a_dec = proj[..., 4 * DN_WIDTH + DN_HEADS:4 * DN_WIDTH + 2 * DN_HEADS]
        p = proj[..., 4 * DN_WIDTH + 2 * DN_HEADS:]
        qkv = jax.nn.silu(_causal_conv(qkv, conv_w[l])).astype(jnp.float32)
        q = _l2norm(qkv[..., :DN_WIDTH].reshape(b, s, DN_HEADS, DN_HEAD_DIM))
        k = _l2norm(qkv[..., DN_WIDTH:2 * DN_WIDTH].reshape(b, s, DN_HEADS, DN_HEAD_DIM))
        v = qkv[..., 2 * DN_WIDTH:].reshape(b, s, DN_HEADS, DN_HEAD_DIM)
        beta = jax.nn.sigmoid(b_gate.astype(jnp.float32))
        g = -jnp.exp(a_log[l].astype(jnp.float32)) * jax.nn.softplus(
            a_dec.astype(jnp.float32) + dt_bias[l].astype(jnp.float32))
        o = _chunk_gated_delta_rule(q, k, v, g, beta)
        o = _rmsnorm(o, dn_norm_w[l]) * jax.nn.silu(
            z.astype(jnp.float32).reshape(b, s, DN_HEADS, DN_HEAD_DIM))
        o = o.reshape(b, s, DN_WIDTH).astype(x.dtype)
        pool_out = _multiscale_pool(p, w_pool[l], pool_scale[l]).astype(x.dtype)
        x = x + jnp.concatenate([o, pool_out], axis=-1) @ w_out[l]
        x = x + _peer(_rmsnorm(x, norm_ffn_w[l]), w_query[l], sub_keys[l], expert_down[l], expert_up[l])
    return _rmsnorm(x, norm_final_w)
```

```python
from contextlib import ExitStack
import numpy as np
import ml_dtypes
import concourse.bass as bass
import concourse.mybir as mybir
from concourse.bass_utils import run_bass_kernel_spmd

F32 = mybir.dt.float32
BF16 = mybir.dt.bfloat16
I32 = mybir.dt.int32
U32 = mybir.dt.uint32
AF = mybir.ActivationFunctionType
ALU = mybir.AluOpType
AX = mybir.AxisListType

ENG = ("pe", "act", "dve", "pool", "sp")
EPOCH = 20000
DEPOCH = 1200
NDMA = 8
EPS = 1e-6
BIG = 1.0e5
GRP = 4


class StopBuild(Exception):
    pass


class Tile:
    __slots__ = ("name", "t")

    def __init__(self, name, t):
        self.name = name
        self.t = t

    def __getitem__(self, k):
        return self.t[k]


class Plan:
    def __init__(self):
        self.streams = {e: [] for e in ENG}
        self.cnt = {e: 0 for e in ENG}
        self.lastw = {}
        self.readers = {}
        self.seen = {e: {} for e in ENG}
        self.dma_rr = {e: 0 for e in ENG}
        self.dma_n = {}
        self.dma_last = {}
        self.semkeys = []
        self.semset = set()
        self.stopped = False

    def _key(self, k):
        if k not in self.semset:
            self.semset.add(k)
            self.semkeys.append(k)
        return k

    def _need(self, e, ev, waits):
        grp, ep, val, key = ev
        cur = self.seen[e].get(grp)
        if cur is not None and cur >= (ep, val):
            return
        self.seen[e][grp] = (ep, val)
        waits.append((key, val))

    def op(self, e, fn, reads=(), writes=(), dma=False):
        if self.stopped:
            return None
        raw = []
        oth = []
        for b in reads:
            w = self.lastw.get(b)
            if w is not None:
                raw.append(w)
        for b in writes:
            w = self.lastw.get(b)
            if w is not None:
                oth.append(w)
            oth.extend(self.readers.get(b, ()))
        waits = []
        for ev in raw:
            self._need(e, ev, waits)
        for ev in oth:
            self._need(e, ev, waits)
        if dma:
            j = self.dma_rr[e] % NDMA
            self.dma_rr[e] += 1
            prev = self.dma_last.get((e, j))
            if prev is not None:
                self._need(e, prev, waits)
            n = self.dma_n.get((e, j), 0)
            self.dma_n[(e, j)] = n + 1
            ep = n // DEPOCH
            val = (n % DEPOCH + 1) * 16
            key = self._key(("d", e, j, ep))
            ev = (("d", e, j), ep, val, key)
            self.dma_last[(e, j)] = ev
            inc = 16
        else:
            n = self.cnt[e]
            self.cnt[e] = n + 1
            ep = n // EPOCH
            val = n % EPOCH + 1
            key = self._key(("e", e, ep))
            ev = (("e", e), ep, val, key)
            inc = 1
        self.streams[e].append((waits, fn, key, inc))
        for b in reads:
            self.readers.setdefault(b, []).append(ev)
        for b in writes:
            self.lastw[b] = ev
            self.readers[b] = []
        return ev

    def finish(self):
        waits = []
        for (e, j), ev in self.dma_last.items():
            self._need("sp", ev, waits)
        for e in ENG:
            if e == "sp" or self.cnt[e] == 0:
                continue
            n = self.cnt[e] - 1
            ep = n // EPOCH
            val = n % EPOCH + 1
            self._need("sp", (("e", e), ep, val, ("e", e, ep)), waits)
        self.streams["sp"].append((waits, None, None, 0))

    def emit(self, nc):
        with ExitStack() as es:
            sems = {}
            for i, k in enumerate(self.semkeys):
                sems[k] = es.enter_context(nc.semaphore("s%d" % i))
            with nc.Block() as block:
                def run(name):
                    def body(E):
                        for waits, fn, key, inc in self.streams[name]:
                            for (wk, wv) in waits:
                                E.wait_ge(sems[wk], wv)
                            if fn is not None:
                                fn(E).then_inc(sems[key], inc)
                    return body
                block.tensor(run("pe"))
                block.scalar(run("act"))
                block.vector(run("dve"))
                block.gpsimd(run("pool"))
                block.sync(run("sp"))


def host_consts():
    c = {}
    c["c_ident"] = np.eye(128, dtype=np.float32)
    c["c_ones"] = np.ones((128, 128), dtype=np.float32)
    k = np.arange(128)[:, None]
    f = np.arange(128)[None, :]
    c["c_U"] = (k <= f).astype(np.float32)
    m1 = np.where(k <= f, BIG, 0.0).astype(np.float32)
    m2 = np.where(f < k, -BIG, 0.0).astype(np.float32)
    c["c_M1"] = np.tile(m1, (1, 4))
    c["c_M2"] = np.tile(m2, (1, 4))
    wins = (2, 4, 8, 16)
    mc = np.zeros((4, 128, 128), np.float32)
    mp = np.zeros((4, 128, 128), np.float32)
    mf = np.zeros((4, 128, 128), np.float32)
    for gi, w in enumerate(wins):
        for t in range(128):
            for j in range(w):
                tp = t - j
                if tp >= 0:
                    mc[gi, tp, t] += 1.0 / w
                    mf[gi, tp, t] += 1.0 / min(t + 1, w)
                else:
                    mp[gi, tp + 128, t] += 1.0 / w
            mc[gi, t, t] -= 1.0
            mf[gi, t, t] -= 1.0
    c["c_MC"] = np.ascontiguousarray(mc.transpose(1, 0, 2).reshape(128, 512))
    c["c_MP"] = np.ascontiguousarray(mp.transpose(1, 0, 2).reshape(128, 512))
    c["c_MF"] = np.ascontiguousarray(mf.transpose(1, 0, 2).reshape(128, 512))
    c["c_iota"] = np.tile(np.arange(16, dtype=np.float32)[None, :], (128, 1))
    return c


CONST_SHAPES = {"c_ident": [128, 128], "c_ones": [128, 128], "c_U": [128, 128], "c_M1": [128, 512],
                "c_M2": [128, 512], "c_MC": [128, 512], "c_MP": [128, 512], "c_MF": [128, 512],
                "c_iota": [128, 16]}


def build(NT, mode="full"):
    nc = bass.Bass("TRN2", target_bir_lowering=False)
    T = NT * 128

    def din(name, shape, dt=F32):
        return nc.dram_tensor(name, shape, dt, kind="ExternalInput").ap()

    x_d = din("x", [T, 1024])
    nmix_d = din("norm_mix_w", [128, 8])
    win_d = din("w_in", [1024, 2568])
    cw_d = din("conv_w", [4, 1536])
    alog_d = din("a_log", [1, 4])
    dtb_d = din("dt_bias", [1, 4])
    dnw_d = din("dn_norm_w", [1, 128])
    wpool_d = din("w_pool", [4, 128, 128])
    pscale_d = din("pool_scale", [1, 512])
    wout_d = din("w_out", [1024, 1024])
    nffn_d = din("norm_ffn_w", [1, 1024])
    wq_d = din("w_query", [1024, 2048])
    sk_d = din("sub_keys", [16, 128, 128])
    ed_d = din("expert_down", [16384, 1024])
    eu_d = din("expert_up", [16384, 1024])
    nfin_d = din("norm_final_w", [1, 1024])
    cd = {k: din(k, s) for k, s in CONST_SHAPES.items()}
    out_d = nc.dram_tensor("out", [T, 1024], F32, kind="ExternalOutput").ap()
    x1_d = nc.dram_tensor("x1s", [T, 1024], F32, kind="Internal").ap()

    P = Plan()

    def MM(out, lhsT, rhs, r, w, start=True, stop=True):
        P.op("pe", lambda E: E.matmul(out, lhsT=lhsT, rhs=rhs, start=start, stop=stop), reads=r, writes=w)

    def TR(out, in_, ident, r, w):
        P.op("pe", lambda E: E.transpose(out=out, in_=in_, identity=ident), reads=r, writes=w)

    def ACT(out, in_, func, r, w, **kw):
        P.op("act", lambda E: E.activation(out=out, in_=in_, func=func, **kw), reads=r, writes=w)

    def TT(eng, out, in0, in1, op, r, w):
        P.op(eng, lambda E: E.tensor_tensor(out=out, in0=in0, in1=in1, op=op), reads=r, writes=w)

    def TS(eng, out, in0, s1, s2, op0, op1, r, w):
        if op1 is None:
            P.op(eng, lambda E: E.tensor_scalar(out=out, in0=in0, scalar1=s1, scalar2=None, op0=op0), reads=r, writes=w)
        else:
            P.op(eng, lambda E: E.tensor_scalar(out=out, in0=in0, scalar1=s1, scalar2=s2, op0=op0, op1=op1), reads=r, writes=w)

    def STT(out, in0, scalar, in1, op0, op1, r, w, accum=None):
        if accum is None:
            P.op("dve", lambda E: E.scalar_tensor_tensor(out=out, in0=in0, scalar=scalar, in1=in1, op0=op0, op1=op1), reads=r, writes=w)
        else:
            P.op("dve", lambda E: E.scalar_tensor_tensor(out=out, in0=in0, scalar=scalar, in1=in1, op0=op0, op1=op1, accum_out=accum), reads=r, writes=w)

    def CP(eng, out, in_, r, w):
        if eng == "act":
            P.op("act", lambda E: E.copy(out=out, in_=in_), reads=r, writes=w)
        else:
            P.op(eng, lambda E: E.tensor_copy(out=out, in_=in_), reads=r, writes=w)

    def RED(out, in_, r, w, op=ALU.add):
        P.op("dve", lambda E: E.tensor_reduce(out=out, in_=in_, axis=AX.X, op=op), reads=r, writes=w)

    def DMA(out, in_, r, w, eng="sp"):
        P.op(eng, lambda E: E.dma_start(out=out, in_=in_), reads=r, writes=w, dma=True)

    def RSTD(out, ss, scale, r_junk=None):
        ACT(out[:], ss[:], AF.Sqrt, [ss, epsb], [out], scale=scale, bias=epsb[:, 0:1])
        P.op("dve", lambda E: E.reciprocal(out=out[:], in_=out[:]), reads=[out], writes=[out])

    def STOP(tag, pieces):
        if mode == tag:
            col = 0
            for (ap, tl, n) in pieces:
                DMA(out_d[0:128, col:col + n], ap, [tl], [])
                col += n
            P.stopped = True

    with ExitStack() as es0:
        def sb0(name, shape, dt=F32):
            return Tile(name, es0.enter_context(nc.sbuf_tensor(name, shape, dt)))

        def ps0(name, shape, dt=F32):
            return Tile(name, es0.enter_context(nc.psum_tensor(name, shape, dt)))

        banks = [ps0("pb%d" % i, [128, 512]) for i in range(7)]
        pT = ps0("pT", [128, 1024], BF16)
        bstate = {"i": 0}

        def nb():
            b = banks[bstate["i"] % 7]
            bstate["i"] += 1
            return b

        ident = sb0("ident", [128, 128])
        identb = sb0("identb", [128, 128], BF16)
        ones = sb0("ones", [128, 128])
        epsb = sb0("epsb", [128, 1])
        xt = sb0("xt", [128, 1024])
        junk = sb0("junk", [128, 2568])
        ss = sb0("ss", [128, 1])
        rstd = sb0("rstd", [128, 1])
        hbf = sb0("hbf", [128, 1024], BF16)
        hT = sb0("hT", [128, 8, 128], BF16)
        stage = junk

        DMA(ident[:], cd["c_ident"], [], [ident])
        DMA(ones[:], cd["c_ones"], [], [ones])
        CP("dve", identb[:], ident[:], [ident], [identb])
        P.op("dve", lambda E: E.memset(epsb[:], EPS), writes=[epsb])

        def load_weight_bf(dst, src_d, ncols, scale_tile=None):
            for k in range(8):
                DMA(stage[:, 0:ncols], src_d[k * 128:(k + 1) * 128, :], [], [stage])
                if scale_tile is None:
                    CP("dve", dst[:, k, :], stage[:, 0:ncols], [stage], [dst])
                else:
                    TS("dve", dst[:, k, :], stage[:, 0:ncols], scale_tile[:, k:k + 1], None, ALU.mult, None,
                       [stage, scale_tile], [dst])

        def transpose8(src_bf, dstT):
            for k in range(8):
                TR(pT[:, k * 128:(k + 1) * 128], src_bf[:, k * 128:(k + 1) * 128], identb[:], [src_bf, identb], [pT])
            CP("act", dstT[:].rearrange("p k t -> p (k t)"), pT[:], [pT], [dstT])

        try:
          with ExitStack() as es1:
              def sb(name, shape, dt=F32):
                  return Tile(name, es1.enter_context(nc.sbuf_tensor(name, shape, dt)))

              Win = sb("Win", [128, 8, 2568], BF16)
              Wout = sb("Wout", [128, 8, 1024], BF16)
              nmix = sb("nmix", [128, 8])
              cwT = sb("cwT", [128, 12, 4])
              wpool = sb("wpool", [128, 4, 128])
              dnw_b = sb("dnw_b", [128, 128])
              pscale_b = sb("pscale_b", [128, 512])
              dtb_b = sb("dtb_b", [128, 4])
              negA = sb("negA", [128, 4])
              Um = sb("Um", [128, 128])
              M1 = sb("M1", [128, 512])
              M2 = sb("M2", [128, 512])
              MC = sb("MC", [128, 512])
              MPm = sb("MPm", [128, 512])
              MF = sb("MF", [128, 512])

              DMA(nmix[:], nmix_d, [], [nmix])
              load_weight_bf(Win, win_d, 2568, nmix)
              load_weight_bf(Wout, wout_d, 1024)
              DMA(stage[0:4, 0:1536], cw_d, [], [stage])
              bcw = nb()
              for c in range(12):
                  TR(bcw[:, c * 4:(c + 1) * 4], stage[0:4, c * 128:(c + 1) * 128], ident[0:4, 0:4], [stage, ident], [bcw])
              CP("act", cwT[:].rearrange("p c j -> p (c j)"), bcw[:, 0:48], [bcw], [cwT])
              DMA(wpool[:], wpool_d.rearrange("g c d -> c g d"), [], [wpool])
              DMA(dnw_b[:], dnw_d.partition_broadcast(128), [], [dnw_b])
              DMA(pscale_b[:], pscale_d.partition_broadcast(128), [], [pscale_b])
              DMA(dtb_b[:], dtb_d.partition_broadcast(128), [], [dtb_b])
              DMA(negA[:], alog_d.partition_broadcast(128), [], [negA])
              ACT(negA[:], negA[:], AF.Exp, [negA], [negA])
              TS("dve", negA[:], negA[:], -1.0, None, ALU.mult, None, [negA], [negA])
              for tl, nm in ((Um, "c_U"), (M1, "c_M1"), (M2, "c_M2"), (MC, "c_MC"), (MPm, "c_MP"), (MF, "c_MF")):
                  DMA(tl[:], cd[nm], [], [tl])

              qkvT = sb("qkvT", [128, 12, 131])
              cacc = sb("cacc", [128, 12, 128])
              qkv = sb("qkv", [128, 1536])
              proj2 = sb("proj2", [128, 1032])
              pprev = sb("pprev", [128, 512])
              S = sb("S", [128, 4, 128])
              qn = sb("qn", [128, 512])
              qg = sb("qg", [128, 512])
              kn = sb("kn", [128, 512])
              ss8 = sb("ss8", [128, 8])
              rn8 = sb("rn8", [128, 8])
              beta = sb("beta", [128, 4])
              nbeta = sb("nbeta", [128, 4])
              gt = sb("gt", [128, 4])
              gc = sb("gc", [128, 4])
              ngc = sb("ngc", [128, 4])
              glb = sb("glb", [128, 4])
              eg = sb("eg", [128, 4])
              egl = sb("egl", [128, 4])
              ekd = sb("ekd", [128, 4])
              bk = sb("bk", [128, 4])
              gU4 = sb("gU4", [128, 4, 128])
              decS = sb("decS", [128, 4, 128])
              decT = sb("decT", [128, 4, 128])
              vb = sb("vb", [128, 4, 128])
              kbg = sb("kbg", [128, 4, 128])
              kd = sb("kd", [128, 4, 128])
              kT4 = sb("kT4", [128, 4, 128])
              qT4 = sb("qT4", [128, 4, 128])
              qgT4 = sb("qgT4", [128, 4, 128])
              Qb = [sb("Qb%d" % i, [128, 128]) for i in range(2)]
              Pb = [sb("Pb%d" % i, [128, 128]) for i in range(2)]
              Xb = [sb("Xb%d" % i, [128, 128]) for i in range(2)]
              AT = sb("AT", [128, 128])
              nkcdT = sb("nkcdT", [128, 128])
              vnew = sb("vnew", [128, 128])
              osb = sb("osb", [128, 512])
              ss4 = sb("ss4", [128, 4])
              rn4 = sb("rn4", [128, 4])
              gz = sb("gz", [128, 512])
              mix = sb("mix", [128, 1024])
              plT = sb("plT", [128, 4, 128])

              P.op("dve", lambda E: E.memset(qkvT[:], 0.0), writes=[qkvT])
              P.op("dve", lambda E: E.memset(pprev[:], 0.0), writes=[pprev])
              P.op("dve", lambda E: E.memset(S[:], 0.0), writes=[S])

              for t in range(NT):
                  DMA(xt[:], x_d[t * 128:(t + 1) * 128, :], [], [xt])
                  ACT(junk[:, 0:1024], xt[:], AF.Square, [xt], [junk, ss], accum_out=ss[:])
                  RSTD(rstd, ss, 1.0 / 1024)
                  TS("dve", hbf[:], xt[:], rstd[:, 0:1], None, ALU.mult, None, [xt, rstd], [hbf])
                  transpose8(hbf, hT)
                  for b3 in range(3):
                      bk_ = nb()
                      for cc in range(4):
                          c = b3 * 4 + cc
                          for k in range(8):
                              MM(bk_[:, cc * 128:(cc + 1) * 128], Win[:, k, c * 128:(c + 1) * 128], hT[:, k, :],
                                 [Win, hT], [bk_], start=(k == 0), stop=(k == 7))
                      CP("act", qkvT[:, b3 * 4:(b3 + 1) * 4, 3:131],
                         bk_[:].rearrange("p (c t) -> p c t", c=4), [bk_], [qkvT])
                  pz = nb()
                  pg = nb()
                  pp = nb()
                  for k in range(8):
                      MM(pz[:, :], hT[:, k, :], Win[:, k, 1536:2048], [Win, hT], [pz], start=(k == 0), stop=(k == 7))
                  for k in range(8):
                      MM(pg[:, 0:8], hT[:, k, :], Win[:, k, 2048:2056], [Win, hT], [pg], start=(k == 0), stop=(k == 7))
                  for k in range(8):
                      MM(pp[:, :], hT[:, k, :], Win[:, k, 2056:2568], [Win, hT], [pp], start=(k == 0), stop=(k == 7))
                  CP("act", proj2[:, 0:512], pz[:], [pz], [proj2])
                  CP("act", proj2[:, 512:520], pg[:, 0:8], [pg], [proj2])
                  CP("act", proj2[:, 520:1032], pp[:], [pp], [proj2])
                  STOP("a", [(proj2[:, 0:1024], proj2, 1024)])
                  for c in range(12):
                      TS("dve", cacc[:, c, :], qkvT[:, c, 3:131], cwT[:, c, 3:4], None, ALU.mult, None, [qkvT, cwT], [cacc])
                      for j in range(3):
                          STT(cacc[:, c, :], qkvT[:, c, j:j + 128], cwT[:, c, j:j + 1], cacc[:, c, :], ALU.mult, ALU.add,
                              [qkvT, cwT, cacc], [cacc])
                  CP("dve", qkvT[:, :, 0:3], qkvT[:, :, 128:131], [qkvT], [qkvT])
                  ACT(cacc[:], cacc[:], AF.Silu, [cacc], [cacc])
                  for b3 in range(3):
                      bk_ = nb()
                      for cc in range(4):
                          c = b3 * 4 + cc
                          TR(bk_[:, cc * 128:(cc + 1) * 128], cacc[:, c, :], ident[:], [cacc, ident], [bk_])
                      CP("act", qkv[:, b3 * 512:(b3 + 1) * 512], bk_[:], [bk_], [qkv])
                  TT("dve", junk[:, 0:1024], qkv[:, 0:1024], qkv[:, 0:1024], ALU.mult, [qkv], [junk])
                  RED(ss8[:], junk[:, 0:1024].rearrange("p (h c) -> p h c", h=8), [junk], [ss8])
                  RSTD(rn8, ss8, 1.0)
                  TT("dve", kn[:].rearrange("p (h c) -> p h c", h=4), qkv[:, 512:1024].rearrange("p (h c) -> p h c", h=4),
                     rn8[:, 4:8].unsqueeze(2).to_broadcast([128, 4, 128]), ALU.mult, [qkv, rn8], [kn])
                  TS("dve", rn8[:, 0:4], rn8[:, 0:4], 128.0 ** -0.5, None, ALU.mult, None, [rn8], [rn8])
                  TT("dve", qn[:].rearrange("p (h c) -> p h c", h=4), qkv[:, 0:512].rearrange("p (h c) -> p h c", h=4),
                     rn8[:, 0:4].unsqueeze(2).to_broadcast([128, 4, 128]), ALU.mult, [qkv, rn8], [qn])
                  STOP("b", [(qn[:], qn, 512), (kn[:], kn, 512)])
                  ACT(beta[:], proj2[:, 512:516], AF.Sigmoid, [proj2], [beta])
                  TS("dve", nbeta[:], beta[:], -1.0, None, ALU.mult, None, [beta], [nbeta])
                  TT("dve", gt[:], proj2[:, 516:520], dtb_b[:], ALU.add, [proj2, dtb_b], [gt])
                  ACT(gt[:], gt[:], AF.Exp, [gt], [gt])
                  ACT(gt[:], gt[:], AF.Ln, [gt], [gt], bias=1.0)
                  TT("dve", gt[:], gt[:], negA[:], ALU.mult, [gt, negA], [gt])
                  pgc = nb()
                  MM(pgc[:, 0:4], Um[:], gt[:], [Um, gt], [pgc])
                  MM(pgc[:, 8:12], ones[:], gt[:], [ones, gt], [pgc])
                  CP("dve", gc[:], pgc[:, 0:4], [pgc], [gc])
                  CP("dve", glb[:], pgc[:, 8:12], [pgc], [glb])
                  TS("dve", ngc[:], gc[:], -1.0, None, ALU.mult, None, [gc], [ngc])
                  ACT(eg[:], gc[:], AF.Exp, [gc], [eg])
                  ACT(egl[:], glb[:], AF.Exp, [glb], [egl])
                  TT("dve", ekd[:], glb[:], gc[:], ALU.subtract, [glb, gc], [ekd])
                  ACT(ekd[:], ekd[:], AF.Exp, [ekd], [ekd])
                  TT("dve", bk[:], beta[:], eg[:], ALU.mult, [beta, eg], [bk])
                  TT("dve", gU4[:], Um[:].unsqueeze(1).to_broadcast([128, 4, 128]),
                     gt[:].unsqueeze(2).to_broadcast([128, 4, 128]), ALU.mult, [Um, gt], [gU4])
                  R1 = nb()
                  R2 = nb()
                  MM(R1[:], ones[:], gU4[:].rearrange("p h f -> p (h f)"), [ones, gU4], [R1], start=True, stop=False)
                  MM(R1[:], ident[:], M1[:], [ident, M1], [R1], start=False, stop=True)
                  MM(R2[:], ones[:], gU4[:].rearrange("p h f -> p (h f)"), [ones, gU4], [R2], start=True, stop=False)
                  MM(R2[:], ident[:], M2[:], [ident, M2], [R2], start=False, stop=True)
                  for h in range(4):
                      ACT(decS[:, h, :], R1[:, h * 128:(h + 1) * 128], AF.Exp, [R1, gc], [decS], scale=-1.0, bias=gc[:, h:h + 1])
                      ACT(decT[:, h, :], R2[:, h * 128:(h + 1) * 128], AF.Exp, [R2, ngc], [decT], scale=1.0, bias=ngc[:, h:h + 1])
                  v3 = qkv[:, 1024:1536].rearrange("p (h c) -> p h c", h=4)
                  kn3 = kn[:].rearrange("p (h c) -> p h c", h=4)
                  TT("dve", vb[:], v3, beta[:].unsqueeze(2).to_broadcast([128, 4, 128]), ALU.mult, [qkv, beta], [vb])
                  TT("dve", kbg[:], kn3, bk[:].unsqueeze(2).to_broadcast([128, 4, 128]), ALU.mult, [kn, bk], [kbg])
                  TT("dve", kd[:], kn3, ekd[:].unsqueeze(2).to_broadcast([128, 4, 128]), ALU.mult, [kn, ekd], [kd])
                  TT("dve", qg[:].rearrange("p (h c) -> p h c", h=4), qn[:].rearrange("p (h c) -> p h c", h=4),
                     eg[:].unsqueeze(2).to_broadcast([128, 4, 128]), ALU.mult, [qn, eg], [qg])
                  for (src, dst) in ((kn, kT4), (qn, qT4), (qg, qgT4)):
                      bk_ = nb()
                      for h in range(4):
                          TR(bk_[:, h * 128:(h + 1) * 128], src[:, h * 128:(h + 1) * 128], ident[:], [src, ident], [bk_])
                      CP("act", dst[:].rearrange("p h t -> p (h t)"), bk_[:], [bk_], [dst])
                      STOP("c", [(decS[:].rearrange("p h t -> p (h t)"), decS, 512), (kT4[:].rearrange("p h t -> p (h t)"), kT4, 512)])
                  for h in range(4):
                      pG = nb()
                      MM(pG[:, 0:128], kT4[:, h, :], kT4[:, h, :], [kT4], [pG])
                      MM(pG[:, 128:256], kT4[:, h, :], qT4[:, h, :], [kT4, qT4], [pG])
                      if mode == "d0a":
                          CP("act", AT[:], pG[:, 0:128], [pG], [AT])
                      STOP("d0a", [(AT[:], AT, 128)])
                      Q, Pm, X = Qb[0], Pb[0], Xb[0]
                      STT(Q[:], pG[:, 0:128], nbeta[:, h:h + 1], decS[:, h, :], ALU.mult, ALU.mult, [pG, nbeta, decS], [Q])
                      STOP("d0b", [(Q[:], Q, 128)])
                      TT("dve", AT[:], pG[:, 128:256], decT[:, h, :], ALU.mult, [pG, decT], [AT])
                      STOP("d0d", [(AT[:], AT, 128)])
                      pP = nb()
                      TR(pP[:, 0:128], Q[:], ident[:], [Q, ident], [pP])
                      CP("dve", Pm[:], pP[:, 0:128], [pP], [Pm])
                      STOP("d0c", [(Pm[:], Pm, 128)])
                      TT("dve", X[:], pP[:, 0:128], ident[:], ALU.add, [pP, ident], [X])
                      STOP("d1", [(Q[:], Q, 128), (AT[:], AT, 128), (Pm[:], Pm, 128), (X[:], X, 128)])
                      qi = pi = xi = 0
                      for m in range(6):
                          Qn_, Pn_, Xn_ = Qb[1 - qi], Pb[1 - pi], Xb[1 - xi]
                          pq = nb()
                          MM(pq[:, 0:128], Pm[:], Q[:], [Pm, Q], [pq])
                          if m < 5:
                              MM(pq[:, 128:256], Q[:], Pm[:], [Pm, Q], [pq])
                          CP("act", Qn_[:], pq[:, 0:128], [pq], [Qn_])
                          if m < 5:
                              CP("act", Pn_[:], pq[:, 128:256], [pq], [Pn_])
                          px = nb()
                          MM(px[:, 0:128], Qn_[:], X[:], [Qn_, X], [px])
                          TT("dve", Xn_[:], px[:, 0:128], X[:], ALU.add, [px, X], [Xn_])
                          Q, Pm, X = Qn_, Pn_, Xn_
                          qi, pi, xi = 1 - qi, 1 - pi, 1 - xi
                          STOP("d2%d" % m, [(Q[:], Q, 128), (X[:], X, 128)])
                      pk = nb()
                      MM(pk[:, 0:128], kbg[:, h, :], X[:], [kbg, X], [pk])
                      TS("dve", nkcdT[:], pk[:, 0:128], -1.0, None, ALU.mult, None, [pk], [nkcdT])
                      pv = nb()
                      MM(pv[:, 0:128], X[:], vb[:, h, :], [X, vb], [pv], start=True, stop=False)
                      MM(pv[:, 0:128], nkcdT[:], S[:, h, :], [nkcdT, S], [pv], start=False, stop=True)
                      CP("act", vnew[:], pv[:, 0:128], [pv], [vnew])
                      STOP("d3", [(vnew[:], vnew, 128), (nkcdT[:], nkcdT, 128)])
                      po = nb()
                      MM(po[:, 0:128], qgT4[:, h, :], S[:, h, :], [qgT4, S], [po], start=True, stop=False)
                      MM(po[:, 0:128], AT[:], vnew[:], [AT, vnew], [po], start=False, stop=True)
                      MM(po[:, 128:256], kd[:, h, :], vnew[:], [kd, vnew], [po])
                      CP("dve", osb[:, h * 128:(h + 1) * 128], po[:, 0:128], [po], [osb])
                      STT(S[:, h, :], S[:, h, :], egl[:, h:h + 1], po[:, 128:256], ALU.mult, ALU.add, [S, egl, po], [S])
                      STOP("d", [(osb[:], osb, 512), (S[:].rearrange("p h t -> p (h t)"), S, 512)])
                  TT("dve", junk[:, 0:512], osb[:], osb[:], ALU.mult, [osb], [junk])
                  RED(ss4[:], junk[:, 0:512].rearrange("p (h c) -> p h c", h=4), [junk], [ss4])
                  RSTD(rn4, ss4, 1.0 / 128)
                  ACT(gz[:], proj2[:, 0:512], AF.Silu, [proj2], [gz])
                  TT("dve", gz[:].rearrange("p (h c) -> p h c", h=4), gz[:].rearrange("p (h c) -> p h c", h=4),
                     dnw_b[:].unsqueeze(1).to_broadcast([128, 4, 128]), ALU.mult, [gz, dnw_b], [gz])
                  TT("dve", osb[:].rearrange("p (h c) -> p h c", h=4), osb[:].rearrange("p (h c) -> p h c", h=4),
                     rn4[:].unsqueeze(2).to_broadcast([128, 4, 128]), ALU.mult, [osb, rn4], [osb])
                  TT("dve", mix[:, 0:512], osb[:], gz[:], ALU.mult, [osb, gz], [mix])
                  ppl = nb()
                  Mcur = MF if t == 0 else MC
                  for g in range(4):
                      MM(ppl[:, g * 128:(g + 1) * 128], proj2[:, 520 + g * 128:520 + (g + 1) * 128], Mcur[:, g * 128:(g + 1) * 128],
                         [proj2, Mcur], [ppl], start=True, stop=False)
                      MM(ppl[:, g * 128:(g + 1) * 128], pprev[:, g * 128:(g + 1) * 128], MPm[:, g * 128:(g + 1) * 128],
                         [pprev, MPm], [ppl], start=False, stop=True)
                  CP("act", plT[:].rearrange("p g t -> p (g t)"), ppl[:], [ppl], [plT])
                  CP("dve", pprev[:], proj2[:, 520:1032], [proj2], [pprev])
                  pmx = nb()
                  for g in range(4):
                      MM(pmx[:, g * 128:(g + 1) * 128], plT[:, g, :], wpool[:, g, :], [plT, wpool], [pmx])
                  TT("dve", mix[:, 512:1024], pmx[:], pscale_b[:], ALU.mult, [pmx, pscale_b], [mix])
                  CP("dve", hbf[:], mix[:], [mix], [hbf])
                  transpose8(hbf, hT)
                  for n in range(2):
                      py = nb()
                      for k in range(8):
                          MM(py[:], hT[:, k, :], Wout[:, k, n * 512:(n + 1) * 512], [hT, Wout], [py], start=(k == 0), stop=(k == 7))
                      TT("dve", xt[:, n * 512:(n + 1) * 512], xt[:, n * 512:(n + 1) * 512], py[:], ALU.add, [xt, py], [xt])
                  DMA(x1_d[t * 128:(t + 1) * 128, :], xt[:], [xt], ["x1d%d" % t])
                  if mode == "p1":
                      DMA(out_d[t * 128:(t + 1) * 128, :], xt[:], [xt], [])

        except StopBuild:
            pass
        with ExitStack() as es2:
          if mode != "p1":
              def sb(name, shape, dt=F32):
                  return Tile(name, es2.enter_context(nc.sbuf_tensor(name, shape, dt)))

              Wq = sb("Wq", [128, 8, 2048], BF16)
              skT = sb("skT", [128, 16, 128])
              nffn_b = sb("nffn_b", [128, 1024])
              nfin_b = sb("nfin_b", [128, 1024])
              iota16 = sb("iota16", [128, 16])
              xn = sb("xn", [128, 1024])
              qTs = sb("qTs", [128, 16, 128])
              s16 = sb("s16", [128, 16, 16])
              i16 = sb("i16", [128, 16, 16], U32)
              if16 = sb("if16", [128, 16, 16])
              work = sb("work", [128, 256])
              cand = sb("cand", [128, 8, 256])
              ts = sb("ts", [128, 8, 16])
              pos = sb("pos", [128, 8, 16], U32)
              posi = sb("posi", [128, 8, 16], U32)
              pa = sb("pa", [128, 8, 16])
              pbm = sb("pbm", [128, 8, 16])
              eq = sb("eq", [128, 8, 256])
              eidx = sb("eidx", [128, 8, 16])
              eidx2 = sb("eidx2", [128, 8, 16])
              eidi = sb("eidi", [128, 128], I32)
              gate = sb("gate", [128, 8, 16])
              gsum = sb("gsum", [128, 8])
              araw = sb("araw", [128, 128])
              coef = sb("coef", [128, 128])
              NB = 2 * GRP
              Ub = [sb("Ub%d" % i, [128, 1024]) for i in range(NB)]
              Vb = [sb("Vb%d" % i, [128, 1024]) for i in range(NB)]

              load_weight_bf(Wq, wq_d, 2048)
              DMA(nffn_b[:], nffn_d.partition_broadcast(128), [], [nffn_b])
              DMA(nfin_b[:], nfin_d.partition_broadcast(128), [], [nfin_b])
              DMA(iota16[:], cd["c_iota"], [], [iota16])
              for b4 in range(4):
                  DMA(stage[:, 0:512].rearrange("p (b c) -> p b c", b=4), sk_d[b4 * 4:(b4 + 1) * 4].rearrange("b k c -> k b c"),
                      [], [stage])
                  bk_ = nb()
                  for bb in range(4):
                      TR(bk_[:, bb * 128:(bb + 1) * 128], stage[:, bb * 128:(bb + 1) * 128], ident[:], [stage, ident], [bk_])
                  CP("act", skT[:, b4 * 4:(b4 + 1) * 4, :].rearrange("p b k -> p (b k)"), bk_[:], [bk_], [skT])

              gidx = 0
              for t in range(NT):
                  DMA(xt[:], x1_d[t * 128:(t + 1) * 128, :], ["x1d%d" % t], [xt])
                  ACT(junk[:, 0:1024], xt[:], AF.Square, [xt], [junk, ss], accum_out=ss[:])
                  RSTD(rstd, ss, 1.0 / 1024)
                  STT(xn[:], xt[:], rstd[:, 0:1], nffn_b[:], ALU.mult, ALU.mult, [xt, rstd, nffn_b], [xn])
                  CP("act", hbf[:], xn[:], [xn], [hbf])
                  transpose8(hbf, hT)
                  for b4 in range(4):
                      bk_ = nb()
                      for bb in range(4):
                          blk = b4 * 4 + bb
                          for k in range(8):
                              MM(bk_[:, bb * 128:(bb + 1) * 128], Wq[:, k, blk * 128:(blk + 1) * 128], hT[:, k, :],
                                 [Wq, hT], [bk_], start=(k == 0), stop=(k == 7))
                      CP("act", qTs[:, b4 * 4:(b4 + 1) * 4, :].rearrange("p b t -> p (b t)"), bk_[:], [bk_], [qTs])
                  for b4 in range(4):
                      bk_ = nb()
                      for bb in range(4):
                          blk = b4 * 4 + bb
                          MM(bk_[:, bb * 128:(bb + 1) * 128], qTs[:, blk, :], skT[:, blk, :], [qTs, skT], [bk_])
                      for bb in range(4):
                          blk = b4 * 4 + bb
                          sc = bk_[:, bb * 128:(bb + 1) * 128]
                          P.op("dve", lambda E, sc=sc, blk=blk: E.max(out=s16[:, blk, 0:8], in_=sc), reads=[bk_], writes=[s16])
                          P.op("dve", lambda E, sc=sc, blk=blk: E.max_index(out=i16[:, blk, 0:8], in_max=s16[:, blk, 0:8], in_values=sc),
                               reads=[bk_, s16], writes=[i16])
                          P.op("dve", lambda E, sc=sc, blk=blk: E.match_replace(out=work[:, 0:128], in_to_replace=s16[:, blk, 0:8],
                                                                               in_values=sc, imm_value=-1e30),
                               reads=[bk_, s16], writes=[work])
                          P.op("dve", lambda E, blk=blk: E.max(out=s16[:, blk, 8:16], in_=work[:, 0:128]), reads=[work], writes=[s16])
                          P.op("dve", lambda E, blk=blk: E.max_index(out=i16[:, blk, 8:16], in_max=s16[:, blk, 8:16],
                                                                    in_values=work[:, 0:128]), reads=[work, s16], writes=[i16])
                  CP("dve", if16[:], i16[:], [i16], [if16])
                  s4 = s16[:].rearrange("p (h two) k -> p h two k", two=2)
                  i4 = if16[:].rearrange("p (h two) k -> p h two k", two=2)
                  TT("dve", cand[:].rearrange("p h (a b) -> p h a b", a=16),
                     s4[:, :, 0, :].unsqueeze(3).to_broadcast([128, 8, 16, 16]),
                     s4[:, :, 1, :].unsqueeze(2).to_broadcast([128, 8, 16, 16]), ALU.add, [s16], [cand])
                  for h in range(8):
                      P.op("dve", lambda E, h=h: E.max(out=ts[:, h, 0:8], in_=cand[:, h, :]), reads=[cand], writes=[ts])
                      P.op("dve", lambda E, h=h: E.max_index(out=pos[:, h, 0:8], in_max=ts[:, h, 0:8], in_values=cand[:, h, :]),
                           reads=[cand, ts], writes=[pos])
                      P.op("dve", lambda E, h=h: E.match_replace(out=work[:], in_to_replace=ts[:, h, 0:8], in_values=cand[:, h, :],
                                                                 imm_value=-1e30), reads=[cand, ts], writes=[work])
                      P.op("dve", lambda E, h=h: E.max(out=ts[:, h, 8:16], in_=work[:]), reads=[work], writes=[ts])
                      P.op("dve", lambda E, h=h: E.max_index(out=pos[:, h, 8:16], in_max=ts[:, h, 8:16], in_values=work[:]),
                           reads=[work, ts], writes=[pos])
                  TS("dve", posi[:], pos[:], 15, None, ALU.bitwise_and, None, [pos], [posi])
                  CP("dve", pbm[:], posi[:], [posi], [pbm])
                  TS("dve", posi[:], pos[:], 4, None, ALU.logical_shift_right, None, [pos], [posi])
                  CP("dve", pa[:], posi[:], [posi], [pa])
                  eq4 = eq[:].rearrange("p h (k a) -> p h k a", k=16)
                  io4 = iota16[:].unsqueeze(1).unsqueeze(1).to_broadcast([128, 8, 16, 16])
                  TT("dve", eq4, pa[:].unsqueeze(3).to_broadcast([128, 8, 16, 16]), io4, ALU.is_equal, [pa, iota16], [eq])
                  TT("dve", eq4, eq4, i4[:, :, 0, :].unsqueeze(2).to_broadcast([128, 8, 16, 16]), ALU.mult, [eq, if16], [eq])
                  RED(eidx[:], eq4, [eq], [eidx])
                  TT("dve", eq4, pbm[:].unsqueeze(3).to_broadcast([128, 8, 16, 16]), io4, ALU.is_equal, [pbm, iota16], [eq])
                  TT("dve", eq4, eq4, i4[:, :, 1, :].unsqueeze(2).to_broadcast([128, 8, 16, 16]), ALU.mult, [eq, if16], [eq])
                  RED(eidx2[:], eq4, [eq], [eidx2])
                  STT(eidx[:].rearrange("p h k -> p (h k)"), eidx[:].rearrange("p h k -> p (h k)"), 128.0,
                      eidx2[:].rearrange("p h k -> p (h k)"), ALU.mult, ALU.add, [eidx, eidx2], [eidx])
                  CP("dve", eidi[:], eidx[:].rearrange("p h k -> p (h k)"), [eidx], [eidi])
                  TT("dve", gate[:], ts[:], ts[:, :, 0:1].to_broadcast([128, 8, 16]), ALU.subtract, [ts], [gate])
                  ACT(gate[:], gate[:], AF.Exp, [gate], [gate])
                  RED(gsum[:], gate[:], [gate], [gsum])
                  P.op("dve", lambda E: E.reciprocal(out=gsum[:], in_=gsum[:]), reads=[gsum], writes=[gsum])
                  TT("dve", gate[:], gate[:], gsum[:].unsqueeze(2).to_broadcast([128, 8, 16]), ALU.mult, [gate, gsum], [gate])
                  gflat = gate[:].rearrange("p h k -> p (h k)")
                  P.op("dve", lambda E: E.memset(araw[:], 0.0), writes=[araw])
                  for g0 in range(0, 128, GRP):
                      bufs = []
                      for s in range(g0, g0 + GRP):
                          bi = gidx % NB
                          gidx += 1
                          U, V = Ub[bi], Vb[bi]
                          bufs.append((s, U, V))
                          P.op("pool", lambda E, U=U, s=s: E.indirect_dma_start(
                              out=U[:], out_offset=None, in_=ed_d,
                              in_offset=bass.IndirectOffsetOnAxis(ap=eidi[:, s:s + 1], axis=0)),
                              reads=[eidi], writes=[U], dma=True)
                          P.op("pool", lambda E, V=V, s=s: E.indirect_dma_start(
                              out=V[:], out_offset=None, in_=eu_d,
                              in_offset=bass.IndirectOffsetOnAxis(ap=eidi[:, s:s + 1], axis=0)),
                              reads=[eidi], writes=[V], dma=True)
                      for (s, U, V) in bufs:
                          STT(junk[:, 0:1024], U[:], 1.0, xn[:], ALU.mult, ALU.mult, [U, xn], [junk, araw], accum=araw[:, s:s + 1])
                      ACT(coef[:, g0:g0 + GRP], araw[:, g0:g0 + GRP], AF.Gelu, [araw], [coef])
                      TT("dve", coef[:, g0:g0 + GRP], coef[:, g0:g0 + GRP], gflat[:, g0:g0 + GRP], ALU.mult, [coef, gate], [coef])
                      for (s, U, V) in bufs:
                          STT(xt[:], V[:], coef[:, s:s + 1], xt[:], ALU.mult, ALU.add, [V, coef, xt], [xt])
                  ACT(junk[:, 0:1024], xt[:], AF.Square, [xt], [junk, ss], accum_out=ss[:])
                  RSTD(rstd, ss, 1.0 / 1024)
                  STT(xn[:], xt[:], rstd[:, 0:1], nfin_b[:], ALU.mult, ALU.mult, [xt, rstd, nfin_b], [xn])
                  DMA(out_d[t * 128:(t + 1) * 128, :], xn[:], [xn], [])

        P.finish()
        P.emit(nc)
    return nc


_CACHE = {}


def make_in_maps(inputs, ncores, NT):
    consts = host_consts()
    f = lambda a: np.ascontiguousarray(np.asarray(a, dtype=np.float32))
    shared = {
        "norm_mix_w": np.ascontiguousarray(f(inputs["norm_mix_w"]).reshape(8, 128).T),
        "w_in": f(inputs["w_in"]).reshape(1024, 2568),
        "conv_w": f(inputs["conv_w"]).reshape(4, 1536),
        "a_log": f(inputs["a_log"]).reshape(1, 4),
        "dt_bias": f(inputs["dt_bias"]).reshape(1, 4),
        "dn_norm_w": f(inputs["dn_norm_w"]).reshape(1, 128),
        "w_pool": f(inputs["w_pool"]).reshape(4, 128, 128),
        "pool_scale": f(inputs["pool_scale"]).reshape(1, 512),
        "w_out": f(inputs["w_out"]).reshape(1024, 1024),
        "norm_ffn_w": f(inputs["norm_ffn_w"]).reshape(1, 1024),
        "w_query": f(inputs["w_query"]).reshape(1024, 2048),
        "sub_keys": f(inputs["sub_keys"]).reshape(16, 128, 128),
        "expert_down": f(inputs["expert_down"]).reshape(16384, 1024),
        "expert_up": f(inputs["expert_up"]).reshape(16384, 1024),
        "norm_final_w": f(inputs["norm_final_w"]).reshape(1, 1024),
    }
    shared.update(consts)
    x = f(inputs["x"])
    maps = []
    for c in range(ncores):
        m = dict(shared)
        m["x"] = np.ascontiguousarray(x[c, :NT * 128, :])
        maps.append(m)
    return maps


def kernel(**inputs):
    NT = 64
    ncores = 8
    if "nc" not in _CACHE:
        _CACHE["nc"] = build(NT)
    nc = _CACHE["nc"]
    maps = make_in_maps(inputs, ncores, NT)
    res = run_bass_kernel_spmd(nc, maps, core_ids=list(range(ncores)))
    out = np.stack([np.asarray(r["out"]) for r in res.results], axis=0)
    return out.astype(np.float32)
```

```python
from contextlib import ExitStack
import numpy as np
import ml_dtypes
import concourse.bass as bass
import concourse.mybir as mybir
from concourse.bass_utils import run_bass_kernel_spmd

F32 = mybir.dt.float32
F32R = mybir.dt.float32r
BF16 = mybir.dt.bfloat16
I32 = mybir.dt.int32
U32 = mybir.dt.uint32
AF = mybir.ActivationFunctionType
ALU = mybir.AluOpType
AX = mybir.AxisListType

ENG = ("pe", "act", "dve", "pool", "sp")
EPOCH = 20000
DEPOCH = 1200
NDMA = 8
EPS = 1e-6
BIG = 1.0e5
GRP = 4


class StopBuild(Exception):
    pass


class Tile:
    __slots__ = ("name", "t")

    def __init__(self, name, t):
        self.name = name
        self.t = t

    def __getitem__(self, k):
        return self.t[k]


class Plan:
    def __init__(self):
        self.streams = {e: [] for e in ENG}
        self.cnt = {e: 0 for e in ENG}
        self.lastw = {}
        self.readers = {}
        self.seen = {e: {} for e in ENG}
        self.dma_rr = {e: 0 for e in ENG}
        self.dma_n = {}
        self.dma_last = {}
        self.semkeys = []
        self.semset = set()
        self.stopped = False

    def _key(self, k):
        if k not in self.semset:
            self.semset.add(k)
            self.semkeys.append(k)
        return k

    def _need(self, e, ev, waits):
        grp, ep, val, key = ev
        cur = self.seen[e].get(grp)
        if cur is not None and cur >= (ep, val):
            return
        self.seen[e][grp] = (ep, val)
        waits.append((key, val))

    def op(self, e, fn, reads=(), writes=(), dma=False):
        if self.stopped:
            return None
        raw = []
        oth = []
        for b in reads:
            w = self.lastw.get(b)
            if w is not None:
                raw.append(w)
        for b in writes:
            w = self.lastw.get(b)
            if w is not None:
                oth.append(w)
            oth.extend(self.readers.get(b, ()))
        waits = []
        for ev in raw:
            self._need(e, ev, waits)
        for ev in oth:
            self._need(e, ev, waits)
        if dma:
            j = self.dma_rr[e] % NDMA
            self.dma_rr[e] += 1
            prev = self.dma_last.get((e, j))
            if prev is not None:
                self._need(e, prev, waits)
            n = self.dma_n.get((e, j), 0)
            self.dma_n[(e, j)] = n + 1
            ep = n // DEPOCH
            val = (n % DEPOCH + 1) * 16
            key = self._key(("d", e, j, ep))
            ev = (("d", e, j), ep, val, key)
            self.dma_last[(e, j)] = ev
            inc = 16
        else:
            n = self.cnt[e]
            self.cnt[e] = n + 1
            ep = n // EPOCH
            val = n % EPOCH + 1
            key = self._key(("e", e, ep))
            ev = (("e", e), ep, val, key)
            inc = 1
        self.streams[e].append((waits, fn, key, inc))
        for b in reads:
            self.readers.setdefault(b, []).append(ev)
        for b in writes:
            self.lastw[b] = ev
            self.readers[b] = []
        return ev

    def finish(self):
        waits = []
        for (e, j), ev in self.dma_last.items():
            self._need("sp", ev, waits)
        for e in ENG:
            if e == "sp" or self.cnt[e] == 0:
                continue
            n = self.cnt[e] - 1
            ep = n // EPOCH
            val = n % EPOCH + 1
            self._need("sp", (("e", e), ep, val, ("e", e, ep)), waits)
        self.streams["sp"].append((waits, None, None, 0))

    def emit(self, nc):
        with ExitStack() as es:
            sems = {}
            for i, k in enumerate(self.semkeys):
                sems[k] = es.enter_context(nc.semaphore("s%d" % i))
            with nc.Block() as block:
                def run(name):
                    def body(E):
                        for waits, fn, key, inc in self.streams[name]:
                            for (wk, wv) in waits:
                                E.wait_ge(sems[wk], wv)
                            if fn is not None:
                                fn(E).then_inc(sems[key], inc)
                    return body
                block.tensor(run("pe"))
                block.scalar(run("act"))
                block.vector(run("dve"))
                block.gpsimd(run("pool"))
                block.sync(run("sp"))


def host_consts():
    c = {}
    c["c_ident"] = np.eye(128, dtype=np.float32)
    c["c_ones"] = np.ones((128, 128), dtype=np.float32)
    k = np.arange(128)[:, None]
    f = np.arange(128)[None, :]
    c["c_U"] = (k <= f).astype(np.float32)
    m1 = np.where(k <= f, BIG, 0.0).astype(np.float32)
    m2 = np.where(f < k, -BIG, 0.0).astype(np.float32)
    c["c_M1"] = np.tile(m1, (1, 4))
    c["c_M2"] = np.tile(m2, (1, 4))
    wins = (2, 4, 8, 16)
    mc = np.zeros((4, 128, 128), np.float32)
    mp = np.zeros((4, 128, 128), np.float32)
    mf = np.zeros((4, 128, 128), np.float32)
    for gi, w in enumerate(wins):
        for t in range(128):
            for j in range(w):
                tp = t - j
                if tp >= 0:
                    mc[gi, tp, t] += 1.0 / w
                    mf[gi, tp, t] += 1.0 / min(t + 1, w)
                else:
                    mp[gi, tp + 128, t] += 1.0 / w
            mc[gi, t, t] -= 1.0
            mf[gi, t, t] -= 1.0
    c["c_MC"] = np.ascontiguousarray(mc.transpose(1, 0, 2).reshape(128, 512))
    c["c_MP"] = np.ascontiguousarray(mp.transpose(1, 0, 2).reshape(128, 512))
    c["c_MF"] = np.ascontiguousarray(mf.transpose(1, 0, 2).reshape(128, 512))
    c["c_iota"] = np.tile(np.arange(16, dtype=np.float32)[None, :], (128, 1))
    return c


CONST_SHAPES = {"c_ident": [128, 128], "c_ones": [128, 128], "c_U": [128, 128], "c_M1": [128, 512],
                "c_M2": [128, 512], "c_MC": [128, 512], "c_MP": [128, 512], "c_MF": [128, 512],
                "c_iota": [128, 16]}


def build(NT, mode="full"):
    nc = bass.Bass("TRN2", target_bir_lowering=False)
    T = NT * 128

    def din(name, shape, dt=F32):
        return nc.dram_tensor(name, shape, dt, kind="ExternalInput").ap()

    x_d = din("x", [T, 1024])
    nmix_d = din("norm_mix_w", [128, 8])
    win_d = din("w_in", [1024, 2568])
    cw_d = din("conv_w", [4, 1536])
    alog_d = din("a_log", [1, 4])
    dtb_d = din("dt_bias", [1, 4])
    dnw_d = din("dn_norm_w", [1, 128])
    wpool_d = din("w_pool", [4, 128, 128])
    pscale_d = din("pool_scale", [1, 512])
    wout_d = din("w_out", [1024, 1024])
    nffn_d = din("norm_ffn_w", [1, 1024])
    wq_d = din("w_query", [1024, 2048])
    sk_d = din("sub_keys", [16, 128, 128])
    ec_d = din("expert_cat", [16384, 2048])
    nfin_d = din("norm_final_w", [1, 1024])
    cd = {k: din(k, s) for k, s in CONST_SHAPES.items()}
    out_d = nc.dram_tensor("out", [T, 1024], F32, kind="ExternalOutput").ap()
    x1_d = nc.dram_tensor("x1s", [T, 1024], F32, kind="Internal").ap()

    P = Plan()

    def MM(out, lhsT, rhs, r, w, start=True, stop=True):
        P.op("pe", lambda E: E.matmul(out, lhsT=lhsT, rhs=rhs, start=start, stop=stop), reads=r, writes=w)

    def TR(out, in_, ident, r, w):
        P.op("pe", lambda E: E.transpose(out=out, in_=in_, identity=ident), reads=r, writes=w)

    def ACT(out, in_, func, r, w, **kw):
        P.op("act", lambda E: E.activation(out=out, in_=in_, func=func, **kw), reads=r, writes=w)

    def TT(eng, out, in0, in1, op, r, w):
        P.op(eng, lambda E: E.tensor_tensor(out=out, in0=in0, in1=in1, op=op), reads=r, writes=w)

    def TS(eng, out, in0, s1, s2, op0, op1, r, w):
        if op1 is None:
            P.op(eng, lambda E: E.tensor_scalar(out=out, in0=in0, scalar1=s1, scalar2=None, op0=op0), reads=r, writes=w)
        else:
            P.op(eng, lambda E: E.tensor_scalar(out=out, in0=in0, scalar1=s1, scalar2=s2, op0=op0, op1=op1), reads=r, writes=w)

    def STT(out, in0, scalar, in1, op0, op1, r, w, accum=None):
        if accum is None:
            P.op("dve", lambda E: E.scalar_tensor_tensor(out=out, in0=in0, scalar=scalar, in1=in1, op0=op0, op1=op1), reads=r, writes=w)
        else:
            P.op("dve", lambda E: E.scalar_tensor_tensor(out=out, in0=in0, scalar=scalar, in1=in1, op0=op0, op1=op1, accum_out=accum), reads=r, writes=w)

    def CP(eng, out, in_, r, w):
        if eng == "act":
            P.op("act", lambda E: E.copy(out=out, in_=in_), reads=r, writes=w)
        else:
            P.op(eng, lambda E: E.tensor_copy(out=out, in_=in_), reads=r, writes=w)

    def RED(out, in_, r, w, op=ALU.add):
        P.op("dve", lambda E: E.tensor_reduce(out=out, in_=in_, axis=AX.X, op=op), reads=r, writes=w)

    def DMA(out, in_, r, w, eng="sp"):
        P.op(eng, lambda E: E.dma_start(out=out, in_=in_), reads=r, writes=w, dma=True)

    def RSTD(out, ss, scale, r_junk=None):
        ACT(out[:], ss[:], AF.Sqrt, [ss, epsb], [out], scale=scale, bias=epsb[:, 0:1])
        P.op("dve", lambda E: E.reciprocal(out=out[:], in_=out[:]), reads=[out], writes=[out])

    def STOP(tag, pieces):
        if mode == tag:
            col = 0
            for (ap, tl, n) in pieces:
                DMA(out_d[0:128, col:col + n], ap, [tl], [])
                col += n
            P.stopped = True

    with ExitStack() as es0:
        def sb0(name, shape, dt=F32):
            return Tile(name, es0.enter_context(nc.sbuf_tensor(name, shape, dt)))

        def ps0(name, shape, dt=F32):
            return Tile(name, es0.enter_context(nc.psum_tensor(name, shape, dt)))

        banks = [ps0("pb%d" % i, [128, 512]) for i in range(7)]
        pT = ps0("pT", [128, 1024], BF16)
        bstate = {"i": 0, "n": 7}

        def nb():
            b = banks[bstate["i"] % bstate["n"]]
            bstate["i"] += 1
            return b

        ident = sb0("ident", [128, 128])
        identb = sb0("identb", [128, 128], BF16)
        ones = sb0("ones", [128, 128])
        epsb = sb0("epsb", [128, 1])
        xt = sb0("xt", [128, 1024])
        junk = sb0("junk", [128, 2568])
        ss = sb0("ss", [128, 1])
        rstd = sb0("rstd", [128, 1])
        hbf = sb0("hbf", [128, 1024], BF16)
        hT = sb0("hT", [128, 8, 128], BF16)
        stage = junk

        DMA(ident[:], cd["c_ident"], [], [ident])
        DMA(ones[:], cd["c_ones"], [], [ones])
        CP("dve", identb[:], ident[:], [ident], [identb])
        P.op("dve", lambda E: E.memset(epsb[:], EPS), writes=[epsb])

        def load_weight_bf(dst, src_d, ncols, scale_tile=None):
            for k in range(8):
                DMA(stage[:, 0:ncols], src_d[k * 128:(k + 1) * 128, :], [], [stage])
                if scale_tile is None:
                    CP("dve", dst[:, k, :], stage[:, 0:ncols], [stage], [dst])
                else:
                    TS("dve", dst[:, k, :], stage[:, 0:ncols], scale_tile[:, k:k + 1], None, ALU.mult, None,
                       [stage, scale_tile], [dst])

        def transpose8(src_bf, dstT):
            for k in range(8):
                TR(pT[:, k * 128:(k + 1) * 128], src_bf[:, k * 128:(k + 1) * 128], identb[:], [src_bf, identb], [pT])
            CP("act", dstT[:].rearrange("p k t -> p (k t)"), pT[:], [pT], [dstT])

        try:
          with ExitStack() as es1:
              def sb(name, shape, dt=F32):
                  return Tile(name, es1.enter_context(nc.sbuf_tensor(name, shape, dt)))

              Win = sb("Win", [128, 8, 2568], BF16)
              Wout = sb("Wout", [128, 8, 1024], BF16)
              nmix = sb("nmix", [128, 8])
              cwT = sb("cwT", [128, 12, 4])
              wpool = sb("wpool", [128, 4, 128])
              dnw_b = sb("dnw_b", [128, 128])
              pscale_b = sb("pscale_b", [128, 512])
              dtb_b = sb("dtb_b", [128, 4])
              negA = sb("negA", [128, 4])
              Um = sb("Um", [128, 128])
              M1 = sb("M1", [128, 512])
              M2 = sb("M2", [128, 512])
              MC = sb("MC", [128, 512])
              MPm = sb("MPm", [128, 512])
              MF = sb("MF", [128, 512])

              DMA(nmix[:], nmix_d, [], [nmix])
              load_weight_bf(Win, win_d, 2568, nmix)
              load_weight_bf(Wout, wout_d, 1024)
              DMA(stage[0:4, 0:1536], cw_d, [], [stage])
              bcw = nb()
              for c in range(12):
                  TR(bcw[:, c * 4:(c + 1) * 4], stage[0:4, c * 128:(c + 1) * 128], ident[0:4, 0:4], [stage, ident], [bcw])
              CP("act", cwT[:].rearrange("p c j -> p (c j)"), bcw[:, 0:48], [bcw], [cwT])
              DMA(wpool[:], wpool_d.rearrange("g c d -> c g d"), [], [wpool])
              DMA(dnw_b[:], dnw_d.partition_broadcast(128), [], [dnw_b])
              DMA(pscale_b[:], pscale_d.partition_broadcast(128), [], [pscale_b])
              DMA(dtb_b[:], dtb_d.partition_broadcast(128), [], [dtb_b])
              DMA(negA[:], alog_d.partition_broadcast(128), [], [negA])
              ACT(negA[:], negA[:], AF.Exp, [negA], [negA])
              TS("dve", negA[:], negA[:], -1.0, None, ALU.mult, None, [negA], [negA])
              for tl, nm in ((Um, "c_U"), (M1, "c_M1"), (M2, "c_M2"), (MC, "c_MC"), (MPm, "c_MP"), (MF, "c_MF")):
                  DMA(tl[:], cd[nm], [], [tl])

              qkvT = sb("qkvT", [128, 12, 131])
              cacc = sb("cacc", [128, 12, 128])
              qkv = sb("qkv", [128, 1536])
              proj2 = sb("proj2", [128, 1032])
              pprev = sb("pprev", [128, 512])
              S = sb("S", [128, 4, 128])
              qn = sb("qn", [128, 512])
              qg = sb("qg", [128, 512])
              kn = sb("kn", [128, 512])
              ss8 = sb("ss8", [128, 8])
              rn8 = sb("rn8", [128, 8])
              beta = sb("beta", [128, 4])
              nbeta = sb("nbeta", [128, 4])
              gt = sb("gt", [128, 4])
              gc = sb("gc", [128, 4])
              ngc = sb("ngc", [128, 4])
              glb = sb("glb", [128, 4])
              eg = sb("eg", [128, 4])
              egl = sb("egl", [128, 4])
              ekd = sb("ekd", [128, 4])
              bk = sb("bk", [128, 4])
              gU4 = sb("gU4", [128, 4, 128])
              decS = sb("decS", [128, 4, 128])
              decT = sb("decT", [128, 4, 128])
              vb = sb("vb", [128, 4, 128])
              kbg = sb("kbg", [128, 4, 128])
              kd = sb("kd", [128, 4, 128])
              kT4 = sb("kT4", [128, 4, 128])
              qT4 = sb("qT4", [128, 4, 128])
              qgT4 = sb("qgT4", [128, 4, 128])
              Qb = [sb("Qb%d" % i, [128, 128]) for i in range(2)]
              Pb = [sb("Pb%d" % i, [128, 128]) for i in range(2)]
              Xb = [sb("Xb%d" % i, [128, 128]) for i in range(2)]
              AT = sb("AT", [128, 128])
              nkcdT = sb("nkcdT", [128, 128])
              vnew = sb("vnew", [128, 128])
              osb = sb("osb", [128, 512])
              ss4 = sb("ss4", [128, 4])
              rn4 = sb("rn4", [128, 4])
              gz = sb("gz", [128, 512])
              mix = sb("mix", [128, 1024])
              plT = sb("plT", [128, 4, 128])

              P.op("dve", lambda E: E.memset(qkvT[:], 0.0), writes=[qkvT])
              P.op("dve", lambda E: E.memset(pprev[:], 0.0), writes=[pprev])
              P.op("dve", lambda E: E.memset(S[:], 0.0), writes=[S])

              for t in range(NT):
                  DMA(xt[:], x_d[t * 128:(t + 1) * 128, :], [], [xt])
                  ACT(junk[:, 0:1024], xt[:], AF.Square, [xt], [junk, ss], accum_out=ss[:])
                  RSTD(rstd, ss, 1.0 / 1024)
                  TS("dve", hbf[:], xt[:], rstd[:, 0:1], None, ALU.mult, None, [xt, rstd], [hbf])
                  transpose8(hbf, hT)
                  for b3 in range(3):
                      bk_ = nb()
                      for cc in range(4):
                          c = b3 * 4 + cc
                          for k in range(8):
                              MM(bk_[:, cc * 128:(cc + 1) * 128], Win[:, k, c * 128:(c + 1) * 128], hT[:, k, :],
                                 [Win, hT], [bk_], start=(k == 0), stop=(k == 7))
                      CP("act", qkvT[:, b3 * 4:(b3 + 1) * 4, 3:131],
                         bk_[:].rearrange("p (c t) -> p c t", c=4), [bk_], [qkvT])
                  pz = nb()
                  pg = nb()
                  pp = nb()
                  for k in range(8):
                      MM(pz[:, :], hT[:, k, :], Win[:, k, 1536:2048], [Win, hT], [pz], start=(k == 0), stop=(k == 7))
                  for k in range(8):
                      MM(pg[:, 0:8], hT[:, k, :], Win[:, k, 2048:2056], [Win, hT], [pg], start=(k == 0), stop=(k == 7))
                  for k in range(8):
                      MM(pp[:, :], hT[:, k, :], Win[:, k, 2056:2568], [Win, hT], [pp], start=(k == 0), stop=(k == 7))
                  CP("act", proj2[:, 0:512], pz[:], [pz], [proj2])
                  CP("act", proj2[:, 512:520], pg[:, 0:8], [pg], [proj2])
                  CP("act", proj2[:, 520:1032], pp[:], [pp], [proj2])
                  STOP("a", [(proj2[:, 0:1024], proj2, 1024)])
                  for c in range(12):
                      TS("dve", cacc[:, c, :], qkvT[:, c, 3:131], cwT[:, c, 3:4], None, ALU.mult, None, [qkvT, cwT], [cacc])
                      for j in range(3):
                          STT(cacc[:, c, :], qkvT[:, c, j:j + 128], cwT[:, c, j:j + 1], cacc[:, c, :], ALU.mult, ALU.add,
                              [qkvT, cwT, cacc], [cacc])
                  CP("dve", qkvT[:, :, 0:3], qkvT[:, :, 128:131], [qkvT], [qkvT])
                  ACT(cacc[:], cacc[:], AF.Silu, [cacc], [cacc])
                  for b3 in range(3):
                      bk_ = nb()
                      for cc in range(4):
                          c = b3 * 4 + cc
                          TR(bk_[:, cc * 128:(cc + 1) * 128], cacc[:, c, :], ident[:], [cacc, ident], [bk_])
                      CP("act", qkv[:, b3 * 512:(b3 + 1) * 512], bk_[:], [bk_], [qkv])
                  TT("dve", junk[:, 0:1024], qkv[:, 0:1024], qkv[:, 0:1024], ALU.mult, [qkv], [junk])
                  RED(ss8[:], junk[:, 0:1024].rearrange("p (h c) -> p h c", h=8), [junk], [ss8])
                  RSTD(rn8, ss8, 1.0)
                  TT("dve", kn[:].rearrange("p (h c) -> p h c", h=4), qkv[:, 512:1024].rearrange("p (h c) -> p h c", h=4),
                     rn8[:, 4:8].unsqueeze(2).to_broadcast([128, 4, 128]), ALU.mult, [qkv, rn8], [kn])
                  TS("dve", rn8[:, 0:4], rn8[:, 0:4], 128.0 ** -0.5, None, ALU.mult, None, [rn8], [rn8])
                  TT("dve", qn[:].rearrange("p (h c) -> p h c", h=4), qkv[:, 0:512].rearrange("p (h c) -> p h c", h=4),
                     rn8[:, 0:4].unsqueeze(2).to_broadcast([128, 4, 128]), ALU.mult, [qkv, rn8], [qn])
                  STOP("b", [(qn[:], qn, 512), (kn[:], kn, 512)])
                  ACT(beta[:], proj2[:, 512:516], AF.Sigmoid, [proj2], [beta])
                  TS("dve", nbeta[:], beta[:], -1.0, None, ALU.mult, None, [beta], [nbeta])
                  TT("dve", gt[:], proj2[:, 516:520], dtb_b[:], ALU.add, [proj2, dtb_b], [gt])
                  ACT(gt[:], gt[:], AF.Exp, [gt], [gt])
                  ACT(gt[:], gt[:], AF.Ln, [gt], [gt], bias=1.0)
                  TT("dve", gt[:], gt[:], negA[:], ALU.mult, [gt, negA], [gt])
                  pgc = nb()
                  MM(pgc[:, 0:4], Um[:], gt[:], [Um, gt], [pgc])
                  MM(pgc[:, 8:12], ones[:], gt[:], [ones, gt], [pgc])
                  CP("dve", gc[:], pgc[:, 0:4], [pgc], [gc])
                  CP("dve", glb[:], pgc[:, 8:12], [pgc], [glb])
                  TS("dve", ngc[:], gc[:], -1.0, None, ALU.mult, None, [gc], [ngc])
                  ACT(eg[:], gc[:], AF.Exp, [gc], [eg])
                  ACT(egl[:], glb[:], AF.Exp, [glb], [egl])
                  TT("dve", ekd[:], glb[:], gc[:], ALU.subtract, [glb, gc], [ekd])
                  ACT(ekd[:], ekd[:], AF.Exp, [ekd], [ekd])
                  TT("dve", bk[:], beta[:], eg[:], ALU.mult, [beta, eg], [bk])
                  TT("dve", gU4[:], Um[:].unsqueeze(1).to_broadcast([128, 4, 128]),
                     gt[:].unsqueeze(2).to_broadcast([128, 4, 128]), ALU.mult, [Um, gt], [gU4])
                  R1 = nb()
                  R2 = nb()
                  MM(R1[:], ones[:], gU4[:].rearrange("p h f -> p (h f)"), [ones, gU4], [R1], start=True, stop=False)
                  MM(R1[:], ident[:], M1[:], [ident, M1], [R1], start=False, stop=True)
                  MM(R2[:], ones[:], gU4[:].rearrange("p h f -> p (h f)"), [ones, gU4], [R2], start=True, stop=False)
                  MM(R2[:], ident[:], M2[:], [ident, M2], [R2], start=False, stop=True)
                  for h in range(4):
                      ACT(decS[:, h, :], R1[:, h * 128:(h + 1) * 128], AF.Exp, [R1, gc], [decS], scale=-1.0, bias=gc[:, h:h + 1])
                      ACT(decT[:, h, :], R2[:, h * 128:(h + 1) * 128], AF.Exp, [R2, ngc], [decT], scale=1.0, bias=ngc[:, h:h + 1])
                  v3 = qkv[:, 1024:1536].rearrange("p (h c) -> p h c", h=4)
                  kn3 = kn[:].rearrange("p (h c) -> p h c", h=4)
                  TT("dve", vb[:], v3, beta[:].unsqueeze(2).to_broadcast([128, 4, 128]), ALU.mult, [qkv, beta], [vb])
                  TT("dve", kbg[:], kn3, bk[:].unsqueeze(2).to_broadcast([128, 4, 128]), ALU.mult, [kn, bk], [kbg])
                  TT("dve", kd[:], kn3, ekd[:].unsqueeze(2).to_broadcast([128, 4, 128]), ALU.mult, [kn, ekd], [kd])
                  TT("dve", qg[:].rearrange("p (h c) -> p h c", h=4), qn[:].rearrange("p (h c) -> p h c", h=4),
                     eg[:].unsqueeze(2).to_broadcast([128, 4, 128]), ALU.mult, [qn, eg], [qg])
                  for (src, dst) in ((kn, kT4), (qn, qT4), (qg, qgT4)):
                      bk_ = nb()
                      for h in range(4):
                          TR(bk_[:, h * 128:(h + 1) * 128], src[:, h * 128:(h + 1) * 128], ident[:], [src, ident], [bk_])
                      CP("act", dst[:].rearrange("p h t -> p (h t)"), bk_[:], [bk_], [dst])
                      STOP("c", [(decS[:].rearrange("p h t -> p (h t)"), decS, 512), (kT4[:].rearrange("p h t -> p (h t)"), kT4, 512)])
                  for h in range(4):
                      pG = nb()
                      MM(pG[:, 0:128], kT4[:, h, :], kT4[:, h, :], [kT4], [pG])
                      MM(pG[:, 128:256], kT4[:, h, :], qT4[:, h, :], [kT4, qT4], [pG])
                      if mode == "d0a":
                          CP("act", AT[:], pG[:, 0:128], [pG], [AT])
                      STOP("d0a", [(AT[:], AT, 128)])
                      Q, Pm, X = Qb[0], Pb[0], Xb[0]
                      STT(Q[:], pG[:, 0:128], nbeta[:, h:h + 1], decS[:, h, :], ALU.mult, ALU.mult, [pG, nbeta, decS], [Q])
                      STOP("d0b", [(Q[:], Q, 128)])
                      TT("dve", AT[:], pG[:, 128:256], decT[:, h, :], ALU.mult, [pG, decT], [AT])
                      STOP("d0d", [(AT[:], AT, 128)])
                      pP = nb()
                      TR(pP[:, 0:128], Q[:], ident[:], [Q, ident], [pP])
                      CP("dve", Pm[:], pP[:, 0:128], [pP], [Pm])
                      STOP("d0c", [(Pm[:], Pm, 128)])
                      TT("dve", X[:], pP[:, 0:128], ident[:], ALU.add, [pP, ident], [X])
                      STOP("d1", [(Q[:], Q, 128), (AT[:], AT, 128), (Pm[:], Pm, 128), (X[:], X, 128)])
                      qi = pi = xi = 0
                      for m in range(6):
                          Qn_, Pn_, Xn_ = Qb[1 - qi], Pb[1 - pi], Xb[1 - xi]
                          pq = nb()
                          MM(pq[:, 0:128], Pm[:], Q[:], [Pm, Q], [pq])
                          if m < 5:
                              MM(pq[:, 128:256], Q[:], Pm[:], [Pm, Q], [pq])
                          CP("act", Qn_[:], pq[:, 0:128], [pq], [Qn_])
                          if m < 5:
                              CP("act", Pn_[:], pq[:, 128:256], [pq], [Pn_])
                          px = nb()
                          MM(px[:, 0:128], Qn_[:], X[:], [Qn_, X], [px])
                          TT("dve", Xn_[:], px[:, 0:128], X[:], ALU.add, [px, X], [Xn_])
                          Q, Pm, X = Qn_, Pn_, Xn_
                          qi, pi, xi = 1 - qi, 1 - pi, 1 - xi
                          STOP("d2%d" % m, [(Q[:], Q, 128), (X[:], X, 128)])
                      pk = nb()
                      MM(pk[:, 0:128], kbg[:, h, :], X[:], [kbg, X], [pk])
                      TS("dve", nkcdT[:], pk[:, 0:128], -1.0, None, ALU.mult, None, [pk], [nkcdT])
                      pv = nb()
                      MM(pv[:, 0:128], X[:], vb[:, h, :], [X, vb], [pv], start=True, stop=False)
                      MM(pv[:, 0:128], nkcdT[:], S[:, h, :], [nkcdT, S], [pv], start=False, stop=True)
                      CP("act", vnew[:], pv[:, 0:128], [pv], [vnew])
                      STOP("d3", [(vnew[:], vnew, 128), (nkcdT[:], nkcdT, 128)])
                      po = nb()
                      MM(po[:, 0:128], qgT4[:, h, :], S[:, h, :], [qgT4, S], [po], start=True, stop=False)
                      MM(po[:, 0:128], AT[:], vnew[:], [AT, vnew], [po], start=False, stop=True)
                      MM(po[:, 128:256], kd[:, h, :], vnew[:], [kd, vnew], [po])
                      CP("dve", osb[:, h * 128:(h + 1) * 128], po[:, 0:128], [po], [osb])
                      STT(S[:, h, :], S[:, h, :], egl[:, h:h + 1], po[:, 128:256], ALU.mult, ALU.add, [S, egl, po], [S])
                      STOP("d", [(osb[:], osb, 512), (S[:].rearrange("p h t -> p (h t)"), S, 512)])
                  TT("dve", junk[:, 0:512], osb[:], osb[:], ALU.mult, [osb], [junk])
                  RED(ss4[:], junk[:, 0:512].rearrange("p (h c) -> p h c", h=4), [junk], [ss4])
                  RSTD(rn4, ss4, 1.0 / 128)
                  ACT(gz[:], proj2[:, 0:512], AF.Silu, [proj2], [gz])
                  TT("dve", gz[:].rearrange("p (h c) -> p h c", h=4), gz[:].rearrange("p (h c) -> p h c", h=4),
                     dnw_b[:].unsqueeze(1).to_broadcast([128, 4, 128]), ALU.mult, [gz, dnw_b], [gz])
                  TT("dve", osb[:].rearrange("p (h c) -> p h c", h=4), osb[:].rearrange("p (h c) -> p h c", h=4),
                     rn4[:].unsqueeze(2).to_broadcast([128, 4, 128]), ALU.mult, [osb, rn4], [osb])
                  TT("dve", mix[:, 0:512], osb[:], gz[:], ALU.mult, [osb, gz], [mix])
                  ppl = nb()
                  Mcur = MF if t == 0 else MC
                  for g in range(4):
                      MM(ppl[:, g * 128:(g + 1) * 128], proj2[:, 520 + g * 128:520 + (g + 1) * 128], Mcur[:, g * 128:(g + 1) * 128],
                         [proj2, Mcur], [ppl], start=True, stop=False)
                      MM(ppl[:, g * 128:(g + 1) * 128], pprev[:, g * 128:(g + 1) * 128], MPm[:, g * 128:(g + 1) * 128],
                         [pprev, MPm], [ppl], start=False, stop=True)
                  CP("act", plT[:].rearrange("p g t -> p (g t)"), ppl[:], [ppl], [plT])
                  CP("dve", pprev[:], proj2[:, 520:1032], [proj2], [pprev])
                  pmx = nb()
                  for g in range(4):
                      MM(pmx[:, g * 128:(g + 1) * 128], plT[:, g, :], wpool[:, g, :], [plT, wpool], [pmx])
                  TT("dve", mix[:, 512:1024], pmx[:], pscale_b[:], ALU.mult, [pmx, pscale_b], [mix])
                  CP("dve", hbf[:], mix[:], [mix], [hbf])
                  transpose8(hbf, hT)
                  for n in range(2):
                      py = nb()
                      for k in range(8):
                          MM(py[:], hT[:, k, :], Wout[:, k, n * 512:(n + 1) * 512], [hT, Wout], [py], start=(k == 0), stop=(k == 7))
                      TT("dve", xt[:, n * 512:(n + 1) * 512], xt[:, n * 512:(n + 1) * 512], py[:], ALU.add, [xt, py], [xt])
                  DMA(x1_d[t * 128:(t + 1) * 128, :], xt[:], [xt], ["x1d%d" % t])
                  if mode == "p1":
                      DMA(out_d[t * 128:(t + 1) * 128, :], xt[:], [xt], [])

        except StopBuild:
            pass
        with ExitStack() as es2:
          if mode != "p1":
              def sb(name, shape, dt=F32):
                  return Tile(name, es2.enter_context(nc.sbuf_tensor(name, shape, dt)))

              Wq = sb("Wq", [128, 8, 2048], BF16)
              skT = sb("skT", [128, 16, 128])
              nffn_b = sb("nffn_b", [128, 1024])
              nfin_b = sb("nfin_b", [128, 1024])
              iota16 = sb("iota16", [128, 16])
              xn = sb("xn", [128, 1024])
              qTs = sb("qTs", [128, 16, 128])
              s16 = sb("s16", [128, 16, 16])
              i16 = sb("i16", [128, 16, 16], U32)
              if16 = sb("if16", [128, 16, 16])
              work = sb("work", [128, 256])
              cand = sb("cand", [128, 8, 256])
              ts = sb("ts", [128, 8, 16])
              pos = sb("pos", [128, 8, 16], U32)
              posi = sb("posi", [128, 8, 16], U32)
              pa = sb("pa", [128, 8, 16])
              pbm = sb("pbm", [128, 8, 16])
              eq = sb("eq", [128, 8, 256])
              eidx = sb("eidx", [128, 8, 16])
              eidx2 = sb("eidx2", [128, 8, 16])
              eidi = sb("eidi", [128, 128], I32)
              gate = sb("gate", [128, 8, 16])
              gsum = sb("gsum", [128, 8])
              araw = sb("araw", [128, 128])
              coef = sb("coef", [128, 128])
              NB = 2 * GRP
              UVb = [sb("UVb%d" % i, [128, 2048]) for i in range(NB)]
              dgb = [sb("dgb%d" % i, [128, 128], F32R) for i in range(NB)]
              NVR = 6
              Vrb = [sb("Vrb%d" % i, [128, 1024], F32R) for i in range(NVR)]
              gl = sb("gl", [128, 128])
              djunk = sb("djunk", [128, 2048], BF16)
              bstate["n"] = 5
              accA, accB = banks[5], banks[6]

              load_weight_bf(Wq, wq_d, 2048)
              DMA(nffn_b[:], nffn_d.partition_broadcast(128), [], [nffn_b])
              DMA(nfin_b[:], nfin_d.partition_broadcast(128), [], [nfin_b])
              DMA(iota16[:], cd["c_iota"], [], [iota16])
              for b4 in range(4):
                  DMA(stage[:, 0:512].rearrange("p (b c) -> p b c", b=4), sk_d[b4 * 4:(b4 + 1) * 4].rearrange("b k c -> k b c"),
                      [], [stage])
                  bk_ = nb()
                  for bb in range(4):
                      TR(bk_[:, bb * 128:(bb + 1) * 128], stage[:, bb * 128:(bb + 1) * 128], ident[:], [stage, ident], [bk_])
                  CP("act", skT[:, b4 * 4:(b4 + 1) * 4, :].rearrange("p b k -> p (b k)"), bk_[:], [bk_], [skT])

              gidx = 0
              for t in range(NT):
                  DMA(xt[:], x1_d[t * 128:(t + 1) * 128, :], ["x1d%d" % t], [xt])
                  ACT(junk[:, 0:1024], xt[:], AF.Square, [xt], [junk, ss], accum_out=ss[:])
                  RSTD(rstd, ss, 1.0 / 1024)
                  STT(xn[:], xt[:], rstd[:, 0:1], nffn_b[:], ALU.mult, ALU.mult, [xt, rstd, nffn_b], [xn])
                  CP("act", hbf[:], xn[:], [xn], [hbf])
                  transpose8(hbf, hT)
                  for b4 in range(4):
                      bk_ = nb()
                      for bb in range(4):
                          blk = b4 * 4 + bb
                          for k in range(8):
                              MM(bk_[:, bb * 128:(bb + 1) * 128], Wq[:, k, blk * 128:(blk + 1) * 128], hT[:, k, :],
                                 [Wq, hT], [bk_], start=(k == 0), stop=(k == 7))
                      CP("act", qTs[:, b4 * 4:(b4 + 1) * 4, :].rearrange("p b t -> p (b t)"), bk_[:], [bk_], [qTs])
                  for b4 in range(4):
                      bk_ = nb()
                      for bb in range(4):
                          blk = b4 * 4 + bb
                          MM(bk_[:, bb * 128:(bb + 1) * 128], qTs[:, blk, :], skT[:, blk, :], [qTs, skT], [bk_])
                      for bb in range(4):
                          blk = b4 * 4 + bb
                          sc = bk_[:, bb * 128:(bb + 1) * 128]
                          P.op("dve", lambda E, sc=sc, blk=blk: E.max(out=s16[:, blk, 0:8], in_=sc), reads=[bk_], writes=[s16])
                          P.op("dve", lambda E, sc=sc, blk=blk: E.max_index(out=i16[:, blk, 0:8], in_max=s16[:, blk, 0:8], in_values=sc),
                               reads=[bk_, s16], writes=[i16])
                          P.op("dve", lambda E, sc=sc, blk=blk: E.match_replace(out=work[:, 0:128], in_to_replace=s16[:, blk, 0:8],
                                                                               in_values=sc, imm_value=-1e30),
                               reads=[bk_, s16], writes=[work])
                          P.op("dve", lambda E, blk=blk: E.max(out=s16[:, blk, 8:16], in_=work[:, 0:128]), reads=[work], writes=[s16])
                          P.op("dve", lambda E, blk=blk: E.max_index(out=i16[:, blk, 8:16], in_max=s16[:, blk, 8:16],
                                                                    in_values=work[:, 0:128]), reads=[work, s16], writes=[i16])
                  CP("dve", if16[:], i16[:], [i16], [if16])
                  s4 = s16[:].rearrange("p (h two) k -> p h two k", two=2)
                  i4 = if16[:].rearrange("p (h two) k -> p h two k", two=2)
                  TT("dve", cand[:].rearrange("p h (a b) -> p h a b", a=16),
                     s4[:, :, 0, :].unsqueeze(3).to_broadcast([128, 8, 16, 16]),
                     s4[:, :, 1, :].unsqueeze(2).to_broadcast([128, 8, 16, 16]), ALU.add, [s16], [cand])
                  for h in range(8):
                      P.op("dve", lambda E, h=h: E.max(out=ts[:, h, 0:8], in_=cand[:, h, :]), reads=[cand], writes=[ts])
                      P.op("dve", lambda E, h=h: E.max_index(out=pos[:, h, 0:8], in_max=ts[:, h, 0:8], in_values=cand[:, h, :]),
                           reads=[cand, ts], writes=[pos])
                      P.op("dve", lambda E, h=h: E.match_replace(out=work[:], in_to_replace=ts[:, h, 0:8], in_values=cand[:, h, :],
                                                                 imm_value=-1e30), reads=[cand, ts], writes=[work])
                      P.op("dve", lambda E, h=h: E.max(out=ts[:, h, 8:16], in_=work[:]), reads=[work], writes=[ts])
                      P.op("dve", lambda E, h=h: E.max_index(out=pos[:, h, 8:16], in_max=ts[:, h, 8:16], in_values=work[:]),
                           reads=[work, ts], writes=[pos])
                  TS("dve", posi[:], pos[:], 15, None, ALU.bitwise_and, None, [pos], [posi])
                  CP("dve", pbm[:], posi[:], [posi], [pbm])
                  TS("dve", posi[:], pos[:], 4, None, ALU.logical_shift_right, None, [pos], [posi])
                  CP("dve", pa[:], posi[:], [posi], [pa])
                  eq4 = eq[:].rearrange("p h (k a) -> p h k a", k=16)
                  io4 = iota16[:].unsqueeze(1).unsqueeze(1).to_broadcast([128, 8, 16, 16])
                  TT("dve", eq4, pa[:].unsqueeze(3).to_broadcast([128, 8, 16, 16]), io4, ALU.is_equal, [pa, iota16], [eq])
                  TT("dve", eq4, eq4, i4[:, :, 0, :].unsqueeze(2).to_broadcast([128, 8, 16, 16]), ALU.mult, [eq, if16], [eq])
                  RED(eidx[:], eq4, [eq], [eidx])
                  TT("dve", eq4, pbm[:].unsqueeze(3).to_broadcast([128, 8, 16, 16]), io4, ALU.is_equal, [pbm, iota16], [eq])
                  TT("dve", eq4, eq4, i4[:, :, 1, :].unsqueeze(2).to_broadcast([128, 8, 16, 16]), ALU.mult, [eq, if16], [eq])
                  RED(eidx2[:], eq4, [eq], [eidx2])
                  STT(eidx[:].rearrange("p h k -> p (h k)"), eidx[:].rearrange("p h k -> p (h k)"), 128.0,
                      eidx2[:].rearrange("p h k -> p (h k)"), ALU.mult, ALU.add, [eidx, eidx2], [eidx])
                  CP("dve", eidi[:], eidx[:].rearrange("p h k -> p (h k)"), [eidx], [eidi])
                  TT("dve", gate[:], ts[:], ts[:, :, 0:1].to_broadcast([128, 8, 16]), ALU.subtract, [ts], [gate])
                  ACT(gate[:], gate[:], AF.Exp, [gate], [gate])
                  RED(gsum[:], gate[:], [gate], [gsum])
                  P.op("dve", lambda E: E.reciprocal(out=gsum[:], in_=gsum[:]), reads=[gsum], writes=[gsum])
                  TT("dve", gate[:], gate[:], gsum[:].unsqueeze(2).to_broadcast([128, 8, 16]), ALU.mult, [gate, gsum], [gate])
                  gflat = gate[:].rearrange("p h k -> p (h k)")
                  P.op("dve", lambda E: E.memset(araw[:], 0.0), writes=[("araw", q) for q in range(0, 128, GRP)])
                  for g0 in range(0, 0 if mode == "ng" else 128, GRP):
                      bufs = []
                      for s in range(g0, g0 + GRP):
                          bi = gidx % NB
                          gidx += 1
                          UV, dg = UVb[bi], dgb[bi]
                          bufs.append((s, UV, dg))
                          P.op("pool", lambda E, UV=UV, s=s: E.indirect_dma_start(
                              out=UV[:], out_offset=None, in_=ec_d,
                              in_offset=bass.IndirectOffsetOnAxis(ap=eidi[:, s:s + 1], axis=0)),
                              reads=[eidi], writes=[UV], dma=True)
                      for (s, UV, dg) in bufs:
                          jk = s % 2
                          STT(djunk[:, jk * 1024:(jk + 1) * 1024], UV[:, 0:1024], 1.0, xn[:], ALU.mult, ALU.mult,
                              [UV, xn], [("dj", jk), ("araw", g0)], accum=araw[:, s:s + 1])
                      ACT(gl[:, g0:g0 + GRP], araw[:, g0:g0 + GRP], AF.Gelu, [("araw", g0)], [("gl", g0)])
                      for (s, UV, dg) in bufs:
                          TS("dve", dg[:], ident[:], gl[:, s:s + 1], gflat[:, s:s + 1], ALU.mult, ALU.mult,
                             [ident, ("gl", g0), gate], [dg])
                          Vr = Vrb[s % NVR]
                          CP("act", Vr[:], UV[:, 1024:2048], [UV], [Vr])
                          MM(accA[:], dg[:], Vr[:, 0:512], [dg, Vr], [accA], start=(s == 0), stop=(s == 127))
                          MM(accB[:], dg[:], Vr[:, 512:1024], [dg, Vr], [accB], start=(s == 0), stop=(s == 127))
                  if mode != "ng":
                      TT("dve", xt[:, 0:512], xt[:, 0:512], accA[:], ALU.add, [xt, accA], [xt])
                      TT("dve", xt[:, 512:1024], xt[:, 512:1024], accB[:], ALU.add, [xt, accB], [xt])
                  ACT(junk[:, 0:1024], xt[:], AF.Square, [xt], [junk, ss], accum_out=ss[:])
                  RSTD(rstd, ss, 1.0 / 1024)
                  STT(xn[:], xt[:], rstd[:, 0:1], nfin_b[:], ALU.mult, ALU.mult, [xt, rstd, nfin_b], [xn])
                  DMA(out_d[t * 128:(t + 1) * 128, :], xn[:], [xn], [])

        P.finish()
        P.emit(nc)
    return nc


_CACHE = {}


def make_in_maps(inputs, ncores, NT):
    consts = host_consts()
    f = lambda a: np.ascontiguousarray(np.asarray(a, dtype=np.float32))
    shared = {
        "norm_mix_w": np.ascontiguousarray(f(inputs["norm_mix_w"]).reshape(8, 128).T),
        "w_in": f(inputs["w_in"]).reshape(1024, 2568),
        "conv_w": f(inputs["conv_w"]).reshape(4, 1536),
        "a_log": f(inputs["a_log"]).reshape(1, 4),
        "dt_bias": f(inputs["dt_bias"]).reshape(1, 4),
        "dn_norm_w": f(inputs["dn_norm_w"]).reshape(1, 128),
        "w_pool": f(inputs["w_pool"]).reshape(4, 128, 128),
        "pool_scale": f(inputs["pool_scale"]).reshape(1, 512),
        "w_out": f(inputs["w_out"]).reshape(1024, 1024),
        "norm_ffn_w": f(inputs["norm_ffn_w"]).reshape(1, 1024),
        "w_query": f(inputs["w_query"]).reshape(1024, 2048),
        "sub_keys": f(inputs["sub_keys"]).reshape(16, 128, 128),
        "expert_cat": np.concatenate([f(inputs["expert_down"]).reshape(16384, 1024),
                                      f(inputs["expert_up"]).reshape(16384, 1024)], axis=1),
        "norm_final_w": f(inputs["norm_final_w"]).reshape(1, 1024),
    }
    shared.update(consts)
    x = f(inputs["x"])
    maps = []
    for c in range(ncores):
        m = dict(shared)
        m["x"] = np.ascontiguousarray(x[c, :NT * 128, :])
        maps.append(m)
    return maps


def kernel(**inputs):
    NT = 64
    ncores = 8
    if "nc" not in _CACHE:
        _CACHE["nc"] = build(NT)
    nc = _CACHE["nc"]
    maps = make_in_maps(inputs, ncores, NT)
    res = run_bass_kernel_spmd(nc, maps, core_ids=list(range(ncores)))
    out = np.stack([np.asarray(r["out"]) for r in res.results], axis=0)
    return out.astype(np.float32)
```

```python
from contextlib import ExitStack
import numpy as np
import ml_dtypes
import concourse.bass as bass
import concourse.mybir as mybir
from concourse.bass_utils import run_bass_kernel_spmd

F32 = mybir.dt.float32
F32R = mybir.dt.float32r
BF16 = mybir.dt.bfloat16
I32 = mybir.dt.int32
U32 = mybir.dt.uint32
AF = mybir.ActivationFunctionType
ALU = mybir.AluOpType
AX = mybir.AxisListType

ENG = ("pe", "act", "dve", "pool", "sp")
EPOCH = 20000
DEPOCH = 1200
NDMA = 8
EPS = 1e-6
BIG = 1.0e5
GRP = 4


class StopBuild(Exception):
    pass


class Tile:
    __slots__ = ("name", "t")

    def __init__(self, name, t):
        self.name = name
        self.t = t

    def __getitem__(self, k):
        return self.t[k]


class Plan:
    def __init__(self):
        self.streams = {e: [] for e in ENG}
        self.cnt = {e: 0 for e in ENG}
        self.lastw = {}
        self.readers = {}
        self.seen = {e: {} for e in ENG}
        self.dma_rr = {e: 0 for e in ENG}
        self.dma_n = {}
        self.dma_last = {}
        self.semkeys = []
        self.semset = set()
        self.stopped = False

    def _key(self, k):
        if k not in self.semset:
            self.semset.add(k)
            self.semkeys.append(k)
        return k

    def _need(self, e, ev, waits):
        grp, ep, val, key = ev
        cur = self.seen[e].get(grp)
        if cur is not None and cur >= (ep, val):
            return
        self.seen[e][grp] = (ep, val)
        waits.append((key, val))

    def op(self, e, fn, reads=(), writes=(), dma=False):
        if self.stopped:
            return None
        raw = []
        oth = []
        for b in reads:
            w = self.lastw.get(b)
            if w is not None:
                raw.append(w)
        for b in writes:
            w = self.lastw.get(b)
            if w is not None:
                oth.append(w)
            oth.extend(self.readers.get(b, ()))
        waits = []
        for ev in raw:
            self._need(e, ev, waits)
        for ev in oth:
            self._need(e, ev, waits)
        if dma:
            j = self.dma_rr[e] % NDMA
            self.dma_rr[e] += 1
            prev = self.dma_last.get((e, j))
            if prev is not None:
                self._need(e, prev, waits)
            n = self.dma_n.get((e, j), 0)
            self.dma_n[(e, j)] = n + 1
            ep = n // DEPOCH
            val = (n % DEPOCH + 1) * 16
            key = self._key(("d", e, j, ep))
            ev = (("d", e, j), ep, val, key)
            self.dma_last[(e, j)] = ev
            inc = 16
        else:
            n = self.cnt[e]
            self.cnt[e] = n + 1
            ep = n // EPOCH
            val = n % EPOCH + 1
            key = self._key(("e", e, ep))
            ev = (("e", e), ep, val, key)
            inc = 1
        self.streams[e].append((waits, fn, key, inc))
        for b in reads:
            self.readers.setdefault(b, []).append(ev)
        for b in writes:
            self.lastw[b] = ev
            self.readers[b] = []
        return ev

    def finish(self):
        waits = []
        for (e, j), ev in self.dma_last.items():
            self._need("sp", ev, waits)
        for e in ENG:
            if e == "sp" or self.cnt[e] == 0:
                continue
            n = self.cnt[e] - 1
            ep = n // EPOCH
            val = n % EPOCH + 1
            self._need("sp", (("e", e), ep, val, ("e", e, ep)), waits)
        self.streams["sp"].append((waits, None, None, 0))

    def emit(self, nc):
        with ExitStack() as es:
            sems = {}
            for i, k in enumerate(self.semkeys):
                sems[k] = es.enter_context(nc.semaphore("s%d" % i))
            with nc.Block() as block:
                def run(name):
                    def body(E):
                        for waits, fn, key, inc in self.streams[name]:
                            for (wk, wv) in waits:
                                E.wait_ge(sems[wk], wv)
                            if fn is not None:
                                fn(E).then_inc(sems[key], inc)
                    return body
                block.tensor(run("pe"))
                block.scalar(run("act"))
                block.vector(run("dve"))
                block.gpsimd(run("pool"))
                block.sync(run("sp"))


def host_consts():
    c = {}
    c["c_ident"] = np.eye(128, dtype=np.float32)
    c["c_ones"] = np.ones((128, 128), dtype=np.float32)
    k = np.arange(128)[:, None]
    f = np.arange(128)[None, :]
    c["c_U"] = (k <= f).astype(np.float32)
    m1 = np.where(k <= f, BIG, 0.0).astype(np.float32)
    m2 = np.where(f < k, -BIG, 0.0).astype(np.float32)
    c["c_M1"] = np.tile(m1, (1, 4))
    c["c_M2"] = np.tile(m2, (1, 4))
    wins = (2, 4, 8, 16)
    mc = np.zeros((4, 128, 128), np.float32)
    mp = np.zeros((4, 128, 128), np.float32)
    mf = np.zeros((4, 128, 128), np.float32)
    for gi, w in enumerate(wins):
        for t in range(128):
            for j in range(w):
                tp = t - j
                if tp >= 0:
                    mc[gi, tp, t] += 1.0 / w
                    mf[gi, tp, t] += 1.0 / min(t + 1, w)
                else:
                    mp[gi, tp + 128, t] += 1.0 / w
            mc[gi, t, t] -= 1.0
            mf[gi, t, t] -= 1.0
    c["c_MC"] = np.ascontiguousarray(mc.transpose(1, 0, 2).reshape(128, 512))
    c["c_MP"] = np.ascontiguousarray(mp.transpose(1, 0, 2).reshape(128, 512))
    c["c_MF"] = np.ascontiguousarray(mf.transpose(1, 0, 2).reshape(128, 512))
    c["c_iota"] = np.tile(np.arange(16, dtype=np.float32)[None, :], (128, 1))
    return c


CONST_SHAPES = {"c_ident": [128, 128], "c_ones": [128, 128], "c_U": [128, 128], "c_M1": [128, 512],
                "c_M2": [128, 512], "c_MC": [128, 512], "c_MP": [128, 512], "c_MF": [128, 512],
                "c_iota": [128, 16]}


def build(NT, mode="full"):
    nc = bass.Bass("TRN2", target_bir_lowering=False)
    T = NT * 128

    def din(name, shape, dt=F32):
        return nc.dram_tensor(name, shape, dt, kind="ExternalInput").ap()

    x_d = din("x", [T, 1024])
    nmix_d = din("norm_mix_w", [128, 8])
    win_d = din("w_in", [1024, 2568])
    cw_d = din("conv_w", [4, 1536])
    alog_d = din("a_log", [1, 4])
    dtb_d = din("dt_bias", [1, 4])
    dnw_d = din("dn_norm_w", [1, 128])
    wpool_d = din("w_pool", [4, 128, 128])
    pscale_d = din("pool_scale", [1, 512])
    wout_d = din("w_out", [1024, 1024])
    nffn_d = din("norm_ffn_w", [1, 1024])
    wq_d = din("w_query", [1024, 2048])
    sk_d = din("sub_keys", [16, 128, 128])
    ec_d = din("expert_cat", [16384, 2048])
    nfin_d = din("norm_final_w", [1, 1024])
    cd = {k: din(k, s) for k, s in CONST_SHAPES.items()}
    out_d = nc.dram_tensor("out", [T, 1024], F32, kind="ExternalOutput").ap()
    x1_d = nc.dram_tensor("x1s", [T, 1024], F32, kind="Internal").ap()
    ecb_d = nc.dram_tensor("ecb", [16384, 2048], BF16, kind="Internal").ap()

    P = Plan()

    def MM(out, lhsT, rhs, r, w, start=True, stop=True):
        P.op("pe", lambda E: E.matmul(out, lhsT=lhsT, rhs=rhs, start=start, stop=stop), reads=r, writes=w)

    def TR(out, in_, ident, r, w):
        P.op("pe", lambda E: E.transpose(out=out, in_=in_, identity=ident), reads=r, writes=w)

    def ACT(out, in_, func, r, w, **kw):
        P.op("act", lambda E: E.activation(out=out, in_=in_, func=func, **kw), reads=r, writes=w)

    def TT(eng, out, in0, in1, op, r, w):
        P.op(eng, lambda E: E.tensor_tensor(out=out, in0=in0, in1=in1, op=op), reads=r, writes=w)

    def TS(eng, out, in0, s1, s2, op0, op1, r, w):
        if op1 is None:
            P.op(eng, lambda E: E.tensor_scalar(out=out, in0=in0, scalar1=s1, scalar2=None, op0=op0), reads=r, writes=w)
        else:
            P.op(eng, lambda E: E.tensor_scalar(out=out, in0=in0, scalar1=s1, scalar2=s2, op0=op0, op1=op1), reads=r, writes=w)

    def STT(out, in0, scalar, in1, op0, op1, r, w, accum=None):
        if accum is None:
            P.op("dve", lambda E: E.scalar_tensor_tensor(out=out, in0=in0, scalar=scalar, in1=in1, op0=op0, op1=op1), reads=r, writes=w)
        else:
            P.op("dve", lambda E: E.scalar_tensor_tensor(out=out, in0=in0, scalar=scalar, in1=in1, op0=op0, op1=op1, accum_out=accum), reads=r, writes=w)

    def CP(eng, out, in_, r, w):
        if eng == "act":
            P.op("act", lambda E: E.copy(out=out, in_=in_), reads=r, writes=w)
        else:
            P.op(eng, lambda E: E.tensor_copy(out=out, in_=in_), reads=r, writes=w)

    def RED(out, in_, r, w, op=ALU.add):
        P.op("dve", lambda E: E.tensor_reduce(out=out, in_=in_, axis=AX.X, op=op), reads=r, writes=w)

    def DMA(out, in_, r, w, eng="sp"):
        P.op(eng, lambda E: E.dma_start(out=out, in_=in_), reads=r, writes=w, dma=True)

    def RSTD(out, ss, scale, r_junk=None):
        ACT(out[:], ss[:], AF.Sqrt, [ss, epsb], [out], scale=scale, bias=epsb[:, 0:1])
        P.op("dve", lambda E: E.reciprocal(out=out[:], in_=out[:]), reads=[out], writes=[out])

    def STOP(tag, pieces):
        if mode == tag:
            col = 0
            for (ap, tl, n) in pieces:
                DMA(out_d[0:128, col:col + n], ap, [tl], [])
                col += n
            P.stopped = True

    with ExitStack() as es0:
        def sb0(name, shape, dt=F32):
            return Tile(name, es0.enter_context(nc.sbuf_tensor(name, shape, dt)))

        def ps0(name, shape, dt=F32):
            return Tile(name, es0.enter_context(nc.psum_tensor(name, shape, dt)))

        banks = [ps0("pb%d" % i, [128, 512]) for i in range(7)]
        pT = ps0("pT", [128, 1024], BF16)
        bstate = {"i": 0, "n": 7}

        def nb():
            b = banks[bstate["i"] % bstate["n"]]
            bstate["i"] += 1
            return b

        ident = sb0("ident", [128, 128])
        identb = sb0("identb", [128, 128], BF16)
        ones = sb0("ones", [128, 128])
        epsb = sb0("epsb", [128, 1])
        xt = sb0("xt", [128, 1024])
        junk = sb0("junk", [128, 2568])
        ss = sb0("ss", [128, 1])
        rstd = sb0("rstd", [128, 1])
        hbf = sb0("hbf", [128, 1024], BF16)
        hT = sb0("hT", [128, 8, 128], BF16)
        stage = junk

        DMA(ident[:], cd["c_ident"], [], [ident])
        DMA(ones[:], cd["c_ones"], [], [ones])
        CP("dve", identb[:], ident[:], [ident], [identb])
        P.op("dve", lambda E: E.memset(epsb[:], EPS), writes=[epsb])

        def load_weight_bf(dst, src_d, ncols, scale_tile=None):
            for k in range(8):
                DMA(stage[:, 0:ncols], src_d[k * 128:(k + 1) * 128, :], [], [stage])
                if scale_tile is None:
                    CP("dve", dst[:, k, :], stage[:, 0:ncols], [stage], [dst])
                else:
                    TS("dve", dst[:, k, :], stage[:, 0:ncols], scale_tile[:, k:k + 1], None, ALU.mult, None,
                       [stage, scale_tile], [dst])

        def transpose8(src_bf, dstT):
            for k in range(8):
                TR(pT[:, k * 128:(k + 1) * 128], src_bf[:, k * 128:(k + 1) * 128], identb[:], [src_bf, identb], [pT])
            CP("act", dstT[:].rearrange("p k t -> p (k t)"), pT[:], [pT], [dstT])

        try:
          with ExitStack() as es1:
              def sb(name, shape, dt=F32):
                  return Tile(name, es1.enter_context(nc.sbuf_tensor(name, shape, dt)))

              Win = sb("Win", [128, 8, 2568], BF16)
              Wout = sb("Wout", [128, 8, 1024], BF16)
              nmix = sb("nmix", [128, 8])
              cwT = sb("cwT", [128, 12, 4])
              wpool = sb("wpool", [128, 4, 128])
              dnw_b = sb("dnw_b", [128, 128])
              pscale_b = sb("pscale_b", [128, 512])
              dtb_b = sb("dtb_b", [128, 4])
              negA = sb("negA", [128, 4])
              Um = sb("Um", [128, 128])
              M1 = sb("M1", [128, 512])
              M2 = sb("M2", [128, 512])
              MC = sb("MC", [128, 512])
              MPm = sb("MPm", [128, 512])
              MF = sb("MF", [128, 512])

              DMA(nmix[:], nmix_d, [], [nmix])
              load_weight_bf(Win, win_d, 2568, nmix)
              load_weight_bf(Wout, wout_d, 1024)
              DMA(stage[0:4, 0:1536], cw_d, [], [stage])
              bcw = nb()
              for c in range(12):
                  TR(bcw[:, c * 4:(c + 1) * 4], stage[0:4, c * 128:(c + 1) * 128], ident[0:4, 0:4], [stage, ident], [bcw])
              CP("act", cwT[:].rearrange("p c j -> p (c j)"), bcw[:, 0:48], [bcw], [cwT])
              DMA(wpool[:], wpool_d.rearrange("g c d -> c g d"), [], [wpool])
              DMA(dnw_b[:], dnw_d.partition_broadcast(128), [], [dnw_b])
              DMA(pscale_b[:], pscale_d.partition_broadcast(128), [], [pscale_b])
              DMA(dtb_b[:], dtb_d.partition_broadcast(128), [], [dtb_b])
              DMA(negA[:], alog_d.partition_broadcast(128), [], [negA])
              ACT(negA[:], negA[:], AF.Exp, [negA], [negA])
              TS("dve", negA[:], negA[:], -1.0, None, ALU.mult, None, [negA], [negA])
              for tl, nm in ((Um, "c_U"), (M1, "c_M1"), (M2, "c_M2"), (MC, "c_MC"), (MPm, "c_MP"), (MF, "c_MF")):
                  DMA(tl[:], cd[nm], [], [tl])

              qkvT = sb("qkvT", [128, 12, 131])
              cacc = sb("cacc", [128, 12, 128])
              qkv = sb("qkv", [128, 1536])
              proj2 = sb("proj2", [128, 1032])
              pprev = sb("pprev", [128, 512])
              S = sb("S", [128, 4, 128])
              qn = sb("qn", [128, 512])
              qg = sb("qg", [128, 512])
              kn = sb("kn", [128, 512])
              ss8 = sb("ss8", [128, 8])
              rn8 = sb("rn8", [128, 8])
              beta = sb("beta", [128, 4])
              nbeta = sb("nbeta", [128, 4])
              gt = sb("gt", [128, 4])
              gc = sb("gc", [128, 4])
              ngc = sb("ngc", [128, 4])
              glb = sb("glb", [128, 4])
              eg = sb("eg", [128, 4])
              egl = sb("egl", [128, 4])
              ekd = sb("ekd", [128, 4])
              bk = sb("bk", [128, 4])
              gU4 = sb("gU4", [128, 4, 128])
              decS = sb("decS", [128, 4, 128])
              decT = sb("decT", [128, 4, 128])
              vb = sb("vb", [128, 4, 128])
              kbg = sb("kbg", [128, 4, 128])
              kd = sb("kd", [128, 4, 128])
              kT4 = sb("kT4", [128, 4, 128])
              qT4 = sb("qT4", [128, 4, 128])
              qgT4 = sb("qgT4", [128, 4, 128])
              Qb = [sb("Qb%d" % i, [128, 128]) for i in range(2)]
              Pb = [sb("Pb%d" % i, [128, 128]) for i in range(2)]
              Xb = [sb("Xb%d" % i, [128, 128]) for i in range(2)]
              AT = sb("AT", [128, 128])
              nkcdT = sb("nkcdT", [128, 128])
              vnew = sb("vnew", [128, 128])
              osb = sb("osb", [128, 512])
              ss4 = sb("ss4", [128, 4])
              rn4 = sb("rn4", [128, 4])
              gz = sb("gz", [128, 512])
              mix = sb("mix", [128, 1024])
              plT = sb("plT", [128, 4, 128])

              P.op("dve", lambda E: E.memset(qkvT[:], 0.0), writes=[qkvT])
              P.op("dve", lambda E: E.memset(pprev[:], 0.0), writes=[pprev])
              P.op("dve", lambda E: E.memset(S[:], 0.0), writes=[S])

              for t in range(NT):
                  DMA(xt[:], x_d[t * 128:(t + 1) * 128, :], [], [xt])
                  ACT(junk[:, 0:1024], xt[:], AF.Square, [xt], [junk, ss], accum_out=ss[:])
                  RSTD(rstd, ss, 1.0 / 1024)
                  TS("dve", hbf[:], xt[:], rstd[:, 0:1], None, ALU.mult, None, [xt, rstd], [hbf])
                  transpose8(hbf, hT)
                  for b3 in range(3):
                      bk_ = nb()
                      for cc in range(4):
                          c = b3 * 4 + cc
                          for k in range(8):
                              MM(bk_[:, cc * 128:(cc + 1) * 128], Win[:, k, c * 128:(c + 1) * 128], hT[:, k, :],
                                 [Win, hT], [bk_], start=(k == 0), stop=(k == 7))
                      CP("act", qkvT[:, b3 * 4:(b3 + 1) * 4, 3:131],
                         bk_[:].rearrange("p (c t) -> p c t", c=4), [bk_], [qkvT])
                  pz = nb()
                  pg = nb()
                  pp = nb()
                  for k in range(8):
                      MM(pz[:, :], hT[:, k, :], Win[:, k, 1536:2048], [Win, hT], [pz], start=(k == 0), stop=(k == 7))
                  for k in range(8):
                      MM(pg[:, 0:8], hT[:, k, :], Win[:, k, 2048:2056], [Win, hT], [pg], start=(k == 0), stop=(k == 7))
                  for k in range(8):
                      MM(pp[:, :], hT[:, k, :], Win[:, k, 2056:2568], [Win, hT], [pp], start=(k == 0), stop=(k == 7))
                  CP("act", proj2[:, 0:512], pz[:], [pz], [proj2])
                  CP("act", proj2[:, 512:520], pg[:, 0:8], [pg], [proj2])
                  CP("act", proj2[:, 520:1032], pp[:], [pp], [proj2])
                  STOP("a", [(proj2[:, 0:1024], proj2, 1024)])
                  for c in range(12):
                      TS("dve", cacc[:, c, :], qkvT[:, c, 3:131], cwT[:, c, 3:4], None, ALU.mult, None, [qkvT, cwT], [cacc])
                      for j in range(3):
                          STT(cacc[:, c, :], qkvT[:, c, j:j + 128], cwT[:, c, j:j + 1], cacc[:, c, :], ALU.mult, ALU.add,
                              [qkvT, cwT, cacc], [cacc])
                  CP("dve", qkvT[:, :, 0:3], qkvT[:, :, 128:131], [qkvT], [qkvT])
                  ACT(cacc[:], cacc[:], AF.Silu, [cacc], [cacc])
                  for b3 in range(3):
                      bk_ = nb()
                      for cc in range(4):
                          c = b3 * 4 + cc
                          TR(bk_[:, cc * 128:(cc + 1) * 128], cacc[:, c, :], ident[:], [cacc, ident], [bk_])
                      CP("act", qkv[:, b3 * 512:(b3 + 1) * 512], bk_[:], [bk_], [qkv])
                  TT("dve", junk[:, 0:1024], qkv[:, 0:1024], qkv[:, 0:1024], ALU.mult, [qkv], [junk])
                  RED(ss8[:], junk[:, 0:1024].rearrange("p (h c) -> p h c", h=8), [junk], [ss8])
                  RSTD(rn8, ss8, 1.0)
                  TT("dve", kn[:].rearrange("p (h c) -> p h c", h=4), qkv[:, 512:1024].rearrange("p (h c) -> p h c", h=4),
                     rn8[:, 4:8].unsqueeze(2).to_broadcast([128, 4, 128]), ALU.mult, [qkv, rn8], [kn])
                  TS("dve", rn8[:, 0:4], rn8[:, 0:4], 128.0 ** -0.5, None, ALU.mult, None, [rn8], [rn8])
                  TT("dve", qn[:].rearrange("p (h c) -> p h c", h=4), qkv[:, 0:512].rearrange("p (h c) -> p h c", h=4),
                     rn8[:, 0:4].unsqueeze(2).to_broadcast([128, 4, 128]), ALU.mult, [qkv, rn8], [qn])
                  STOP("b", [(qn[:], qn, 512), (kn[:], kn, 512)])
                  ACT(beta[:], proj2[:, 512:516], AF.Sigmoid, [proj2], [beta])
                  TS("dve", nbeta[:], beta[:], -1.0, None, ALU.mult, None, [beta], [nbeta])
                  TT("dve", gt[:], proj2[:, 516:520], dtb_b[:], ALU.add, [proj2, dtb_b], [gt])
                  ACT(gt[:], gt[:], AF.Exp, [gt], [gt])
                  ACT(gt[:], gt[:], AF.Ln, [gt], [gt], bias=1.0)
                  TT("dve", gt[:], gt[:], negA[:], ALU.mult, [gt, negA], [gt])
                  pgc = nb()
                  MM(pgc[:, 0:4], Um[:], gt[:], [Um, gt], [pgc])
                  MM(pgc[:, 8:12], ones[:], gt[:], [ones, gt], [pgc])
                  CP("dve", gc[:], pgc[:, 0:4], [pgc], [gc])
                  CP("dve", glb[:], pgc[:, 8:12], [pgc], [glb])
                  TS("dve", ngc[:], gc[:], -1.0, None, ALU.mult, None, [gc], [ngc])
                  ACT(eg[:], gc[:], AF.Exp, [gc], [eg])
                  ACT(egl[:], glb[:], AF.Exp, [glb], [egl])
                  TT("dve", ekd[:], glb[:], gc[:], ALU.subtract, [glb, gc], [ekd])
                  ACT(ekd[:], ekd[:], AF.Exp, [ekd], [ekd])
                  TT("dve", bk[:], beta[:], eg[:], ALU.mult, [beta, eg], [bk])
                  TT("dve", gU4[:], Um[:].unsqueeze(1).to_broadcast([128, 4, 128]),
                     gt[:].unsqueeze(2).to_broadcast([128, 4, 128]), ALU.mult, [Um, gt], [gU4])
                  R1 = nb()
                  R2 = nb()
                  MM(R1[:], ones[:], gU4[:].rearrange("p h f -> p (h f)"), [ones, gU4], [R1], start=True, stop=False)
                  MM(R1[:], ident[:], M1[:], [ident, M1], [R1], start=False, stop=True)
                  MM(R2[:], ones[:], gU4[:].rearrange("p h f -> p (h f)"), [ones, gU4], [R2], start=True, stop=False)
                  MM(R2[:], ident[:], M2[:], [ident, M2], [R2], start=False, stop=True)
                  for h in range(4):
                      ACT(decS[:, h, :], R1[:, h * 128:(h + 1) * 128], AF.Exp, [R1, gc], [decS], scale=-1.0, bias=gc[:, h:h + 1])
                      ACT(decT[:, h, :], R2[:, h * 128:(h + 1) * 128], AF.Exp, [R2, ngc], [decT], scale=1.0, bias=ngc[:, h:h + 1])
                  v3 = qkv[:, 1024:1536].rearrange("p (h c) -> p h c", h=4)
                  kn3 = kn[:].rearrange("p (h c) -> p h c", h=4)
                  TT("dve", vb[:], v3, beta[:].unsqueeze(2).to_broadcast([128, 4, 128]), ALU.mult, [qkv, beta], [vb])
                  TT("dve", kbg[:], kn3, bk[:].unsqueeze(2).to_broadcast([128, 4, 128]), ALU.mult, [kn, bk], [kbg])
                  TT("dve", kd[:], kn3, ekd[:].unsqueeze(2).to_broadcast([128, 4, 128]), ALU.mult, [kn, ekd], [kd])
                  TT("dve", qg[:].rearrange("p (h c) -> p h c", h=4), qn[:].rearrange("p (h c) -> p h c", h=4),
                     eg[:].unsqueeze(2).to_broadcast([128, 4, 128]), ALU.mult, [qn, eg], [qg])
                  for (src, dst) in ((kn, kT4), (qn, qT4), (qg, qgT4)):
                      bk_ = nb()
                      for h in range(4):
                          TR(bk_[:, h * 128:(h + 1) * 128], src[:, h * 128:(h + 1) * 128], ident[:], [src, ident], [bk_])
                      CP("act", dst[:].rearrange("p h t -> p (h t)"), bk_[:], [bk_], [dst])
                      STOP("c", [(decS[:].rearrange("p h t -> p (h t)"), decS, 512), (kT4[:].rearrange("p h t -> p (h t)"), kT4, 512)])
                  for h in range(4):
                      pG = nb()
                      MM(pG[:, 0:128], kT4[:, h, :], kT4[:, h, :], [kT4], [pG])
                      MM(pG[:, 128:256], kT4[:, h, :], qT4[:, h, :], [kT4, qT4], [pG])
                      if mode == "d0a":
                          CP("act", AT[:], pG[:, 0:128], [pG], [AT])
                      STOP("d0a", [(AT[:], AT, 128)])
                      Q, Pm, X = Qb[0], Pb[0], Xb[0]
                      STT(Q[:], pG[:, 0:128], nbeta[:, h:h + 1], decS[:, h, :], ALU.mult, ALU.mult, [pG, nbeta, decS], [Q])
                      STOP("d0b", [(Q[:], Q, 128)])
                      TT("dve", AT[:], pG[:, 128:256], decT[:, h, :], ALU.mult, [pG, decT], [AT])
                      STOP("d0d", [(AT[:], AT, 128)])
                      pP = nb()
                      TR(pP[:, 0:128], Q[:], ident[:], [Q, ident], [pP])
                      CP("dve", Pm[:], pP[:, 0:128], [pP], [Pm])
                      STOP("d0c", [(Pm[:], Pm, 128)])
                      TT("dve", X[:], pP[:, 0:128], ident[:], ALU.add, [pP, ident], [X])
                      STOP("d1", [(Q[:], Q, 128), (AT[:], AT, 128), (Pm[:], Pm, 128), (X[:], X, 128)])
                      qi = pi = xi = 0
                      for m in range(6):
                          Qn_, Pn_, Xn_ = Qb[1 - qi], Pb[1 - pi], Xb[1 - xi]
                          pq = nb()
                          MM(pq[:, 0:128], Pm[:], Q[:], [Pm, Q], [pq])
                          if m < 5:
                              MM(pq[:, 128:256], Q[:], Pm[:], [Pm, Q], [pq])
                          CP("act", Qn_[:], pq[:, 0:128], [pq], [Qn_])
                          if m < 5:
                              CP("act", Pn_[:], pq[:, 128:256], [pq], [Pn_])
                          px = nb()
                          MM(px[:, 0:128], Qn_[:], X[:], [Qn_, X], [px])
                          TT("dve", Xn_[:], px[:, 0:128], X[:], ALU.add, [px, X], [Xn_])
                          Q, Pm, X = Qn_, Pn_, Xn_
                          qi, pi, xi = 1 - qi, 1 - pi, 1 - xi
                          STOP("d2%d" % m, [(Q[:], Q, 128), (X[:], X, 128)])
                      pk = nb()
                      MM(pk[:, 0:128], kbg[:, h, :], X[:], [kbg, X], [pk])
                      TS("dve", nkcdT[:], pk[:, 0:128], -1.0, None, ALU.mult, None, [pk], [nkcdT])
                      pv = nb()
                      MM(pv[:, 0:128], X[:], vb[:, h, :], [X, vb], [pv], start=True, stop=False)
                      MM(pv[:, 0:128], nkcdT[:], S[:, h, :], [nkcdT, S], [pv], start=False, stop=True)
                      CP("act", vnew[:], pv[:, 0:128], [pv], [vnew])
                      STOP("d3", [(vnew[:], vnew, 128), (nkcdT[:], nkcdT, 128)])
                      po = nb()
                      MM(po[:, 0:128], qgT4[:, h, :], S[:, h, :], [qgT4, S], [po], start=True, stop=False)
                      MM(po[:, 0:128], AT[:], vnew[:], [AT, vnew], [po], start=False, stop=True)
                      MM(po[:, 128:256], kd[:, h, :], vnew[:], [kd, vnew], [po])
                      CP("dve", osb[:, h * 128:(h + 1) * 128], po[:, 0:128], [po], [osb])
                      STT(S[:, h, :], S[:, h, :], egl[:, h:h + 1], po[:, 128:256], ALU.mult, ALU.add, [S, egl, po], [S])
                      STOP("d", [(osb[:], osb, 512), (S[:].rearrange("p h t -> p (h t)"), S, 512)])
                  TT("dve", junk[:, 0:512], osb[:], osb[:], ALU.mult, [osb], [junk])
                  RED(ss4[:], junk[:, 0:512].rearrange("p (h c) -> p h c", h=4), [junk], [ss4])
                  RSTD(rn4, ss4, 1.0 / 128)
                  ACT(gz[:], proj2[:, 0:512], AF.Silu, [proj2], [gz])
                  TT("dve", gz[:].rearrange("p (h c) -> p h c", h=4), gz[:].rearrange("p (h c) -> p h c", h=4),
                     dnw_b[:].unsqueeze(1).to_broadcast([128, 4, 128]), ALU.mult, [gz, dnw_b], [gz])
                  TT("dve", osb[:].rearrange("p (h c) -> p h c", h=4), osb[:].rearrange("p (h c) -> p h c", h=4),
                     rn4[:].unsqueeze(2).to_broadcast([128, 4, 128]), ALU.mult, [osb, rn4], [osb])
                  TT("dve", mix[:, 0:512], osb[:], gz[:], ALU.mult, [osb, gz], [mix])
                  ppl = nb()
                  Mcur = MF if t == 0 else MC
                  for g in range(4):
                      MM(ppl[:, g * 128:(g + 1) * 128], proj2[:, 520 + g * 128:520 + (g + 1) * 128], Mcur[:, g * 128:(g + 1) * 128],
                         [proj2, Mcur], [ppl], start=True, stop=False)
                      MM(ppl[:, g * 128:(g + 1) * 128], pprev[:, g * 128:(g + 1) * 128], MPm[:, g * 128:(g + 1) * 128],
                         [pprev, MPm], [ppl], start=False, stop=True)
                  CP("act", plT[:].rearrange("p g t -> p (g t)"), ppl[:], [ppl], [plT])
                  CP("dve", pprev[:], proj2[:, 520:1032], [proj2], [pprev])
                  pmx = nb()
                  for g in range(4):
                      MM(pmx[:, g * 128:(g + 1) * 128], plT[:, g, :], wpool[:, g, :], [plT, wpool], [pmx])
                  TT("dve", mix[:, 512:1024], pmx[:], pscale_b[:], ALU.mult, [pmx, pscale_b], [mix])
                  CP("dve", hbf[:], mix[:], [mix], [hbf])
                  transpose8(hbf, hT)
                  for n in range(2):
                      py = nb()
                      for k in range(8):
                          MM(py[:], hT[:, k, :], Wout[:, k, n * 512:(n + 1) * 512], [hT, Wout], [py], start=(k == 0), stop=(k == 7))
                      TT("dve", xt[:, n * 512:(n + 1) * 512], xt[:, n * 512:(n + 1) * 512], py[:], ALU.add, [xt, py], [xt])
                  DMA(x1_d[t * 128:(t + 1) * 128, :], xt[:], [xt], ["x1d%d" % t])
                  if mode == "p1":
                      DMA(out_d[t * 128:(t + 1) * 128, :], xt[:], [xt], [])

        except StopBuild:
            pass
        with ExitStack() as es2:
          if mode != "p1":
              def sb(name, shape, dt=F32):
                  return Tile(name, es2.enter_context(nc.sbuf_tensor(name, shape, dt)))

              Wq = sb("Wq", [128, 8, 2048], BF16)
              skT = sb("skT", [128, 16, 128])
              nffn_b = sb("nffn_b", [128, 1024])
              nfin_b = sb("nfin_b", [128, 1024])
              iota16 = sb("iota16", [128, 16])
              xn = sb("xn", [128, 1024])
              qTs = sb("qTs", [128, 16, 128])
              s16 = sb("s16", [128, 16, 16])
              i16 = sb("i16", [128, 16, 16], U32)
              if16 = sb("if16", [128, 16, 16])
              work = sb("work", [128, 256])
              cand = sb("cand", [128, 8, 256])
              ts = sb("ts", [128, 8, 16])
              pos = sb("pos", [128, 8, 16], U32)
              posi = sb("posi", [128, 8, 16], U32)
              pa = sb("pa", [128, 8, 16])
              pbm = sb("pbm", [128, 8, 16])
              eq = sb("eq", [128, 8, 256])
              eidx = sb("eidx", [128, 8, 16])
              eidx2 = sb("eidx2", [128, 8, 16])
              eidi = sb("eidi", [128, 128], I32)
              gate = sb("gate", [128, 8, 16])
              gsum = sb("gsum", [128, 8])
              araw = sb("araw", [128, 128])
              coef = sb("coef", [128, 128])
              gl = sb("gl", [128, 128])
              djunk = sb("djunk", [128, 2048], BF16)
              with ExitStack() as es3:
                  NST = 3
                  stg = [Tile("stg%d" % i, es3.enter_context(nc.sbuf_tensor("stg%d" % i, [128, 2048], F32))) for i in range(NST)]
                  stb = [Tile("stb%d" % i, es3.enter_context(nc.sbuf_tensor("stb%d" % i, [128, 2048], BF16))) for i in range(NST)]
                  for c in range(128):
                      a, b = stg[c % NST], stb[c % NST]
                      DMA(a[:], ec_d[c * 128:(c + 1) * 128, :], [], [a])
                      CP(("act", "dve", "pool")[c % 3], b[:], a[:], [a], [b])
                      DMA(ecb_d[c * 128:(c + 1) * 128, :], b[:], [b], [("ecb", c)])
              NB = 16
              UVb = [sb("UVb%d" % i, [128, 2048], BF16) for i in range(NB)]
              dgb = [sb("dgb%d" % i, [128, 128], BF16) for i in range(NB)]
              bstate["n"] = 5
              accA, accB = banks[5], banks[6]

              load_weight_bf(Wq, wq_d, 2048)
              DMA(nffn_b[:], nffn_d.partition_broadcast(128), [], [nffn_b])
              DMA(nfin_b[:], nfin_d.partition_broadcast(128), [], [nfin_b])
              DMA(iota16[:], cd["c_iota"], [], [iota16])
              for b4 in range(4):
                  DMA(stage[:, 0:512].rearrange("p (b c) -> p b c", b=4), sk_d[b4 * 4:(b4 + 1) * 4].rearrange("b k c -> k b c"),
                      [], [stage])
                  bk_ = nb()
                  for bb in range(4):
                      TR(bk_[:, bb * 128:(bb + 1) * 128], stage[:, bb * 128:(bb + 1) * 128], ident[:], [stage, ident], [bk_])
                  CP("act", skT[:, b4 * 4:(b4 + 1) * 4, :].rearrange("p b k -> p (b k)"), bk_[:], [bk_], [skT])

              gidx = 0
              ECBK = [("ecb", c) for c in range(128)]
              for t in range(NT):
                  DMA(xt[:], x1_d[t * 128:(t + 1) * 128, :], ["x1d%d" % t], [xt])
                  ACT(junk[:, 0:1024], xt[:], AF.Square, [xt], [junk, ss], accum_out=ss[:])
                  RSTD(rstd, ss, 1.0 / 1024)
                  STT(xn[:], xt[:], rstd[:, 0:1], nffn_b[:], ALU.mult, ALU.mult, [xt, rstd, nffn_b], [xn])
                  CP("act", hbf[:], xn[:], [xn], [hbf])
                  transpose8(hbf, hT)
                  for b4 in range(4):
                      bk_ = nb()
                      for bb in range(4):
                          blk = b4 * 4 + bb
                          for k in range(8):
                              MM(bk_[:, bb * 128:(bb + 1) * 128], Wq[:, k, blk * 128:(blk + 1) * 128], hT[:, k, :],
                                 [Wq, hT], [bk_], start=(k == 0), stop=(k == 7))
                      CP("act", qTs[:, b4 * 4:(b4 + 1) * 4, :].rearrange("p b t -> p (b t)"), bk_[:], [bk_], [qTs])
                  for b4 in range(4):
                      bk_ = nb()
                      for bb in range(4):
                          blk = b4 * 4 + bb
                          MM(bk_[:, bb * 128:(bb + 1) * 128], qTs[:, blk, :], skT[:, blk, :], [qTs, skT], [bk_])
                      for bb in range(4):
                          blk = b4 * 4 + bb
                          sc = bk_[:, bb * 128:(bb + 1) * 128]
                          P.op("dve", lambda E, sc=sc, blk=blk: E.max(out=s16[:, blk, 0:8], in_=sc), reads=[bk_], writes=[s16])
                          P.op("dve", lambda E, sc=sc, blk=blk: E.max_index(out=i16[:, blk, 0:8], in_max=s16[:, blk, 0:8], in_values=sc),
                               reads=[bk_, s16], writes=[i16])
                          P.op("dve", lambda E, sc=sc, blk=blk: E.match_replace(out=work[:, 0:128], in_to_replace=s16[:, blk, 0:8],
                                                                               in_values=sc, imm_value=-1e30),
                               reads=[bk_, s16], writes=[work])
                          P.op("dve", lambda E, blk=blk: E.max(out=s16[:, blk, 8:16], in_=work[:, 0:128]), reads=[work], writes=[s16])
                          P.op("dve", lambda E, blk=blk: E.max_index(out=i16[:, blk, 8:16], in_max=s16[:, blk, 8:16],
                                                                    in_values=work[:, 0:128]), reads=[work, s16], writes=[i16])
                  CP("dve", if16[:], i16[:], [i16], [if16])
                  s4 = s16[:].rearrange("p (h two) k -> p h two k", two=2)
                  i4 = if16[:].rearrange("p (h two) k -> p h two k", two=2)
                  TT("dve", cand[:].rearrange("p h (a b) -> p h a b", a=16),
                     s4[:, :, 0, :].unsqueeze(3).to_broadcast([128, 8, 16, 16]),
                     s4[:, :, 1, :].unsqueeze(2).to_broadcast([128, 8, 16, 16]), ALU.add, [s16], [cand])
                  for h in range(8):
                      P.op("dve", lambda E, h=h: E.max(out=ts[:, h, 0:8], in_=cand[:, h, :]), reads=[cand], writes=[ts])
                      P.op("dve", lambda E, h=h: E.max_index(out=pos[:, h, 0:8], in_max=ts[:, h, 0:8], in_values=cand[:, h, :]),
                           reads=[cand, ts], writes=[pos])
                      P.op("dve", lambda E, h=h: E.match_replace(out=work[:], in_to_replace=ts[:, h, 0:8], in_values=cand[:, h, :],
                                                                 imm_value=-1e30), reads=[cand, ts], writes=[work])
                      P.op("dve", lambda E, h=h: E.max(out=ts[:, h, 8:16], in_=work[:]), reads=[work], writes=[ts])
                      P.op("dve", lambda E, h=h: E.max_index(out=pos[:, h, 8:16], in_max=ts[:, h, 8:16], in_values=work[:]),
                           reads=[work, ts], writes=[pos])
                  TS("dve", posi[:], pos[:], 15, None, ALU.bitwise_and, None, [pos], [posi])
                  CP("dve", pbm[:], posi[:], [posi], [pbm])
                  TS("dve", posi[:], pos[:], 4, None, ALU.logical_shift_right, None, [pos], [posi])
                  CP("dve", pa[:], posi[:], [posi], [pa])
                  eq4 = eq[:].rearrange("p h (k a) -> p h k a", k=16)
                  io4 = iota16[:].unsqueeze(1).unsqueeze(1).to_broadcast([128, 8, 16, 16])
                  TT("dve", eq4, pa[:].unsqueeze(3).to_broadcast([128, 8, 16, 16]), io4, ALU.is_equal, [pa, iota16], [eq])
                  TT("dve", eq4, eq4, i4[:, :, 0, :].unsqueeze(2).to_broadcast([128, 8, 16, 16]), ALU.mult, [eq, if16], [eq])
                  RED(eidx[:], eq4, [eq], [eidx])
                  TT("dve", eq4, pbm[:].unsqueeze(3).to_broadcast([128, 8, 16, 16]), io4, ALU.is_equal, [pbm, iota16], [eq])
                  TT("dve", eq4, eq4, i4[:, :, 1, :].unsqueeze(2).to_broadcast([128, 8, 16, 16]), ALU.mult, [eq, if16], [eq])
                  RED(eidx2[:], eq4, [eq], [eidx2])
                  STT(eidx[:].rearrange("p h k -> p (h k)"), eidx[:].rearrange("p h k -> p (h k)"), 128.0,
                      eidx2[:].rearrange("p h k -> p (h k)"), ALU.mult, ALU.add, [eidx, eidx2], [eidx])
                  CP("dve", eidi[:], eidx[:].rearrange("p h k -> p (h k)"), [eidx], [eidi])
                  TT("dve", gate[:], ts[:], ts[:, :, 0:1].to_broadcast([128, 8, 16]), ALU.subtract, [ts], [gate])
                  ACT(gate[:], gate[:], AF.Exp, [gate], [gate])
                  RED(gsum[:], gate[:], [gate], [gsum])
                  P.op("dve", lambda E: E.reciprocal(out=gsum[:], in_=gsum[:]), reads=[gsum], writes=[gsum])
                  TT("dve", gate[:], gate[:], gsum[:].unsqueeze(2).to_broadcast([128, 8, 16]), ALU.mult, [gate, gsum], [gate])
                  gflat = gate[:].rearrange("p h k -> p (h k)")
                  P.op("dve", lambda E: E.memset(araw[:], 0.0), writes=[("araw", q) for q in range(0, 128, GRP)])
                  for g0 in range(0, 0 if mode == "ng" else 128, GRP):
                      bufs = []
                      for s in range(g0, g0 + GRP):
                          bi = gidx % NB
                          gidx += 1
                          UV, dg = UVb[bi], dgb[bi]
                          bufs.append((s, UV, dg))
                          P.op("pool", lambda E, UV=UV, s=s: E.indirect_dma_start(
                              out=UV[:], out_offset=None, in_=ecb_d,
                              in_offset=bass.IndirectOffsetOnAxis(ap=eidi[:, s:s + 1], axis=0)),
                              reads=[eidi] + ECBK, writes=[UV], dma=True)
                      for (s, UV, dg) in bufs:
                          jk = s % 2
                          STT(djunk[:, jk * 1024:(jk + 1) * 1024], UV[:, 0:1024], 1.0, xn[:], ALU.mult, ALU.mult,
                              [UV, xn], [("dj", jk), ("araw", g0)], accum=araw[:, s:s + 1])
                      ACT(gl[:, g0:g0 + GRP], araw[:, g0:g0 + GRP], AF.Gelu, [("araw", g0)], [("gl", g0)])
                      for (s, UV, dg) in bufs:
                          TS("dve", dg[:], ident[:], gl[:, s:s + 1], gflat[:, s:s + 1], ALU.mult, ALU.mult,
                             [ident, ("gl", g0), gate], [dg])
                          MM(accA[:], dg[:], UV[:, 1024:1536], [dg, UV], [accA], start=(s == 0), stop=(s == 127))
                          MM(accB[:], dg[:], UV[:, 1536:2048], [dg, UV], [accB], start=(s == 0), stop=(s == 127))
                  if mode != "ng":
                      TT("dve", xt[:, 0:512], xt[:, 0:512], accA[:], ALU.add, [xt, accA], [xt])
                      TT("dve", xt[:, 512:1024], xt[:, 512:1024], accB[:], ALU.add, [xt, accB], [xt])
                  ACT(junk[:, 0:1024], xt[:], AF.Square, [xt], [junk, ss], accum_out=ss[:])
                  RSTD(rstd, ss, 1.0 / 1024)
                  STT(xn[:], xt[:], rstd[:, 0:1], nfin_b[:], ALU.mult, ALU.mult, [xt, rstd, nfin_b], [xn])
                  DMA(out_d[t * 128:(t + 1) * 128, :], xn[:], [xn], [])

        P.finish()
        P.emit(nc)
    return nc


_CACHE = {}


def make_in_maps(inputs, ncores, NT):
    consts = host_consts()
    f = lambda a: np.ascontiguousarray(np.asarray(a, dtype=np.float32))
    shared = {
        "norm_mix_w": np.ascontiguousarray(f(inputs["norm_mix_w"]).reshape(8, 128).T),
        "w_in": f(inputs["w_in"]).reshape(1024, 2568),
        "conv_w": f(inputs["conv_w"]).reshape(4, 1536),
        "a_log": f(inputs["a_log"]).reshape(1, 4),
        "dt_bias": f(inputs["dt_bias"]).reshape(1, 4),
        "dn_norm_w": f(inputs["dn_norm_w"]).reshape(1, 128),
        "w_pool": f(inputs["w_pool"]).reshape(4, 128, 128),
        "pool_scale": f(inputs["pool_scale"]).reshape(1, 512),
        "w_out": f(inputs["w_out"]).reshape(1024, 1024),
        "norm_ffn_w": f(inputs["norm_ffn_w"]).reshape(1, 1024),
        "w_query": f(inputs["w_query"]).reshape(1024, 2048),
        "sub_keys": f(inputs["sub_keys"]).reshape(16, 128, 128),
        "expert_cat": np.concatenate([f(inputs["expert_down"]).reshape(16384, 1024),
                                      f(inputs["expert_up"]).reshape(16384, 1024)], axis=1),
        "norm_final_w": f(inputs["norm_final_w"]).reshape(1, 1024),
    }
    shared.update(consts)
    x = f(inputs["x"])
    maps = []
    for c in range(ncores):
        m = dict(shared)
        m["x"] = np.ascontiguousarray(x[c, :NT * 128, :])
        maps.append(m)
    return maps


def kernel(**inputs):
    NT = 64
    ncores = 8
    if "nc" not in _CACHE:
        _CACHE["nc"] = build(NT)
    nc = _CACHE["nc"]
    maps = make_in_maps(inputs, ncores, NT)
    res = run_bass_kernel_spmd(nc, maps, core_ids=list(range(ncores)))
    out = np.stack([np.asarray(r["out"]) for r in res.results], axis=0)
    return out.astype(np.float32)
```

```python
from contextlib import ExitStack
import numpy as np
import ml_dtypes
import concourse.bass as bass
import concourse.mybir as mybir
from concourse.bass_utils import run_bass_kernel_spmd

F32 = mybir.dt.float32
F32R = mybir.dt.float32r
BF16 = mybir.dt.bfloat16
I32 = mybir.dt.int32
U32 = mybir.dt.uint32
AF = mybir.ActivationFunctionType
ALU = mybir.AluOpType
AX = mybir.AxisListType

ENG = ("pe", "act", "dve", "pool", "sp")
EPOCH = 20000
DEPOCH = 1200
NDMA = 8
EPS = 1e-6
BIG = 1.0e5
GRP = 4


class StopBuild(Exception):
    pass


class Tile:
    __slots__ = ("name", "t")

    def __init__(self, name, t):
        self.name = name
        self.t = t

    def __getitem__(self, k):
        return self.t[k]


class Plan:
    def __init__(self):
        self.streams = {e: [] for e in ENG}
        self.cnt = {e: 0 for e in ENG}
        self.lastw = {}
        self.readers = {}
        self.seen = {e: {} for e in ENG}
        self.dma_rr = {e: 0 for e in ENG}
        self.dma_n = {}
        self.dma_last = {}
        self.semkeys = []
        self.semset = set()
        self.stopped = False

    def _key(self, k):
        if k not in self.semset:
            self.semset.add(k)
            self.semkeys.append(k)
        return k

    def _need(self, e, ev, waits):
        grp, ep, val, key = ev
        cur = self.seen[e].get(grp)
        if cur is not None and cur >= (ep, val):
            return
        self.seen[e][grp] = (ep, val)
        waits.append((key, val))

    def op(self, e, fn, reads=(), writes=(), dma=False):
        if self.stopped:
            return None
        raw = []
        oth = []
        for b in reads:
            w = self.lastw.get(b)
            if w is not None:
                raw.append(w)
        for b in writes:
            w = self.lastw.get(b)
            if w is not None:
                oth.append(w)
            oth.extend(self.readers.get(b, ()))
        waits = []
        for ev in raw:
            self._need(e, ev, waits)
        for ev in oth:
            self._need(e, ev, waits)
        if dma:
            nd = NDMA
            j = self.dma_rr[e] % nd
            self.dma_rr[e] += 1
            prev = self.dma_last.get((e, j))
            if prev is not None:
                self._need(e, prev, waits)
            n = self.dma_n.get((e, j), 0)
            self.dma_n[(e, j)] = n + 1
            ep = n // DEPOCH
            val = (n % DEPOCH + 1) * 16
            key = self._key(("d", e, j, ep))
            ev = (("d", e, j), ep, val, key)
            self.dma_last[(e, j)] = ev
            inc = 16
        else:
            n = self.cnt[e]
            self.cnt[e] = n + 1
            ep = n // EPOCH
            val = n % EPOCH + 1
            key = self._key(("e", e, ep))
            ev = (("e", e), ep, val, key)
            inc = 1
        self.streams[e].append((waits, fn, key, inc))
        for b in reads:
            self.readers.setdefault(b, []).append(ev)
        for b in writes:
            self.lastw[b] = ev
            self.readers[b] = []
        return ev

    def finish(self):
        waits = []
        for (e, j), ev in self.dma_last.items():
            self._need("sp", ev, waits)
        for e in ENG:
            if e == "sp" or self.cnt[e] == 0:
                continue
            n = self.cnt[e] - 1
            ep = n // EPOCH
            val = n % EPOCH + 1
            self._need("sp", (("e", e), ep, val, ("e", e, ep)), waits)
        self.streams["sp"].append((waits, None, None, 0))

    def emit(self, nc):
        with ExitStack() as es:
            sems = {}
            for i, k in enumerate(self.semkeys):
                sems[k] = es.enter_context(nc.semaphore("s%d" % i))
            with nc.Block() as block:
                def run(name):
                    def body(E):
                        for waits, fn, key, inc in self.streams[name]:
                            for (wk, wv) in waits:
                                E.wait_ge(sems[wk], wv)
                            if fn is not None:
                                fn(E).then_inc(sems[key], inc)
                    return body
                block.tensor(run("pe"))
                block.scalar(run("act"))
                block.vector(run("dve"))
                block.gpsimd(run("pool"))
                block.sync(run("sp"))


def host_consts():
    c = {}
    c["c_ident"] = np.eye(128, dtype=np.float32)
    c["c_ones"] = np.ones((128, 128), dtype=np.float32)
    k = np.arange(128)[:, None]
    f = np.arange(128)[None, :]
    c["c_U"] = (k <= f).astype(np.float32)
    m1 = np.where(k <= f, BIG, 0.0).astype(np.float32)
    m2 = np.where(f < k, -BIG, 0.0).astype(np.float32)
    c["c_M1"] = np.tile(m1, (1, 4))
    c["c_M2"] = np.tile(m2, (1, 4))
    wins = (2, 4, 8, 16)
    mc = np.zeros((4, 128, 128), np.float32)
    mp = np.zeros((4, 128, 128), np.float32)
    mf = np.zeros((4, 128, 128), np.float32)
    for gi, w in enumerate(wins):
        for t in range(128):
            for j in range(w):
                tp = t - j
                if tp >= 0:
                    mc[gi, tp, t] += 1.0 / w
                    mf[gi, tp, t] += 1.0 / min(t + 1, w)
                else:
                    mp[gi, tp + 128, t] += 1.0 / w
            mc[gi, t, t] -= 1.0
            mf[gi, t, t] -= 1.0
    c["c_MC"] = np.ascontiguousarray(mc.transpose(1, 0, 2).reshape(128, 512))
    c["c_MP"] = np.ascontiguousarray(mp.transpose(1, 0, 2).reshape(128, 512))
    c["c_MF"] = np.ascontiguousarray(mf.transpose(1, 0, 2).reshape(128, 512))
    c["c_iota"] = np.tile(np.arange(16, dtype=np.float32)[None, :], (128, 1))
    return c


CONST_SHAPES = {"c_ident": [128, 128], "c_ones": [128, 128], "c_U": [128, 128], "c_M1": [128, 512],
                "c_M2": [128, 512], "c_MC": [128, 512], "c_MP": [128, 512], "c_MF": [128, 512],
                "c_iota": [128, 16]}


def build(NT, mode="full"):
    nc = bass.Bass("TRN2", target_bir_lowering=False)
    T = NT * 128

    def din(name, shape, dt=F32):
        return nc.dram_tensor(name, shape, dt, kind="ExternalInput").ap()

    x_d = din("x", [T, 1024])
    nmix_d = din("norm_mix_w", [128, 8])
    win_d = din("w_in", [1024, 2568])
    cw_d = din("conv_w", [4, 1536])
    alog_d = din("a_log", [1, 4])
    dtb_d = din("dt_bias", [1, 4])
    dnw_d = din("dn_norm_w", [1, 128])
    wpool_d = din("w_pool", [4, 128, 128])
    pscale_d = din("pool_scale", [1, 512])
    wout_d = din("w_out", [1024, 1024])
    nffn_d = din("norm_ffn_w", [1, 1024])
    wq_d = din("w_query", [1024, 2048])
    sk_d = din("sub_keys", [16, 128, 128])
    ec_d = din("expert_cat", [16384, 2048])
    nfin_d = din("norm_final_w", [1, 1024])
    cd = {k: din(k, s) for k, s in CONST_SHAPES.items()}
    out_d = nc.dram_tensor("out", [T, 1024], F32, kind="ExternalOutput").ap()
    x1_d = nc.dram_tensor("x1s", [T, 1024], F32, kind="Internal").ap()
    ecb_d = nc.dram_tensor("ecb", [16384, 2048], BF16, kind="Internal").ap()

    P = Plan()

    def MM(out, lhsT, rhs, r, w, start=True, stop=True):
        P.op("pe", lambda E: E.matmul(out, lhsT=lhsT, rhs=rhs, start=start, stop=stop), reads=r, writes=w)

    def TR(out, in_, ident, r, w):
        P.op("pe", lambda E: E.transpose(out=out, in_=in_, identity=ident), reads=r, writes=w)

    def ACT(out, in_, func, r, w, **kw):
        P.op("act", lambda E: E.activation(out=out, in_=in_, func=func, **kw), reads=r, writes=w)

    def TT(eng, out, in0, in1, op, r, w):
        P.op(eng, lambda E: E.tensor_tensor(out=out, in0=in0, in1=in1, op=op), reads=r, writes=w)

    def TS(eng, out, in0, s1, s2, op0, op1, r, w):
        if op1 is None:
            P.op(eng, lambda E: E.tensor_scalar(out=out, in0=in0, scalar1=s1, scalar2=None, op0=op0), reads=r, writes=w)
        else:
            P.op(eng, lambda E: E.tensor_scalar(out=out, in0=in0, scalar1=s1, scalar2=s2, op0=op0, op1=op1), reads=r, writes=w)

    def STT(out, in0, scalar, in1, op0, op1, r, w, accum=None):
        if accum is None:
            P.op("dve", lambda E: E.scalar_tensor_tensor(out=out, in0=in0, scalar=scalar, in1=in1, op0=op0, op1=op1), reads=r, writes=w)
        else:
            P.op("dve", lambda E: E.scalar_tensor_tensor(out=out, in0=in0, scalar=scalar, in1=in1, op0=op0, op1=op1, accum_out=accum), reads=r, writes=w)

    def CP(eng, out, in_, r, w):
        if eng == "act":
            P.op("act", lambda E: E.copy(out=out, in_=in_), reads=r, writes=w)
        else:
            P.op(eng, lambda E: E.tensor_copy(out=out, in_=in_), reads=r, writes=w)

    def RED(out, in_, r, w, op=ALU.add):
        P.op("dve", lambda E: E.tensor_reduce(out=out, in_=in_, axis=AX.X, op=op), reads=r, writes=w)

    def DMA(out, in_, r, w, eng="sp"):
        P.op(eng, lambda E: E.dma_start(out=out, in_=in_), reads=r, writes=w, dma=True)

    def RSTD(out, ss, scale, r_junk=None):
        ACT(out[:], ss[:], AF.Sqrt, [ss, epsb], [out], scale=scale, bias=epsb[:, 0:1])
        P.op("dve", lambda E: E.reciprocal(out=out[:], in_=out[:]), reads=[out], writes=[out])

    def STOP(tag, pieces):
        if mode == tag:
            col = 0
            for (ap, tl, n) in pieces:
                DMA(out_d[0:128, col:col + n], ap, [tl], [])
                col += n
            P.stopped = True

    with ExitStack() as es0:
        def sb0(name, shape, dt=F32):
            return Tile(name, es0.enter_context(nc.sbuf_tensor(name, shape, dt)))

        def ps0(name, shape, dt=F32):
            return Tile(name, es0.enter_context(nc.psum_tensor(name, shape, dt)))

        banks = [ps0("pb%d" % i, [128, 512]) for i in range(7)]
        pT = ps0("pT", [128, 1024], BF16)
        bstate = {"i": 0, "n": 7}

        def nb():
            b = banks[bstate["i"] % bstate["n"]]
            bstate["i"] += 1
            return b

        ident = sb0("ident", [128, 128])
        identb = sb0("identb", [128, 128], BF16)
        ones = sb0("ones", [128, 128])
        epsb = sb0("epsb", [128, 1])
        xt = sb0("xt", [128, 1024])
        junk = sb0("junk", [128, 2568])
        ss = sb0("ss", [128, 1])
        rstd = sb0("rstd", [128, 1])
        hbf = sb0("hbf", [128, 1024], BF16)
        hT = sb0("hT", [128, 8, 128], BF16)
        stage = junk

        DMA(ident[:], cd["c_ident"], [], [ident])
        DMA(ones[:], cd["c_ones"], [], [ones])
        CP("dve", identb[:], ident[:], [ident], [identb])
        P.op("dve", lambda E: E.memset(epsb[:], EPS), writes=[epsb])

        def load_weight_bf(dst, src_d, ncols, scale_tile=None):
            for k in range(8):
                DMA(stage[:, 0:ncols], src_d[k * 128:(k + 1) * 128, :], [], [stage])
                if scale_tile is None:
                    CP("dve", dst[:, k, :], stage[:, 0:ncols], [stage], [dst])
                else:
                    TS("dve", dst[:, k, :], stage[:, 0:ncols], scale_tile[:, k:k + 1], None, ALU.mult, None,
                       [stage, scale_tile], [dst])

        def transpose8(src_bf, dstT):
            for k in range(8):
                TR(pT[:, k * 128:(k + 1) * 128], src_bf[:, k * 128:(k + 1) * 128], identb[:], [src_bf, identb], [pT])
            CP("act", dstT[:].rearrange("p k t -> p (k t)"), pT[:], [pT], [dstT])

        try:
          with ExitStack() as es1:
              def sb(name, shape, dt=F32):
                  return Tile(name, es1.enter_context(nc.sbuf_tensor(name, shape, dt)))

              Win = sb("Win", [128, 8, 2568], BF16)
              Wout = sb("Wout", [128, 8, 1024], BF16)
              nmix = sb("nmix", [128, 8])
              cwT = sb("cwT", [128, 12, 4])
              wpool = sb("wpool", [128, 4, 128])
              dnw_b = sb("dnw_b", [128, 128])
              pscale_b = sb("pscale_b", [128, 512])
              dtb_b = sb("dtb_b", [128, 4])
              negA = sb("negA", [128, 4])
              Um = sb("Um", [128, 128])
              M1 = sb("M1", [128, 512])
              M2 = sb("M2", [128, 512])
              MC = sb("MC", [128, 512])
              MPm = sb("MPm", [128, 512])
              MF = sb("MF", [128, 512])

              DMA(nmix[:], nmix_d, [], [nmix])
              load_weight_bf(Win, win_d, 2568, nmix)
              load_weight_bf(Wout, wout_d, 1024)
              DMA(stage[0:4, 0:1536], cw_d, [], [stage])
              bcw = nb()
              for c in range(12):
                  TR(bcw[:, c * 4:(c + 1) * 4], stage[0:4, c * 128:(c + 1) * 128], ident[0:4, 0:4], [stage, ident], [bcw])
              CP("act", cwT[:].rearrange("p c j -> p (c j)"), bcw[:, 0:48], [bcw], [cwT])
              DMA(wpool[:], wpool_d.rearrange("g c d -> c g d"), [], [wpool])
              DMA(dnw_b[:], dnw_d.partition_broadcast(128), [], [dnw_b])
              DMA(pscale_b[:], pscale_d.partition_broadcast(128), [], [pscale_b])
              DMA(dtb_b[:], dtb_d.partition_broadcast(128), [], [dtb_b])
              DMA(negA[:], alog_d.partition_broadcast(128), [], [negA])
              ACT(negA[:], negA[:], AF.Exp, [negA], [negA])
              TS("dve", negA[:], negA[:], -1.0, None, ALU.mult, None, [negA], [negA])
              for tl, nm in ((Um, "c_U"), (M1, "c_M1"), (M2, "c_M2"), (MC, "c_MC"), (MPm, "c_MP"), (MF, "c_MF")):
                  DMA(tl[:], cd[nm], [], [tl])

              qkvT = sb("qkvT", [128, 12, 131])
              cacc = sb("cacc", [128, 12, 128])
              qkv = sb("qkv", [128, 1536])
              proj2 = sb("proj2", [128, 1032])
              pprev = sb("pprev", [128, 512])
              S = sb("S", [128, 4, 128])
              qn = sb("qn", [128, 512])
              qg = sb("qg", [128, 512])
              kn = sb("kn", [128, 512])
              ss8 = sb("ss8", [128, 8])
              rn8 = sb("rn8", [128, 8])
              beta = sb("beta", [128, 4])
              nbeta = sb("nbeta", [128, 4])
              gt = sb("gt", [128, 4])
              gc = sb("gc", [128, 4])
              ngc = sb("ngc", [128, 4])
              glb = sb("glb", [128, 4])
              eg = sb("eg", [128, 4])
              egl = sb("egl", [128, 4])
              ekd = sb("ekd", [128, 4])
              bk = sb("bk", [128, 4])
              gU4 = sb("gU4", [128, 4, 128])
              decS = sb("decS", [128, 4, 128])
              decT = sb("decT", [128, 4, 128])
              vb = sb("vb", [128, 4, 128])
              kbg = sb("kbg", [128, 4, 128])
              kd = sb("kd", [128, 4, 128])
              kT4 = sb("kT4", [128, 4, 128])
              qT4 = sb("qT4", [128, 4, 128])
              qgT4 = sb("qgT4", [128, 4, 128])
              Qb = [[sb("Qb%d_%d" % (h, i), [128, 128]) for i in range(2)] for h in range(4)]
              Pb = [[sb("Pb%d_%d" % (h, i), [128, 128]) for i in range(2)] for h in range(4)]
              Xb = [[sb("Xb%d_%d" % (h, i), [128, 128]) for i in range(2)] for h in range(4)]
              ATb = [sb("AT%d" % h, [128, 128]) for h in range(4)]
              nkcdTb = [sb("nkcdT%d" % h, [128, 128]) for h in range(4)]
              vnewb = [sb("vnew%d" % h, [128, 128]) for h in range(4)]
              osb = sb("osb", [128, 512])
              ss4 = sb("ss4", [128, 4])
              rn4 = sb("rn4", [128, 4])
              gz = sb("gz", [128, 512])
              mix = sb("mix", [128, 1024])
              plT = sb("plT", [128, 4, 128])

              P.op("dve", lambda E: E.memset(qkvT[:], 0.0), writes=[qkvT])
              P.op("dve", lambda E: E.memset(pprev[:], 0.0), writes=[pprev])
              P.op("dve", lambda E: E.memset(S[:], 0.0), writes=[("S", h) for h in range(4)])

              for t in range(NT):
                  DMA(xt[:], x_d[t * 128:(t + 1) * 128, :], [], [xt])
                  ACT(junk[:, 0:1024], xt[:], AF.Square, [xt], [junk, ss], accum_out=ss[:])
                  RSTD(rstd, ss, 1.0 / 1024)
                  TS("dve", hbf[:], xt[:], rstd[:, 0:1], None, ALU.mult, None, [xt, rstd], [hbf])
                  transpose8(hbf, hT)
                  for b3 in range(3):
                      bk_ = nb()
                      for cc in range(4):
                          c = b3 * 4 + cc
                          for k in range(8):
                              MM(bk_[:, cc * 128:(cc + 1) * 128], Win[:, k, c * 128:(c + 1) * 128], hT[:, k, :],
                                 [Win, hT], [bk_], start=(k == 0), stop=(k == 7))
                      CP("act", qkvT[:, b3 * 4:(b3 + 1) * 4, 3:131],
                         bk_[:].rearrange("p (c t) -> p c t", c=4), [bk_], [qkvT])
                  pz = nb()
                  pg = nb()
                  pp = nb()
                  for k in range(8):
                      MM(pz[:, :], hT[:, k, :], Win[:, k, 1536:2048], [Win, hT], [pz], start=(k == 0), stop=(k == 7))
                  for k in range(8):
                      MM(pg[:, 0:8], hT[:, k, :], Win[:, k, 2048:2056], [Win, hT], [pg], start=(k == 0), stop=(k == 7))
                  for k in range(8):
                      MM(pp[:, :], hT[:, k, :], Win[:, k, 2056:2568], [Win, hT], [pp], start=(k == 0), stop=(k == 7))
                  CP("act", proj2[:, 0:512], pz[:], [pz], [proj2])
                  CP("act", proj2[:, 512:520], pg[:, 0:8], [pg], [proj2])
                  CP("act", proj2[:, 520:1032], pp[:], [pp], [proj2])
                  STOP("a", [(proj2[:, 0:1024], proj2, 1024)])
                  for c in range(12):
                      TS("dve", cacc[:, c, :], qkvT[:, c, 3:131], cwT[:, c, 3:4], None, ALU.mult, None, [qkvT, cwT], [cacc])
                      for j in range(3):
                          STT(cacc[:, c, :], qkvT[:, c, j:j + 128], cwT[:, c, j:j + 1], cacc[:, c, :], ALU.mult, ALU.add,
                              [qkvT, cwT, cacc], [cacc])
                  CP("dve", qkvT[:, :, 0:3], qkvT[:, :, 128:131], [qkvT], [qkvT])
                  ACT(cacc[:], cacc[:], AF.Silu, [cacc], [cacc])
                  for b3 in range(3):
                      bk_ = nb()
                      for cc in range(4):
                          c = b3 * 4 + cc
                          TR(bk_[:, cc * 128:(cc + 1) * 128], cacc[:, c, :], ident[:], [cacc, ident], [bk_])
                      CP("act", qkv[:, b3 * 512:(b3 + 1) * 512], bk_[:], [bk_], [qkv])
                  TT("dve", junk[:, 0:1024], qkv[:, 0:1024], qkv[:, 0:1024], ALU.mult, [qkv], [junk])
                  RED(ss8[:], junk[:, 0:1024].rearrange("p (h c) -> p h c", h=8), [junk], [ss8])
                  RSTD(rn8, ss8, 1.0)
                  TT("dve", kn[:].rearrange("p (h c) -> p h c", h=4), qkv[:, 512:1024].rearrange("p (h c) -> p h c", h=4),
                     rn8[:, 4:8].unsqueeze(2).to_broadcast([128, 4, 128]), ALU.mult, [qkv, rn8], [kn])
                  TS("dve", rn8[:, 0:4], rn8[:, 0:4], 128.0 ** -0.5, None, ALU.mult, None, [rn8], [rn8])
                  TT("dve", qn[:].rearrange("p (h c) -> p h c", h=4), qkv[:, 0:512].rearrange("p (h c) -> p h c", h=4),
                     rn8[:, 0:4].unsqueeze(2).to_broadcast([128, 4, 128]), ALU.mult, [qkv, rn8], [qn])
                  STOP("b", [(qn[:], qn, 512), (kn[:], kn, 512)])
                  ACT(beta[:], proj2[:, 512:516], AF.Sigmoid, [proj2], [beta])
                  TS("dve", nbeta[:], beta[:], -1.0, None, ALU.mult, None, [beta], [nbeta])
                  TT("dve", gt[:], proj2[:, 516:520], dtb_b[:], ALU.add, [proj2, dtb_b], [gt])
                  ACT(gt[:], gt[:], AF.Exp, [gt], [gt])
                  ACT(gt[:], gt[:], AF.Ln, [gt], [gt], bias=1.0)
                  TT("dve", gt[:], gt[:], negA[:], ALU.mult, [gt, negA], [gt])
                  pgc = nb()
                  MM(pgc[:, 0:4], Um[:], gt[:], [Um, gt], [pgc])
                  MM(pgc[:, 8:12], ones[:], gt[:], [ones, gt], [pgc])
                  CP("dve", gc[:], pgc[:, 0:4], [pgc], [gc])
                  CP("dve", glb[:], pgc[:, 8:12], [pgc], [glb])
                  TS("dve", ngc[:], gc[:], -1.0, None, ALU.mult, None, [gc], [ngc])
                  ACT(eg[:], gc[:], AF.Exp, [gc], [eg])
                  ACT(egl[:], glb[:], AF.Exp, [glb], [egl])
                  TT("dve", ekd[:], glb[:], gc[:], ALU.subtract, [glb, gc], [ekd])
                  ACT(ekd[:], ekd[:], AF.Exp, [ekd], [ekd])
                  TT("dve", bk[:], beta[:], eg[:], ALU.mult, [beta, eg], [bk])
                  TT("dve", gU4[:], Um[:].unsqueeze(1).to_broadcast([128, 4, 128]),
                     gt[:].unsqueeze(2).to_broadcast([128, 4, 128]), ALU.mult, [Um, gt], [gU4])
                  R1 = nb()
                  R2 = nb()
                  MM(R1[:], ones[:], gU4[:].rearrange("p h f -> p (h f)"), [ones, gU4], [R1], start=True, stop=False)
                  MM(R1[:], ident[:], M1[:], [ident, M1], [R1], start=False, stop=True)
                  MM(R2[:], ones[:], gU4[:].rearrange("p h f -> p (h f)"), [ones, gU4], [R2], start=True, stop=False)
                  MM(R2[:], ident[:], M2[:], [ident, M2], [R2], start=False, stop=True)
                  for h in range(4):
                      ACT(decS[:, h, :], R1[:, h * 128:(h + 1) * 128], AF.Exp, [R1, gc], [decS], scale=-1.0, bias=gc[:, h:h + 1])
                      ACT(decT[:, h, :], R2[:, h * 128:(h + 1) * 128], AF.Exp, [R2, ngc], [decT], scale=1.0, bias=ngc[:, h:h + 1])
                  v3 = qkv[:, 1024:1536].rearrange("p (h c) -> p h c", h=4)
                  kn3 = kn[:].rearrange("p (h c) -> p h c", h=4)
                  TT("dve", vb[:], v3, beta[:].unsqueeze(2).to_broadcast([128, 4, 128]), ALU.mult, [qkv, beta], [vb])
                  TT("dve", kbg[:], kn3, bk[:].unsqueeze(2).to_broadcast([128, 4, 128]), ALU.mult, [kn, bk], [kbg])
                  TT("dve", kd[:], kn3, ekd[:].unsqueeze(2).to_broadcast([128, 4, 128]), ALU.mult, [kn, ekd], [kd])
                  TT("dve", qg[:].rearrange("p (h c) -> p h c", h=4), qn[:].rearrange("p (h c) -> p h c", h=4),
                     eg[:].unsqueeze(2).to_broadcast([128, 4, 128]), ALU.mult, [qn, eg], [qg])
                  for (src, dst) in ((kn, kT4), (qn, qT4), (qg, qgT4)):
                      bk_ = nb()
                      for h in range(4):
                          TR(bk_[:, h * 128:(h + 1) * 128], src[:, h * 128:(h + 1) * 128], ident[:], [src, ident], [bk_])
                      CP("act", dst[:].rearrange("p h t -> p (h t)"), bk_[:], [bk_], [dst])
                      STOP("c", [(decS[:].rearrange("p h t -> p (h t)"), decS, 512), (kT4[:].rearrange("p h t -> p (h t)"), kT4, 512)])
                  hs = []
                  for h in range(4):
                      pG = nb()
                      MM(pG[:, 0:128], kT4[:, h, :], kT4[:, h, :], [kT4], [pG])
                      MM(pG[:, 128:256], kT4[:, h, :], qT4[:, h, :], [kT4, qT4], [pG])
                      Q, Pm, X = Qb[h][0], Pb[h][0], Xb[h][0]
                      STT(Q[:], pG[:, 0:128], nbeta[:, h:h + 1], decS[:, h, :], ALU.mult, ALU.mult, [pG, nbeta, decS], [Q])
                      TT("dve", ATb[h][:], pG[:, 128:256], decT[:, h, :], ALU.mult, [pG, decT], [ATb[h]])
                      pP = nb()
                      TR(pP[:, 0:128], Q[:], ident[:], [Q, ident], [pP])
                      CP("dve", Pm[:], pP[:, 0:128], [pP], [Pm])
                      TT("dve", X[:], pP[:, 0:128], ident[:], ALU.add, [pP, ident], [X])
                      hs.append([Q, Pm, X, 0])
                  for m in range(6):
                      nxt = []
                      for h in range(4):
                          Q, Pm, X, par = hs[h]
                          Qn_, Pn_, Xn_ = Qb[h][1 - par], Pb[h][1 - par], Xb[h][1 - par]
                          pq = nb()
                          MM(pq[:, 0:128], Pm[:], Q[:], [Pm, Q], [pq])
                          if m < 5:
                              MM(pq[:, 128:256], Q[:], Pm[:], [Pm, Q], [pq])
                          CP("act", Qn_[:], pq[:, 0:128], [pq], [Qn_])
                          if m < 5:
                              CP("act", Pn_[:], pq[:, 128:256], [pq], [Pn_])
                          nxt.append([Qn_, Pn_, Xn_, 1 - par])
                      for h in range(4):
                          X = hs[h][2]
                          Qn_, Pn_, Xn_, _ = nxt[h]
                          px = nb()
                          MM(px[:, 0:128], Qn_[:], X[:], [Qn_, X], [px])
                          TT("dve", Xn_[:], px[:, 0:128], X[:], ALU.add, [px, X], [Xn_])
                      hs = nxt
                  pks, pvs, pos_ = [], [], []
                  for h in range(4):
                      pk = nb()
                      MM(pk[:, 0:128], kbg[:, h, :], hs[h][2][:], [kbg, hs[h][2]], [pk])
                      pks.append(pk)
                  for h in range(4):
                      TS("dve", nkcdTb[h][:], pks[h][:, 0:128], -1.0, None, ALU.mult, None, [pks[h]], [nkcdTb[h]])
                  for h in range(4):
                      pv = nb()
                      MM(pv[:, 0:128], hs[h][2][:], vb[:, h, :], [hs[h][2], vb], [pv], start=True, stop=False)
                      MM(pv[:, 0:128], nkcdTb[h][:], S[:, h, :], [nkcdTb[h], ("S", h)], [pv], start=False, stop=True)
                      pvs.append(pv)
                  for h in range(4):
                      CP("act", vnewb[h][:], pvs[h][:, 0:128], [pvs[h]], [vnewb[h]])
                  for h in range(4):
                      po = nb()
                      MM(po[:, 0:128], qgT4[:, h, :], S[:, h, :], [qgT4, ("S", h)], [po], start=True, stop=False)
                      MM(po[:, 0:128], ATb[h][:], vnewb[h][:], [ATb[h], vnewb[h]], [po], start=False, stop=True)
                      MM(po[:, 128:256], kd[:, h, :], vnewb[h][:], [kd, vnewb[h]], [po])
                      pos_.append(po)
                  for h in range(4):
                      po = pos_[h]
                      CP("dve", osb[:, h * 128:(h + 1) * 128], po[:, 0:128], [po], [("osb", h)])
                      STT(S[:, h, :], S[:, h, :], egl[:, h:h + 1], po[:, 128:256], ALU.mult, ALU.add, [("S", h), egl, po], [("S", h)])
                  TT("dve", junk[:, 0:512], osb[:], osb[:], ALU.mult, [("osb", 0), ("osb", 1), ("osb", 2), ("osb", 3)], [junk])
                  RED(ss4[:], junk[:, 0:512].rearrange("p (h c) -> p h c", h=4), [junk], [ss4])
                  RSTD(rn4, ss4, 1.0 / 128)
                  ACT(gz[:], proj2[:, 0:512], AF.Silu, [proj2], [gz])
                  TT("dve", gz[:].rearrange("p (h c) -> p h c", h=4), gz[:].rearrange("p (h c) -> p h c", h=4),
                     dnw_b[:].unsqueeze(1).to_broadcast([128, 4, 128]), ALU.mult, [gz, dnw_b], [gz])
                  TT("dve", osb[:].rearrange("p (h c) -> p h c", h=4), osb[:].rearrange("p (h c) -> p h c", h=4),
                     rn4[:].unsqueeze(2).to_broadcast([128, 4, 128]), ALU.mult, [("osb", 0), ("osb", 1), ("osb", 2), ("osb", 3)] + [rn4], [("osb", 0), ("osb", 1), ("osb", 2), ("osb", 3)])
                  TT("dve", mix[:, 0:512], osb[:], gz[:], ALU.mult, [("osb", 0), ("osb", 1), ("osb", 2), ("osb", 3)] + [gz], [mix])
                  ppl = nb()
                  Mcur = MF if t == 0 else MC
                  for g in range(4):
                      MM(ppl[:, g * 128:(g + 1) * 128], proj2[:, 520 + g * 128:520 + (g + 1) * 128], Mcur[:, g * 128:(g + 1) * 128],
                         [proj2, Mcur], [ppl], start=True, stop=False)
                      MM(ppl[:, g * 128:(g + 1) * 128], pprev[:, g * 128:(g + 1) * 128], MPm[:, g * 128:(g + 1) * 128],
                         [pprev, MPm], [ppl], start=False, stop=True)
                  CP("act", plT[:].rearrange("p g t -> p (g t)"), ppl[:], [ppl], [plT])
                  CP("dve", pprev[:], proj2[:, 520:1032], [proj2], [pprev])
                  pmx = nb()
                  for g in range(4):
                      MM(pmx[:, g * 128:(g + 1) * 128], plT[:, g, :], wpool[:, g, :], [plT, wpool], [pmx])
                  TT("dve", mix[:, 512:1024], pmx[:], pscale_b[:], ALU.mult, [pmx, pscale_b], [mix])
                  CP("dve", hbf[:], mix[:], [mix], [hbf])
                  transpose8(hbf, hT)
                  for n in range(2):
                      py = nb()
                      for k in range(8):
                          MM(py[:], hT[:, k, :], Wout[:, k, n * 512:(n + 1) * 512], [hT, Wout], [py], start=(k == 0), stop=(k == 7))
                      TT("dve", xt[:, n * 512:(n + 1) * 512], xt[:, n * 512:(n + 1) * 512], py[:], ALU.add, [xt, py], [xt])
                  DMA(x1_d[t * 128:(t + 1) * 128, :], xt[:], [xt], ["x1d%d" % t])
                  if mode == "p1":
                      DMA(out_d[t * 128:(t + 1) * 128, :], xt[:], [xt], [])

        except StopBuild:
            pass
        with ExitStack() as es2:
          if mode != "p1":
              def sb(name, shape, dt=F32):
                  return Tile(name, es2.enter_context(nc.sbuf_tensor(name, shape, dt)))

              Wq = sb("Wq", [128, 8, 2048], BF16)
              skT = sb("skT", [128, 16, 128])
              nffn_b = sb("nffn_b", [128, 1024])
              nfin_b = sb("nfin_b", [128, 1024])
              iota16 = sb("iota16", [128, 16])
              xn = sb("xn", [128, 1024])
              qTs = sb("qTs", [128, 16, 128])
              s16 = sb("s16", [128, 16, 16])
              i16 = sb("i16", [128, 16, 16], U32)
              if16 = sb("if16", [128, 16, 16])
              work = sb("work", [128, 256])
              cand = sb("cand", [128, 8, 256])
              ts = sb("ts", [128, 8, 16])
              pos = sb("pos", [128, 8, 16], U32)
              posi = sb("posi", [128, 8, 16], U32)
              pa = sb("pa", [128, 8, 16])
              pbm = sb("pbm", [128, 8, 16])
              eq = sb("eq", [128, 8, 256])
              eidx = sb("eidx", [128, 8, 16])
              eidx2 = sb("eidx2", [128, 8, 16])
              eidi = sb("eidi", [128, 128], I32)
              gate = sb("gate", [128, 8, 16])
              gsum = sb("gsum", [128, 8])
              araw = sb("araw", [128, 128])
              coef = sb("coef", [128, 128])
              gl = sb("gl", [128, 128])
              djunk = sb("djunk", [128, 2048], BF16)
              with ExitStack() as es3:
                  NST = 3
                  stg = [Tile("stg%d" % i, es3.enter_context(nc.sbuf_tensor("stg%d" % i, [128, 2048], F32))) for i in range(NST)]
                  stb = [Tile("stb%d" % i, es3.enter_context(nc.sbuf_tensor("stb%d" % i, [128, 2048], BF16))) for i in range(NST)]
                  for c in range(128):
                      a, b = stg[c % NST], stb[c % NST]
                      DMA(a[:], ec_d[c * 128:(c + 1) * 128, :], [], [a])
                      CP(("act", "dve", "pool")[c % 3], b[:], a[:], [a], [b])
                      DMA(ecb_d[c * 128:(c + 1) * 128, :], b[:], [b], [("ecb", c)])
              NB = 16
              UVb = [sb("UVb%d" % i, [128, 2048], BF16) for i in range(NB)]
              dgb = [sb("dgb%d" % i, [128, 128], BF16) for i in range(NB)]
              bstate["n"] = 5
              accA, accB = banks[5], banks[6]

              load_weight_bf(Wq, wq_d, 2048)
              DMA(nffn_b[:], nffn_d.partition_broadcast(128), [], [nffn_b])
              DMA(nfin_b[:], nfin_d.partition_broadcast(128), [], [nfin_b])
              DMA(iota16[:], cd["c_iota"], [], [iota16])
              for b4 in range(4):
                  DMA(stage[:, 0:512].rearrange("p (b c) -> p b c", b=4), sk_d[b4 * 4:(b4 + 1) * 4].rearrange("b k c -> k b c"),
                      [], [stage])
                  bk_ = nb()
                  for bb in range(4):
                      TR(bk_[:, bb * 128:(bb + 1) * 128], stage[:, bb * 128:(bb + 1) * 128], ident[:], [stage, ident], [bk_])
                  CP("act", skT[:, b4 * 4:(b4 + 1) * 4, :].rearrange("p b k -> p (b k)"), bk_[:], [bk_], [skT])

              gidx = 0
              ECBK = [("ecb", c) for c in range(128)]
              for t in range(NT):
                  DMA(xt[:], x1_d[t * 128:(t + 1) * 128, :], ["x1d%d" % t], [xt])
                  ACT(junk[:, 0:1024], xt[:], AF.Square, [xt], [junk, ss], accum_out=ss[:])
                  RSTD(rstd, ss, 1.0 / 1024)
                  STT(xn[:], xt[:], rstd[:, 0:1], nffn_b[:], ALU.mult, ALU.mult, [xt, rstd, nffn_b], [xn])
                  CP("act", hbf[:], xn[:], [xn], [hbf])
                  transpose8(hbf, hT)
                  for b4 in range(4):
                      bk_ = nb()
                      for bb in range(4):
                          blk = b4 * 4 + bb
                          for k in range(8):
                              MM(bk_[:, bb * 128:(bb + 1) * 128], Wq[:, k, blk * 128:(blk + 1) * 128], hT[:, k, :],
                                 [Wq, hT], [bk_], start=(k == 0), stop=(k == 7))
                      CP("act", qTs[:, b4 * 4:(b4 + 1) * 4, :].rearrange("p b t -> p (b t)"), bk_[:], [bk_], [qTs])
                  for b4 in range(4):
                      bk_ = nb()
                      for bb in range(4):
                          blk = b4 * 4 + bb
                          MM(bk_[:, bb * 128:(bb + 1) * 128], qTs[:, blk, :], skT[:, blk, :], [qTs, skT], [bk_])
                      for bb in range(4):
                          blk = b4 * 4 + bb
                          sc = bk_[:, bb * 128:(bb + 1) * 128]
                          P.op("dve", lambda E, sc=sc, blk=blk: E.max(out=s16[:, blk, 0:8], in_=sc), reads=[bk_], writes=[s16])
                          P.op("dve", lambda E, sc=sc, blk=blk: E.max_index(out=i16[:, blk, 0:8], in_max=s16[:, blk, 0:8], in_values=sc),
                               reads=[bk_, s16], writes=[i16])
                          P.op("dve", lambda E, sc=sc, blk=blk: E.match_replace(out=work[:, 0:128], in_to_replace=s16[:, blk, 0:8],
                                                                               in_values=sc, imm_value=-1e30),
                               reads=[bk_, s16], writes=[work])
                          P.op("dve", lambda E, blk=blk: E.max(out=s16[:, blk, 8:16], in_=work[:, 0:128]), reads=[work], writes=[s16])
                          P.op("dve", lambda E, blk=blk: E.max_index(out=i16[:, blk, 8:16], in_max=s16[:, blk, 8:16],
                                                                    in_values=work[:, 0:128]), reads=[work, s16], writes=[i16])
                  CP("dve", if16[:], i16[:], [i16], [if16])
                  s4 = s16[:].rearrange("p (h two) k -> p h two k", two=2)
                  i4 = if16[:].rearrange("p (h two) k -> p h two k", two=2)
                  TT("dve", cand[:].rearrange("p h (a b) -> p h a b", a=16),
                     s4[:, :, 0, :].unsqueeze(3).to_broadcast([128, 8, 16, 16]),
                     s4[:, :, 1, :].unsqueeze(2).to_broadcast([128, 8, 16, 16]), ALU.add, [s16], [cand])
                  for h in range(8):
                      P.op("dve", lambda E, h=h: E.max(out=ts[:, h, 0:8], in_=cand[:, h, :]), reads=[cand], writes=[ts])
                      P.op("dve", lambda E, h=h: E.max_index(out=pos[:, h, 0:8], in_max=ts[:, h, 0:8], in_values=cand[:, h, :]),
                           reads=[cand, ts], writes=[pos])
                      P.op("dve", lambda E, h=h: E.match_replace(out=work[:], in_to_replace=ts[:, h, 0:8], in_values=cand[:, h, :],
                                                                 imm_value=-1e30), reads=[cand, ts], writes=[work])
                      P.op("dve", lambda E, h=h: E.max(out=ts[:, h, 8:16], in_=work[:]), reads=[work], writes=[ts])
                      P.op("dve", lambda E, h=h: E.max_index(out=pos[:, h, 8:16], in_max=ts[:, h, 8:16], in_values=work[:]),
                           reads=[work, ts], writes=[pos])
                  TS("dve", posi[:], pos[:], 15, None, ALU.bitwise_and, None, [pos], [posi])
                  CP("dve", pbm[:], posi[:], [posi], [pbm])
                  TS("dve", posi[:], pos[:], 4, None, ALU.logical_shift_right, None, [pos], [posi])
                  CP("dve", pa[:], posi[:], [posi], [pa])
                  eq4 = eq[:].rearrange("p h (k a) -> p h k a", k=16)
                  io4 = iota16[:].unsqueeze(1).unsqueeze(1).to_broadcast([128, 8, 16, 16])
                  TT("dve", eq4, pa[:].unsqueeze(3).to_broadcast([128, 8, 16, 16]), io4, ALU.is_equal, [pa, iota16], [eq])
                  TT("dve", eq4, eq4, i4[:, :, 0, :].unsqueeze(2).to_broadcast([128, 8, 16, 16]), ALU.mult, [eq, if16], [eq])
                  RED(eidx[:], eq4, [eq], [eidx])
                  TT("dve", eq4, pbm[:].unsqueeze(3).to_broadcast([128, 8, 16, 16]), io4, ALU.is_equal, [pbm, iota16], [eq])
                  TT("dve", eq4, eq4, i4[:, :, 1, :].unsqueeze(2).to_broadcast([128, 8, 16, 16]), ALU.mult, [eq, if16], [eq])
                  RED(eidx2[:], eq4, [eq], [eidx2])
                  STT(eidx[:].rearrange("p h k -> p (h k)"), eidx[:].rearrange("p h k -> p (h k)"), 128.0,
                      eidx2[:].rearrange("p h k -> p (h k)"), ALU.mult, ALU.add, [eidx, eidx2], [eidx])
                  CP("dve", eidi[:], eidx[:].rearrange("p h k -> p (h k)"), [eidx], [eidi])
                  TT("dve", gate[:], ts[:], ts[:, :, 0:1].to_broadcast([128, 8, 16]), ALU.subtract, [ts], [gate])
                  ACT(gate[:], gate[:], AF.Exp, [gate], [gate])
                  RED(gsum[:], gate[:], [gate], [gsum])
                  P.op("dve", lambda E: E.reciprocal(out=gsum[:], in_=gsum[:]), reads=[gsum], writes=[gsum])
                  TT("dve", gate[:], gate[:], gsum[:].unsqueeze(2).to_broadcast([128, 8, 16]), ALU.mult, [gate, gsum], [gate])
                  gflat = gate[:].rearrange("p h k -> p (h k)")
                  P.op("dve", lambda E: E.memset(araw[:], 0.0), writes=[("araw", q) for q in range(0, 128, GRP)])
                  for g0 in range(0, 0 if mode == "ng" else 128, GRP):
                      bufs = []
                      for s in range(g0, g0 + GRP):
                          bi = gidx % NB
                          gidx += 1
                          UV, dg = UVb[bi], dgb[bi]
                          bufs.append((s, UV, dg))
                          P.op("pool", lambda E, UV=UV, s=s: E.indirect_dma_start(
                              out=UV[:], out_offset=None, in_=ecb_d,
                              in_offset=bass.IndirectOffsetOnAxis(ap=eidi[:, s:s + 1], axis=0)),
                              reads=[eidi] + ECBK, writes=[UV], dma=True)
                      for (s, UV, dg) in bufs:
                          jk = s % 2
                          STT(djunk[:, jk * 1024:(jk + 1) * 1024], UV[:, 0:1024], 1.0, xn[:], ALU.mult, ALU.mult,
                              [UV, xn], [("dj", jk), ("araw", g0)], accum=araw[:, s:s + 1])
                      ACT(gl[:, g0:g0 + GRP], araw[:, g0:g0 + GRP], AF.Gelu, [("araw", g0)], [("gl", g0)])
                      for (s, UV, dg) in bufs:
                          TS("dve", dg[:], ident[:], gl[:, s:s + 1], gflat[:, s:s + 1], ALU.mult, ALU.mult,
                             [ident, ("gl", g0), gate], [dg])
                          MM(accA[:], dg[:], UV[:, 1024:1536], [dg, UV], [accA], start=(s == 0), stop=(s == 127))
                          MM(accB[:], dg[:], UV[:, 1536:2048], [dg, UV], [accB], start=(s == 0), stop=(s == 127))
                  if mode != "ng":
                      TT("dve", xt[:, 0:512], xt[:, 0:512], accA[:], ALU.add, [xt, accA], [xt])
                      TT("dve", xt[:, 512:1024], xt[:, 512:1024], accB[:], ALU.add, [xt, accB], [xt])
                  ACT(junk[:, 0:1024], xt[:], AF.Square, [xt], [junk, ss], accum_out=ss[:])
                  RSTD(rstd, ss, 1.0 / 1024)
                  STT(xn[:], xt[:], rstd[:, 0:1], nfin_b[:], ALU.mult, ALU.mult, [xt, rstd, nfin_b], [xn])
                  DMA(out_d[t * 128:(t + 1) * 128, :], xn[:], [xn], [])

        P.finish()
        P.emit(nc)
    return nc


_CACHE = {}


def make_in_maps(inputs, ncores, NT):
    consts = host_consts()
    f = lambda a: np.ascontiguousarray(np.asarray(a, dtype=np.float32))
    shared = {
        "norm_mix_w": np.ascontiguousarray(f(inputs["norm_mix_w"]).reshape(8, 128).T),
        "w_in": f(inputs["w_in"]).reshape(1024, 2568),
        "conv_w": f(inputs["conv_w"]).reshape(4, 1536),
        "a_log": f(inputs["a_log"]).reshape(1, 4),
        "dt_bias": f(inputs["dt_bias"]).reshape(1, 4),
        "dn_norm_w": f(inputs["dn_norm_w"]).reshape(1, 128),
        "w_pool": f(inputs["w_pool"]).reshape(4, 128, 128),
        "pool_scale": f(inputs["pool_scale"]).reshape(1, 512),
        "w_out": f(inputs["w_out"]).reshape(1024, 1024),
        "norm_ffn_w": f(inputs["norm_ffn_w"]).reshape(1, 1024),
        "w_query": f(inputs["w_query"]).reshape(1024, 2048),
        "sub_keys": f(inputs["sub_keys"]).reshape(16, 128, 128),
        "expert_cat": np.concatenate([f(inputs["expert_down"]).reshape(16384, 1024),
                                      f(inputs["expert_up"]).reshape(16384, 1024)], axis=1),
        "norm_final_w": f(inputs["norm_final_w"]).reshape(1, 1024),
    }
    shared.update(consts)
    x = f(inputs["x"])
    maps = []
    for c in range(ncores):
        m = dict(shared)
        m["x"] = np.ascontiguousarray(x[c, :NT * 128, :])
        maps.append(m)
    return maps


def kernel(**inputs):
    NT = 64
    ncores = 8
    if "nc" not in _CACHE:
        _CACHE["nc"] = build(NT)
    nc = _CACHE["nc"]
    maps = make_in_maps(inputs, ncores, NT)
    res = run_bass_kernel_spmd(nc, maps, core_ids=list(range(ncores)))
    out = np.stack([np.asarray(r["out"]) for r in res.results], axis=0)
    return out.astype(np.float32)
```

```python
from contextlib import ExitStack
import numpy as np
import ml_dtypes
import concourse.bass as bass
import concourse.mybir as mybir
from concourse.bass_utils import run_bass_kernel_spmd

F32 = mybir.dt.float32
F32R = mybir.dt.float32r
BF16 = mybir.dt.bfloat16
I32 = mybir.dt.int32
U32 = mybir.dt.uint32
AF = mybir.ActivationFunctionType
ALU = mybir.AluOpType
AX = mybir.AxisListType

ENG = ("pe", "act", "dve", "pool", "sp")
EPOCH = 20000
DEPOCH = 1200
NDMA = 8
EPS = 1e-6
BIG = 1.0e5
GRP = 4


class StopBuild(Exception):
    pass


class Tile:
    __slots__ = ("name", "t")

    def __init__(self, name, t):
        self.name = name
        self.t = t

    def __getitem__(self, k):
        return self.t[k]


class Plan:
    def __init__(self):
        self.streams = {e: [] for e in ENG}
        self.cnt = {e: 0 for e in ENG}
        self.lastw = {}
        self.readers = {}
        self.seen = {e: {} for e in ENG}
        self.dma_rr = {e: 0 for e in ENG}
        self.dma_n = {}
        self.dma_last = {}
        self.semkeys = []
        self.semset = set()
        self.stopped = False

    def _key(self, k):
        if k not in self.semset:
            self.semset.add(k)
            self.semkeys.append(k)
        return k

    def _need(self, e, ev, waits):
        grp, ep, val, key = ev
        cur = self.seen[e].get(grp)
        if cur is not None and cur >= (ep, val):
            return
        self.seen[e][grp] = (ep, val)
        waits.append((key, val))

    def op(self, e, fn, reads=(), writes=(), dma=False):
        if self.stopped:
            return None
        raw = []
        oth = []
        for b in reads:
            w = self.lastw.get(b)
            if w is not None:
                raw.append(w)
        for b in writes:
            w = self.lastw.get(b)
            if w is not None:
                oth.append(w)
            oth.extend(self.readers.get(b, ()))
        waits = []
        for ev in raw:
            self._need(e, ev, waits)
        for ev in oth:
            self._need(e, ev, waits)
        if dma:
            nd = NDMA
            j = self.dma_rr[e] % nd
            self.dma_rr[e] += 1
            prev = self.dma_last.get((e, j))
            if prev is not None:
                self._need(e, prev, waits)
            n = self.dma_n.get((e, j), 0)
            self.dma_n[(e, j)] = n + 1
            ep = n // DEPOCH
            val = (n % DEPOCH + 1) * 16
            key = self._key(("d", e, j, ep))
            ev = (("d", e, j), ep, val, key)
            self.dma_last[(e, j)] = ev
            inc = 16
        else:
            n = self.cnt[e]
            self.cnt[e] = n + 1
            ep = n // EPOCH
            val = n % EPOCH + 1
            key = self._key(("e", e, ep))
            ev = (("e", e), ep, val, key)
            inc = 1
        self.streams[e].append((waits, fn, key, inc))
        for b in reads:
            self.readers.setdefault(b, []).append(ev)
        for b in writes:
            self.lastw[b] = ev
            self.readers[b] = []
        return ev

    def finish(self):
        waits = []
        for (e, j), ev in self.dma_last.items():
            self._need("sp", ev, waits)
        for e in ENG:
            if e == "sp" or self.cnt[e] == 0:
                continue
            n = self.cnt[e] - 1
            ep = n // EPOCH
            val = n % EPOCH + 1
            self._need("sp", (("e", e), ep, val, ("e", e, ep)), waits)
        self.streams["sp"].append((waits, None, None, 0))

    def emit(self, nc):
        with ExitStack() as es:
            sems = {}
            for i, k in enumerate(self.semkeys):
                sems[k] = es.enter_context(nc.semaphore("s%d" % i))
            with nc.Block() as block:
                def run(name):
                    def body(E):
                        for waits, fn, key, inc in self.streams[name]:
                            for (wk, wv) in waits:
                                E.wait_ge(sems[wk], wv)
                            if fn is not None:
                                fn(E).then_inc(sems[key], inc)
                    return body
                block.tensor(run("pe"))
                block.scalar(run("act"))
                block.vector(run("dve"))
                block.gpsimd(run("pool"))
                block.sync(run("sp"))


def host_consts():
    c = {}
    c["c_ident"] = np.eye(128, dtype=np.float32)
    c["c_ones"] = np.ones((128, 128), dtype=np.float32)
    k = np.arange(128)[:, None]
    f = np.arange(128)[None, :]
    c["c_U"] = (k <= f).astype(np.float32)
    m1 = np.where(k <= f, BIG, 0.0).astype(np.float32)
    m2 = np.where(f < k, -BIG, 0.0).astype(np.float32)
    c["c_M1"] = np.tile(m1, (1, 4))
    c["c_M2"] = np.tile(m2, (1, 4))
    wins = (2, 4, 8, 16)
    mc = np.zeros((4, 128, 128), np.float32)
    mp = np.zeros((4, 128, 128), np.float32)
    mf = np.zeros((4, 128, 128), np.float32)
    for gi, w in enumerate(wins):
        for t in range(128):
            for j in range(w):
                tp = t - j
                if tp >= 0:
                    mc[gi, tp, t] += 1.0 / w
                    mf[gi, tp, t] += 1.0 / min(t + 1, w)
                else:
                    mp[gi, tp + 128, t] += 1.0 / w
            mc[gi, t, t] -= 1.0
            mf[gi, t, t] -= 1.0
    c["c_MC"] = np.ascontiguousarray(mc.transpose(1, 0, 2).reshape(128, 512))
    c["c_MP"] = np.ascontiguousarray(mp.transpose(1, 0, 2).reshape(128, 512))
    c["c_MF"] = np.ascontiguousarray(mf.transpose(1, 0, 2).reshape(128, 512))
    c["c_iota"] = np.tile(np.arange(16, dtype=np.float32)[None, :], (128, 1))
    return c


CONST_SHAPES = {"c_ident": [128, 128], "c_ones": [128, 128], "c_U": [128, 128], "c_M1": [128, 512],
                "c_M2": [128, 512], "c_MC": [128, 512], "c_MP": [128, 512], "c_MF": [128, 512],
                "c_iota": [128, 16]}


def build(NT, mode="full"):
    nc = bass.Bass("TRN2", target_bir_lowering=False)
    T = NT * 128

    def din(name, shape, dt=F32):
        return nc.dram_tensor(name, shape, dt, kind="ExternalInput").ap()

    x_d = din("x", [T, 1024])
    nmix_d = din("norm_mix_w", [128, 8])
    win_d = din("w_in", [1024, 2568])
    cw_d = din("conv_w", [4, 1536])
    alog_d = din("a_log", [1, 4])
    dtb_d = din("dt_bias", [1, 4])
    dnw_d = din("dn_norm_w", [1, 128])
    wpool_d = din("w_pool", [4, 128, 128])
    pscale_d = din("pool_scale", [1, 512])
    wout_d = din("w_out", [1024, 1024])
    nffn_d = din("norm_ffn_w", [1, 1024])
    wq_d = din("w_query", [1024, 2048])
    sk_d = din("sub_keys", [16, 128, 128])
    ec_d = din("expert_cat", [16384, 2048])
    nfin_d = din("norm_final_w", [1, 1024])
    cd = {k: din(k, s) for k, s in CONST_SHAPES.items()}
    out_d = nc.dram_tensor("out", [T, 1024], F32, kind="ExternalOutput").ap()
    x1_d = nc.dram_tensor("x1s", [T, 1024], F32, kind="Internal").ap()
    ecb_d = nc.dram_tensor("ecb", [16384, 2048], BF16, kind="Internal").ap()

    P = Plan()

    def MM(out, lhsT, rhs, r, w, start=True, stop=True):
        P.op("pe", lambda E: E.matmul(out, lhsT=lhsT, rhs=rhs, start=start, stop=stop), reads=r, writes=w)

    def TR(out, in_, ident, r, w):
        P.op("pe", lambda E: E.transpose(out=out, in_=in_, identity=ident), reads=r, writes=w)

    def ACT(out, in_, func, r, w, **kw):
        P.op("act", lambda E: E.activation(out=out, in_=in_, func=func, **kw), reads=r, writes=w)

    def TT(eng, out, in0, in1, op, r, w):
        P.op(eng, lambda E: E.tensor_tensor(out=out, in0=in0, in1=in1, op=op), reads=r, writes=w)

    def TS(eng, out, in0, s1, s2, op0, op1, r, w):
        if op1 is None:
            P.op(eng, lambda E: E.tensor_scalar(out=out, in0=in0, scalar1=s1, scalar2=None, op0=op0), reads=r, writes=w)
        else:
            P.op(eng, lambda E: E.tensor_scalar(out=out, in0=in0, scalar1=s1, scalar2=s2, op0=op0, op1=op1), reads=r, writes=w)

    def STT(out, in0, scalar, in1, op0, op1, r, w, accum=None):
        if accum is None:
            P.op("dve", lambda E: E.scalar_tensor_tensor(out=out, in0=in0, scalar=scalar, in1=in1, op0=op0, op1=op1), reads=r, writes=w)
        else:
            P.op("dve", lambda E: E.scalar_tensor_tensor(out=out, in0=in0, scalar=scalar, in1=in1, op0=op0, op1=op1, accum_out=accum), reads=r, writes=w)

    def CP(eng, out, in_, r, w):
        if eng == "act":
            P.op("act", lambda E: E.copy(out=out, in_=in_), reads=r, writes=w)
        else:
            P.op(eng, lambda E: E.tensor_copy(out=out, in_=in_), reads=r, writes=w)

    def RED(out, in_, r, w, op=ALU.add):
        P.op("dve", lambda E: E.tensor_reduce(out=out, in_=in_, axis=AX.X, op=op), reads=r, writes=w)

    def DMA(out, in_, r, w, eng="sp"):
        P.op(eng, lambda E: E.dma_start(out=out, in_=in_), reads=r, writes=w, dma=True)

    def RSTD(out, ss, scale, r_junk=None):
        ACT(out[:], ss[:], AF.Sqrt, [ss, epsb], [out], scale=scale, bias=epsb[:, 0:1])
        P.op("dve", lambda E: E.reciprocal(out=out[:], in_=out[:]), reads=[out], writes=[out])

    def STOP(tag, pieces):
        if mode == tag:
            col = 0
            for (ap, tl, n) in pieces:
                DMA(out_d[0:128, col:col + n], ap, [tl], [])
                col += n
            P.stopped = True

    with ExitStack() as es0:
        def sb0(name, shape, dt=F32):
            return Tile(name, es0.enter_context(nc.sbuf_tensor(name, shape, dt)))

        def ps0(name, shape, dt=F32):
            return Tile(name, es0.enter_context(nc.psum_tensor(name, shape, dt)))

        banks = [ps0("pb%d" % i, [128, 512]) for i in range(7)]
        pT = ps0("pT", [128, 1024], BF16)
        bstate = {"i": 0, "n": 7}

        def nb():
            b = banks[bstate["i"] % bstate["n"]]
            bstate["i"] += 1
            return b

        ident = sb0("ident", [128, 128])
        identb = sb0("identb", [128, 128], BF16)
        ones = sb0("ones", [128, 128])
        epsb = sb0("epsb", [128, 1])
        xt = sb0("xt", [128, 1024])
        junk = sb0("junk", [128, 2568])
        ss = sb0("ss", [128, 1])
        rstd = sb0("rstd", [128, 1])
        hbf = sb0("hbf", [128, 1024], BF16)
        hT = sb0("hT", [128, 8, 128], BF16)
        stage = junk

        DMA(ident[:], cd["c_ident"], [], [ident])
        DMA(ones[:], cd["c_ones"], [], [ones])
        CP("dve", identb[:], ident[:], [ident], [identb])
        P.op("dve", lambda E: E.memset(epsb[:], EPS), writes=[epsb])

        def load_weight_bf(dst, src_d, ncols, scale_tile=None):
            for k in range(8):
                DMA(stage[:, 0:ncols], src_d[k * 128:(k + 1) * 128, :], [], [stage])
                if scale_tile is None:
                    CP("dve", dst[:, k, :], stage[:, 0:ncols], [stage], [dst])
                else:
                    TS("dve", dst[:, k, :], stage[:, 0:ncols], scale_tile[:, k:k + 1], None, ALU.mult, None,
                       [stage, scale_tile], [dst])

        def transpose8(src_bf, dstT):
            for k in range(8):
                TR(pT[:, k * 128:(k + 1) * 128], src_bf[:, k * 128:(k + 1) * 128], identb[:], [src_bf, identb], [pT])
            CP("act", dstT[:].rearrange("p k t -> p (k t)"), pT[:], [pT], [dstT])

        try:
          with ExitStack() as es1:
              def sb(name, shape, dt=F32):
                  return Tile(name, es1.enter_context(nc.sbuf_tensor(name, shape, dt)))

              Win = sb("Win", [128, 8, 2568], BF16)
              Wout = sb("Wout", [128, 8, 1024], BF16)
              nmix = sb("nmix", [128, 8])
              cwT = sb("cwT", [128, 12, 4])
              wpool = sb("wpool", [128, 4, 128])
              dnw_b = sb("dnw_b", [128, 128])
              pscale_b = sb("pscale_b", [128, 512])
              dtb_b = sb("dtb_b", [128, 4])
              negA = sb("negA", [128, 4])
              Um = sb("Um", [128, 128])
              M1 = sb("M1", [128, 512])
              M2 = sb("M2", [128, 512])
              MC = sb("MC", [128, 512])
              MPm = sb("MPm", [128, 512])
              MF = sb("MF", [128, 512])

              DMA(nmix[:], nmix_d, [], [nmix])
              load_weight_bf(Win, win_d, 2568, nmix)
              load_weight_bf(Wout, wout_d, 1024)
              DMA(stage[0:4, 0:1536], cw_d, [], [stage])
              bcw = nb()
              for c in range(12):
                  TR(bcw[:, c * 4:(c + 1) * 4], stage[0:4, c * 128:(c + 1) * 128], ident[0:4, 0:4], [stage, ident], [bcw])
              CP("act", cwT[:].rearrange("p c j -> p (c j)"), bcw[:, 0:48], [bcw], [cwT])
              DMA(wpool[:], wpool_d.rearrange("g c d -> c g d"), [], [wpool])
              DMA(dnw_b[:], dnw_d.partition_broadcast(128), [], [dnw_b])
              DMA(pscale_b[:], pscale_d.partition_broadcast(128), [], [pscale_b])
              DMA(dtb_b[:], dtb_d.partition_broadcast(128), [], [dtb_b])
              DMA(negA[:], alog_d.partition_broadcast(128), [], [negA])
              ACT(negA[:], negA[:], AF.Exp, [negA], [negA])
              TS("dve", negA[:], negA[:], -1.0, None, ALU.mult, None, [negA], [negA])
              for tl, nm in ((Um, "c_U"), (M1, "c_M1"), (M2, "c_M2"), (MC, "c_MC"), (MPm, "c_MP"), (MF, "c_MF")):
                  DMA(tl[:], cd[nm], [], [tl])

              qkvT = sb("qkvT", [128, 12, 131])
              cacc = sb("cacc", [128, 12, 128])
              qkv = sb("qkv", [128, 1536])
              proj2 = sb("proj2", [128, 1032])
              pprev = sb("pprev", [128, 512])
              S = sb("S", [128, 4, 128])
              qn = sb("qn", [128, 512])
              qg = sb("qg", [128, 512])
              kn = sb("kn", [128, 512])
              ss8 = sb("ss8", [128, 8])
              rn8 = sb("rn8", [128, 8])
              beta = sb("beta", [128, 4])
              nbeta = sb("nbeta", [128, 4])
              gt = sb("gt", [128, 4])
              gc = sb("gc", [128, 4])
              ngc = sb("ngc", [128, 4])
              glb = sb("glb", [128, 4])
              eg = sb("eg", [128, 4])
              egl = sb("egl", [128, 4])
              ekd = sb("ekd", [128, 4])
              bk = sb("bk", [128, 4])
              gU4 = sb("gU4", [128, 4, 128])
              decS = sb("decS", [128, 4, 128])
              decT = sb("decT", [128, 4, 128])
              vb = sb("vb", [128, 4, 128])
              kbg = sb("kbg", [128, 4, 128])
              kd = sb("kd", [128, 4, 128])
              kT4 = sb("kT4", [128, 4, 128])
              qT4 = sb("qT4", [128, 4, 128])
              qgT4 = sb("qgT4", [128, 4, 128])
              Qb = [[sb("Qb%d_%d" % (h, i), [128, 128]) for i in range(2)] for h in range(4)]
              Pb = [[sb("Pb%d_%d" % (h, i), [128, 128]) for i in range(2)] for h in range(4)]
              Xb = [[sb("Xb%d_%d" % (h, i), [128, 128]) for i in range(2)] for h in range(4)]
              ATb = [sb("AT%d" % h, [128, 128]) for h in range(4)]
              nkcdTb = [sb("nkcdT%d" % h, [128, 128]) for h in range(4)]
              vnewb = [sb("vnew%d" % h, [128, 128]) for h in range(4)]
              osb = sb("osb", [128, 512])
              ss4 = sb("ss4", [128, 4])
              rn4 = sb("rn4", [128, 4])
              gz = sb("gz", [128, 512])
              mix = sb("mix", [128, 1024])
              plT = sb("plT", [128, 4, 128])

              P.op("dve", lambda E: E.memset(qkvT[:], 0.0), writes=[qkvT])
              P.op("dve", lambda E: E.memset(pprev[:], 0.0), writes=[pprev])
              P.op("dve", lambda E: E.memset(S[:], 0.0), writes=[("S", h) for h in range(4)])

              for t in range(NT):
                  DMA(xt[:], x_d[t * 128:(t + 1) * 128, :], [], [xt])
                  ACT(junk[:, 0:1024], xt[:], AF.Square, [xt], [junk, ss], accum_out=ss[:])
                  RSTD(rstd, ss, 1.0 / 1024)
                  TS("dve", hbf[:], xt[:], rstd[:, 0:1], None, ALU.mult, None, [xt, rstd], [hbf])
                  transpose8(hbf, hT)
                  for b3 in range(3):
                      bk_ = nb()
                      for cc in range(4):
                          c = b3 * 4 + cc
                          for k in range(8):
                              MM(bk_[:, cc * 128:(cc + 1) * 128], Win[:, k, c * 128:(c + 1) * 128], hT[:, k, :],
                                 [Win, hT], [bk_], start=(k == 0), stop=(k == 7))
                      CP("act", qkvT[:, b3 * 4:(b3 + 1) * 4, 3:131],
                         bk_[:].rearrange("p (c t) -> p c t", c=4), [bk_], [qkvT])
                  pz = nb()
                  pg = nb()
                  pp = nb()
                  for k in range(8):
                      MM(pz[:, :], hT[:, k, :], Win[:, k, 1536:2048], [Win, hT], [pz], start=(k == 0), stop=(k == 7))
                  for k in range(8):
                      MM(pg[:, 0:8], hT[:, k, :], Win[:, k, 2048:2056], [Win, hT], [pg], start=(k == 0), stop=(k == 7))
                  for k in range(8):
                      MM(pp[:, :], hT[:, k, :], Win[:, k, 2056:2568], [Win, hT], [pp], start=(k == 0), stop=(k == 7))
                  CP("act", proj2[:, 0:512], pz[:], [pz], [proj2])
                  CP("act", proj2[:, 512:520], pg[:, 0:8], [pg], [proj2])
                  CP("act", proj2[:, 520:1032], pp[:], [pp], [proj2])
                  STOP("a", [(proj2[:, 0:1024], proj2, 1024)])
                  jv = junk[:, 0:1536].rearrange("p (c t) -> p c t", c=12)
                  TT("dve", cacc[:], qkvT[:, :, 3:131], cwT[:, :, 3:4].to_broadcast([128, 12, 128]), ALU.mult, [qkvT, cwT], [cacc])
                  for j in range(3):
                      TT("dve", jv, qkvT[:, :, j:j + 128], cwT[:, :, j:j + 1].to_broadcast([128, 12, 128]), ALU.mult,
                         [qkvT, cwT], [junk])
                      TT("dve", cacc[:], cacc[:], jv, ALU.add, [cacc, junk], [cacc])
                  CP("dve", qkvT[:, :, 0:3], qkvT[:, :, 128:131], [qkvT], [qkvT])
                  ACT(cacc[:], cacc[:], AF.Silu, [cacc], [cacc])
                  for b3 in range(3):
                      bk_ = nb()
                      for cc in range(4):
                          c = b3 * 4 + cc
                          TR(bk_[:, cc * 128:(cc + 1) * 128], cacc[:, c, :], ident[:], [cacc, ident], [bk_])
                      CP("act", qkv[:, b3 * 512:(b3 + 1) * 512], bk_[:], [bk_], [qkv])
                  TT("dve", junk[:, 0:1024], qkv[:, 0:1024], qkv[:, 0:1024], ALU.mult, [qkv], [junk])
                  RED(ss8[:], junk[:, 0:1024].rearrange("p (h c) -> p h c", h=8), [junk], [ss8])
                  RSTD(rn8, ss8, 1.0)
                  TT("dve", kn[:].rearrange("p (h c) -> p h c", h=4), qkv[:, 512:1024].rearrange("p (h c) -> p h c", h=4),
                     rn8[:, 4:8].unsqueeze(2).to_broadcast([128, 4, 128]), ALU.mult, [qkv, rn8], [kn])
                  TS("dve", rn8[:, 0:4], rn8[:, 0:4], 128.0 ** -0.5, None, ALU.mult, None, [rn8], [rn8])
                  TT("dve", qn[:].rearrange("p (h c) -> p h c", h=4), qkv[:, 0:512].rearrange("p (h c) -> p h c", h=4),
                     rn8[:, 0:4].unsqueeze(2).to_broadcast([128, 4, 128]), ALU.mult, [qkv, rn8], [qn])
                  STOP("b", [(qn[:], qn, 512), (kn[:], kn, 512)])
                  ACT(beta[:], proj2[:, 512:516], AF.Sigmoid, [proj2], [beta])
                  TS("dve", nbeta[:], beta[:], -1.0, None, ALU.mult, None, [beta], [nbeta])
                  TT("dve", gt[:], proj2[:, 516:520], dtb_b[:], ALU.add, [proj2, dtb_b], [gt])
                  ACT(gt[:], gt[:], AF.Exp, [gt], [gt])
                  ACT(gt[:], gt[:], AF.Ln, [gt], [gt], bias=1.0)
                  TT("dve", gt[:], gt[:], negA[:], ALU.mult, [gt, negA], [gt])
                  pgc = nb()
                  MM(pgc[:, 0:4], Um[:], gt[:], [Um, gt], [pgc])
                  MM(pgc[:, 8:12], ones[:], gt[:], [ones, gt], [pgc])
                  CP("dve", gc[:], pgc[:, 0:4], [pgc], [gc])
                  CP("dve", glb[:], pgc[:, 8:12], [pgc], [glb])
                  TS("dve", ngc[:], gc[:], -1.0, None, ALU.mult, None, [gc], [ngc])
                  ACT(eg[:], gc[:], AF.Exp, [gc], [eg])
                  ACT(egl[:], glb[:], AF.Exp, [glb], [egl])
                  TT("dve", ekd[:], glb[:], gc[:], ALU.subtract, [glb, gc], [ekd])
                  ACT(ekd[:], ekd[:], AF.Exp, [ekd], [ekd])
                  TT("dve", bk[:], beta[:], eg[:], ALU.mult, [beta, eg], [bk])
                  TT("dve", gU4[:], Um[:].unsqueeze(1).to_broadcast([128, 4, 128]),
                     gt[:].unsqueeze(2).to_broadcast([128, 4, 128]), ALU.mult, [Um, gt], [gU4])
                  R1 = nb()
                  R2 = nb()
                  MM(R1[:], ones[:], gU4[:].rearrange("p h f -> p (h f)"), [ones, gU4], [R1], start=True, stop=False)
                  MM(R1[:], ident[:], M1[:], [ident, M1], [R1], start=False, stop=True)
                  MM(R2[:], ones[:], gU4[:].rearrange("p h f -> p (h f)"), [ones, gU4], [R2], start=True, stop=False)
                  MM(R2[:], ident[:], M2[:], [ident, M2], [R2], start=False, stop=True)
                  for h in range(4):
                      ACT(decS[:, h, :], R1[:, h * 128:(h + 1) * 128], AF.Exp, [R1, gc], [decS], scale=-1.0, bias=gc[:, h:h + 1])
                      ACT(decT[:, h, :], R2[:, h * 128:(h + 1) * 128], AF.Exp, [R2, ngc], [decT], scale=1.0, bias=ngc[:, h:h + 1])
                  v3 = qkv[:, 1024:1536].rearrange("p (h c) -> p h c", h=4)
                  kn3 = kn[:].rearrange("p (h c) -> p h c", h=4)
                  TT("dve", vb[:], v3, beta[:].unsqueeze(2).to_broadcast([128, 4, 128]), ALU.mult, [qkv, beta], [vb])
                  TT("dve", kbg[:], kn3, bk[:].unsqueeze(2).to_broadcast([128, 4, 128]), ALU.mult, [kn, bk], [kbg])
                  TT("dve", kd[:], kn3, ekd[:].unsqueeze(2).to_broadcast([128, 4, 128]), ALU.mult, [kn, ekd], [kd])
                  TT("dve", qg[:].rearrange("p (h c) -> p h c", h=4), qn[:].rearrange("p (h c) -> p h c", h=4),
                     eg[:].unsqueeze(2).to_broadcast([128, 4, 128]), ALU.mult, [qn, eg], [qg])
                  for (src, dst) in ((kn, kT4), (qn, qT4), (qg, qgT4)):
                      bk_ = nb()
                      for h in range(4):
                          TR(bk_[:, h * 128:(h + 1) * 128], src[:, h * 128:(h + 1) * 128], ident[:], [src, ident], [bk_])
                      CP("act", dst[:].rearrange("p h t -> p (h t)"), bk_[:], [bk_], [dst])
                      STOP("c", [(decS[:].rearrange("p h t -> p (h t)"), decS, 512), (kT4[:].rearrange("p h t -> p (h t)"), kT4, 512)])
                  hs = []
                  for h in range(4):
                      pG = nb()
                      MM(pG[:, 0:128], kT4[:, h, :], kT4[:, h, :], [kT4], [pG])
                      MM(pG[:, 128:256], kT4[:, h, :], qT4[:, h, :], [kT4, qT4], [pG])
                      Q, Pm, X = Qb[h][0], Pb[h][0], Xb[h][0]
                      STT(Q[:], pG[:, 0:128], nbeta[:, h:h + 1], decS[:, h, :], ALU.mult, ALU.mult, [pG, nbeta, decS], [Q])
                      TT("dve", ATb[h][:], pG[:, 128:256], decT[:, h, :], ALU.mult, [pG, decT], [ATb[h]])
                      pP = nb()
                      TR(pP[:, 0:128], Q[:], ident[:], [Q, ident], [pP])
                      CP("dve", Pm[:], pP[:, 0:128], [pP], [Pm])
                      TT("dve", X[:], pP[:, 0:128], ident[:], ALU.add, [pP, ident], [X])
                      hs.append([Q, Pm, X, 0])
                  for m in range(6):
                      nxt = []
                      for h in range(4):
                          Q, Pm, X, par = hs[h]
                          Qn_, Pn_, Xn_ = Qb[h][1 - par], Pb[h][1 - par], Xb[h][1 - par]
                          pq = nb()
                          MM(pq[:, 0:128], Pm[:], Q[:], [Pm, Q], [pq])
                          if m < 5:
                              MM(pq[:, 128:256], Q[:], Pm[:], [Pm, Q], [pq])
                          CP("act", Qn_[:], pq[:, 0:128], [pq], [Qn_])
                          if m < 5:
                              CP("act", Pn_[:], pq[:, 128:256], [pq], [Pn_])
                          nxt.append([Qn_, Pn_, Xn_, 1 - par])
                      for h in range(4):
                          X = hs[h][2]
                          Qn_, Pn_, Xn_, _ = nxt[h]
                          px = nb()
                          MM(px[:, 0:128], Qn_[:], X[:], [Qn_, X], [px])
                          TT("dve", Xn_[:], px[:, 0:128], X[:], ALU.add, [px, X], [Xn_])
                      hs = nxt
                  pks, pvs, pos_ = [], [], []
                  for h in range(4):
                      pk = nb()
                      MM(pk[:, 0:128], kbg[:, h, :], hs[h][2][:], [kbg, hs[h][2]], [pk])
                      pks.append(pk)
                  for h in range(4):
                      TS("dve", nkcdTb[h][:], pks[h][:, 0:128], -1.0, None, ALU.mult, None, [pks[h]], [nkcdTb[h]])
                  for h in range(4):
                      pv = nb()
                      MM(pv[:, 0:128], hs[h][2][:], vb[:, h, :], [hs[h][2], vb], [pv], start=True, stop=False)
                      MM(pv[:, 0:128], nkcdTb[h][:], S[:, h, :], [nkcdTb[h], ("S", h)], [pv], start=False, stop=True)
                      pvs.append(pv)
                  for h in range(4):
                      CP("act", vnewb[h][:], pvs[h][:, 0:128], [pvs[h]], [vnewb[h]])
                  for h in range(4):
                      po = nb()
                      MM(po[:, 0:128], qgT4[:, h, :], S[:, h, :], [qgT4, ("S", h)], [po], start=True, stop=False)
                      MM(po[:, 0:128], ATb[h][:], vnewb[h][:], [ATb[h], vnewb[h]], [po], start=False, stop=True)
                      MM(po[:, 128:256], kd[:, h, :], vnewb[h][:], [kd, vnewb[h]], [po])
                      pos_.append(po)
                  for h in range(4):
                      po = pos_[h]
                      CP("dve", osb[:, h * 128:(h + 1) * 128], po[:, 0:128], [po], [("osb", h)])
                      STT(S[:, h, :], S[:, h, :], egl[:, h:h + 1], po[:, 128:256], ALU.mult, ALU.add, [("S", h), egl, po], [("S", h)])
                  TT("dve", junk[:, 0:512], osb[:], osb[:], ALU.mult, [("osb", 0), ("osb", 1), ("osb", 2), ("osb", 3)], [junk])
                  RED(ss4[:], junk[:, 0:512].rearrange("p (h c) -> p h c", h=4), [junk], [ss4])
                  RSTD(rn4, ss4, 1.0 / 128)
                  ACT(gz[:], proj2[:, 0:512], AF.Silu, [proj2], [gz])
                  TT("dve", gz[:].rearrange("p (h c) -> p h c", h=4), gz[:].rearrange("p (h c) -> p h c", h=4),
                     dnw_b[:].unsqueeze(1).to_broadcast([128, 4, 128]), ALU.mult, [gz, dnw_b], [gz])
                  TT("dve", osb[:].rearrange("p (h c) -> p h c", h=4), osb[:].rearrange("p (h c) -> p h c", h=4),
                     rn4[:].unsqueeze(2).to_broadcast([128, 4, 128]), ALU.mult, [("osb", 0), ("osb", 1), ("osb", 2), ("osb", 3)] + [rn4], [("osb", 0), ("osb", 1), ("osb", 2), ("osb", 3)])
                  TT("dve", mix[:, 0:512], osb[:], gz[:], ALU.mult, [("osb", 0), ("osb", 1), ("osb", 2), ("osb", 3)] + [gz], [mix])
                  ppl = nb()
                  Mcur = MF if t == 0 else MC
                  for g in range(4):
                      MM(ppl[:, g * 128:(g + 1) * 128], proj2[:, 520 + g * 128:520 + (g + 1) * 128], Mcur[:, g * 128:(g + 1) * 128],
                         [proj2, Mcur], [ppl], start=True, stop=False)
                      MM(ppl[:, g * 128:(g + 1) * 128], pprev[:, g * 128:(g + 1) * 128], MPm[:, g * 128:(g + 1) * 128],
                         [pprev, MPm], [ppl], start=False, stop=True)
                  CP("act", plT[:].rearrange("p g t -> p (g t)"), ppl[:], [ppl], [plT])
                  CP("dve", pprev[:], proj2[:, 520:1032], [proj2], [pprev])
                  pmx = nb()
                  for g in range(4):
                      MM(pmx[:, g * 128:(g + 1) * 128], plT[:, g, :], wpool[:, g, :], [plT, wpool], [pmx])
                  TT("dve", mix[:, 512:1024], pmx[:], pscale_b[:], ALU.mult, [pmx, pscale_b], [mix])
                  CP("dve", hbf[:], mix[:], [mix], [hbf])
                  transpose8(hbf, hT)
                  for n in range(2):
                      py = nb()
                      for k in range(8):
                          MM(py[:], hT[:, k, :], Wout[:, k, n * 512:(n + 1) * 512], [hT, Wout], [py], start=(k == 0), stop=(k == 7))
                      TT("dve", xt[:, n * 512:(n + 1) * 512], xt[:, n * 512:(n + 1) * 512], py[:], ALU.add, [xt, py], [xt])
                  DMA(x1_d[t * 128:(t + 1) * 128, :], xt[:], [xt], ["x1d%d" % t])
                  if mode == "p1":
                      DMA(out_d[t * 128:(t + 1) * 128, :], xt[:], [xt], [])

        except StopBuild:
            pass
        with ExitStack() as es2:
          if mode != "p1":
              def sb(name, shape, dt=F32):
                  return Tile(name, es2.enter_context(nc.sbuf_tensor(name, shape, dt)))

              Wq = sb("Wq", [128, 8, 2048], BF16)
              skT = sb("skT", [128, 16, 128])
              nffn_b = sb("nffn_b", [128, 1024])
              nfin_b = sb("nfin_b", [128, 1024])
              iota16 = sb("iota16", [128, 16])
              xn = sb("xn", [128, 1024])
              qTs = sb("qTs", [128, 16, 128])
              s16 = sb("s16", [128, 16, 16])
              i16 = sb("i16", [128, 16, 16], U32)
              if16 = sb("if16", [128, 16, 16])
              work = sb("work", [128, 256])
              cand = sb("cand", [128, 8, 256])
              ts = sb("ts", [128, 8, 16])
              pos = sb("pos", [128, 8, 16], U32)
              posi = sb("posi", [128, 8, 16], U32)
              pa = sb("pa", [128, 8, 16])
              pbm = sb("pbm", [128, 8, 16])
              eq = sb("eq", [128, 8, 256])
              eidx = sb("eidx", [128, 8, 16])
              eidx2 = sb("eidx2", [128, 8, 16])
              eidi = sb("eidi", [128, 128], I32)
              gate = sb("gate", [128, 8, 16])
              gsum = sb("gsum", [128, 8])
              araw = sb("araw", [128, 128])
              coef = sb("coef", [128, 128])
              gl = sb("gl", [128, 128])
              djunk = sb("djunk", [128, 2048], BF16)
              with ExitStack() as es3:
                  NST = 3
                  stg = [Tile("stg%d" % i, es3.enter_context(nc.sbuf_tensor("stg%d" % i, [128, 2048], F32))) for i in range(NST)]
                  stb = [Tile("stb%d" % i, es3.enter_context(nc.sbuf_tensor("stb%d" % i, [128, 2048], BF16))) for i in range(NST)]
                  for c in range(128):
                      a, b = stg[c % NST], stb[c % NST]
                      DMA(a[:], ec_d[c * 128:(c + 1) * 128, :], [], [a])
                      CP(("act", "dve")[c % 2], b[:], a[:], [a], [b])
                      DMA(ecb_d[c * 128:(c + 1) * 128, :], b[:], [b], [("ecb", c)])
              NB = 16
              UVb = [sb("UVb%d" % i, [128, 2048], BF16) for i in range(NB)]
              dgb = [sb("dgb%d" % i, [128, 128], BF16) for i in range(NB)]
              bstate["n"] = 5
              accA, accB = banks[5], banks[6]

              load_weight_bf(Wq, wq_d, 2048)
              DMA(nffn_b[:], nffn_d.partition_broadcast(128), [], [nffn_b])
              DMA(nfin_b[:], nfin_d.partition_broadcast(128), [], [nfin_b])
              DMA(iota16[:], cd["c_iota"], [], [iota16])
              for b4 in range(4):
                  DMA(stage[:, 0:512].rearrange("p (b c) -> p b c", b=4), sk_d[b4 * 4:(b4 + 1) * 4].rearrange("b k c -> k b c"),
                      [], [stage])
                  bk_ = nb()
                  for bb in range(4):
                      TR(bk_[:, bb * 128:(bb + 1) * 128], stage[:, bb * 128:(bb + 1) * 128], ident[:], [stage, ident], [bk_])
                  CP("act", skT[:, b4 * 4:(b4 + 1) * 4, :].rearrange("p b k -> p (b k)"), bk_[:], [bk_], [skT])

              gidx = 0
              ECBK = [("ecb", c) for c in range(128)]
              for t in range(NT):
                  DMA(xt[:], x1_d[t * 128:(t + 1) * 128, :], ["x1d%d" % t], [xt])
                  ACT(junk[:, 0:1024], xt[:], AF.Square, [xt], [junk, ss], accum_out=ss[:])
                  RSTD(rstd, ss, 1.0 / 1024)
                  STT(xn[:], xt[:], rstd[:, 0:1], nffn_b[:], ALU.mult, ALU.mult, [xt, rstd, nffn_b], [xn])
                  CP("act", hbf[:], xn[:], [xn], [hbf])
                  transpose8(hbf, hT)
                  for b4 in range(4):
                      bk_ = nb()
                      for bb in range(4):
                          blk = b4 * 4 + bb
                          for k in range(8):
                              MM(bk_[:, bb * 128:(bb + 1) * 128], Wq[:, k, blk * 128:(blk + 1) * 128], hT[:, k, :],
                                 [Wq, hT], [bk_], start=(k == 0), stop=(k == 7))
                      CP("act", qTs[:, b4 * 4:(b4 + 1) * 4, :].rearrange("p b t -> p (b t)"), bk_[:], [bk_], [qTs])
                  for b4 in range(4):
                      bk_ = nb()
                      for bb in range(4):
                          blk = b4 * 4 + bb
                          MM(bk_[:, bb * 128:(bb + 1) * 128], qTs[:, blk, :], skT[:, blk, :], [qTs, skT], [bk_])
                      for bb in range(4):
                          blk = b4 * 4 + bb
                          sc = bk_[:, bb * 128:(bb + 1) * 128]
                          P.op("dve", lambda E, sc=sc, blk=blk: E.max(out=s16[:, blk, 0:8], in_=sc), reads=[bk_], writes=[s16])
                          P.op("dve", lambda E, sc=sc, blk=blk: E.max_index(out=i16[:, blk, 0:8], in_max=s16[:, blk, 0:8], in_values=sc),
                               reads=[bk_, s16], writes=[i16])
                          P.op("dve", lambda E, sc=sc, blk=blk: E.match_replace(out=work[:, 0:128], in_to_replace=s16[:, blk, 0:8],
                                                                               in_values=sc, imm_value=-1e30),
                               reads=[bk_, s16], writes=[work])
                          P.op("dve", lambda E, blk=blk: E.max(out=s16[:, blk, 8:16], in_=work[:, 0:128]), reads=[work], writes=[s16])
                          P.op("dve", lambda E, blk=blk: E.max_index(out=i16[:, blk, 8:16], in_max=s16[:, blk, 8:16],
                                                                    in_values=work[:, 0:128]), reads=[work, s16], writes=[i16])
                  CP("dve", if16[:], i16[:], [i16], [if16])
                  s4 = s16[:].rearrange("p (h two) k -> p h two k", two=2)
                  i4 = if16[:].rearrange("p (h two) k -> p h two k", two=2)
                  TT("dve", cand[:].rearrange("p h (a b) -> p h a b", a=16),
                     s4[:, :, 0, :].unsqueeze(3).to_broadcast([128, 8, 16, 16]),
                     s4[:, :, 1, :].unsqueeze(2).to_broadcast([128, 8, 16, 16]), ALU.add, [s16], [cand])
                  for h in range(8):
                      P.op("dve", lambda E, h=h: E.max(out=ts[:, h, 0:8], in_=cand[:, h, :]), reads=[cand], writes=[ts])
                      P.op("dve", lambda E, h=h: E.max_index(out=pos[:, h, 0:8], in_max=ts[:, h, 0:8], in_values=cand[:, h, :]),
                           reads=[cand, ts], writes=[pos])
                      P.op("dve", lambda E, h=h: E.match_replace(out=work[:], in_to_replace=ts[:, h, 0:8], in_values=cand[:, h, :],
                                                                 imm_value=-1e30), reads=[cand, ts], writes=[work])
                      P.op("dve", lambda E, h=h: E.max(out=ts[:, h, 8:16], in_=work[:]), reads=[work], writes=[ts])
                      P.op("dve", lambda E, h=h: E.max_index(out=pos[:, h, 8:16], in_max=ts[:, h, 8:16], in_values=work[:]),
                           reads=[work, ts], writes=[pos])
                  TS("dve", posi[:], pos[:], 15, None, ALU.bitwise_and, None, [pos], [posi])
                  CP("dve", pbm[:], posi[:], [posi], [pbm])
                  TS("dve", posi[:], pos[:], 4, None, ALU.logical_shift_right, None, [pos], [posi])
                  CP("dve", pa[:], posi[:], [posi], [pa])
                  eq4 = eq[:].rearrange("p h (k a) -> p h k a", k=16)
                  io4 = iota16[:].unsqueeze(1).unsqueeze(1).to_broadcast([128, 8, 16, 16])
                  TT("dve", eq4, pa[:].unsqueeze(3).to_broadcast([128, 8, 16, 16]), io4, ALU.is_equal, [pa, iota16], [eq])
                  TT("dve", eq4, eq4, i4[:, :, 0, :].unsqueeze(2).to_broadcast([128, 8, 16, 16]), ALU.mult, [eq, if16], [eq])
                  RED(eidx[:], eq4, [eq], [eidx])
                  TT("dve", eq4, pbm[:].unsqueeze(3).to_broadcast([128, 8, 16, 16]), io4, ALU.is_equal, [pbm, iota16], [eq])
                  TT("dve", eq4, eq4, i4[:, :, 1, :].unsqueeze(2).to_broadcast([128, 8, 16, 16]), ALU.mult, [eq, if16], [eq])
                  RED(eidx2[:], eq4, [eq], [eidx2])
                  STT(eidx[:].rearrange("p h k -> p (h k)"), eidx[:].rearrange("p h k -> p (h k)"), 128.0,
                      eidx2[:].rearrange("p h k -> p (h k)"), ALU.mult, ALU.add, [eidx, eidx2], [eidx])
                  CP("dve", eidi[:], eidx[:].rearrange("p h k -> p (h k)"), [eidx], [eidi])
                  TT("dve", gate[:], ts[:], ts[:, :, 0:1].to_broadcast([128, 8, 16]), ALU.subtract, [ts], [gate])
                  ACT(gate[:], gate[:], AF.Exp, [gate], [gate])
                  RED(gsum[:], gate[:], [gate], [gsum])
                  P.op("dve", lambda E: E.reciprocal(out=gsum[:], in_=gsum[:]), reads=[gsum], writes=[gsum])
                  TT("dve", gate[:], gate[:], gsum[:].unsqueeze(2).to_broadcast([128, 8, 16]), ALU.mult, [gate, gsum], [gate])
                  gflat = gate[:].rearrange("p h k -> p (h k)")
                  P.op("dve", lambda E: E.memset(araw[:], 0.0), writes=[("araw", q) for q in range(0, 128, GRP)])
                  for g0 in range(0, 0 if mode == "ng" else 128, GRP):
                      bufs = []
                      for s in range(g0, g0 + GRP):
                          bi = gidx % NB
                          gidx += 1
                          UV, dg = UVb[bi], dgb[bi]
                          bufs.append((s, UV, dg))
                          P.op("pool", lambda E, UV=UV, s=s: E.indirect_dma_start(
                              out=UV[:], out_offset=None, in_=ecb_d,
                              in_offset=bass.IndirectOffsetOnAxis(ap=eidi[:, s:s + 1], axis=0)),
                              reads=[eidi] + ECBK, writes=[UV], dma=True)
                      for (s, UV, dg) in bufs:
                          jk = s % 2
                          STT(djunk[:, jk * 1024:(jk + 1) * 1024], UV[:, 0:1024], 1.0, xn[:], ALU.mult, ALU.mult,
                              [UV, xn], [("dj", jk), ("araw", g0)], accum=araw[:, s:s + 1])
                      ACT(gl[:, g0:g0 + GRP], araw[:, g0:g0 + GRP], AF.Gelu, [("araw", g0)], [("gl", g0)])
                      for (s, UV, dg) in bufs:
                          TS("dve", dg[:], ident[:], gl[:, s:s + 1], gflat[:, s:s + 1], ALU.mult, ALU.mult,
                             [ident, ("gl", g0), gate], [dg])
                          MM(accA[:], dg[:], UV[:, 1024:1536], [dg, UV], [accA], start=(s == 0), stop=(s == 127))
                          MM(accB[:], dg[:], UV[:, 1536:2048], [dg, UV], [accB], start=(s == 0), stop=(s == 127))
                  if mode != "ng":
                      TT("dve", xt[:, 0:512], xt[:, 0:512], accA[:], ALU.add, [xt, accA], [xt])
                      TT("dve", xt[:, 512:1024], xt[:, 512:1024], accB[:], ALU.add, [xt, accB], [xt])
                  ACT(junk[:, 0:1024], xt[:], AF.Square, [xt], [junk, ss], accum_out=ss[:])
                  RSTD(rstd, ss, 1.0 / 1024)
                  STT(xn[:], xt[:], rstd[:, 0:1], nfin_b[:], ALU.mult, ALU.mult, [xt, rstd, nfin_b], [xn])
                  DMA(out_d[t * 128:(t + 1) * 128, :], xn[:], [xn], [])

        P.finish()
        P.emit(nc)
    return nc


_CACHE = {}


def make_in_maps(inputs, ncores, NT):
    consts = host_consts()
    f = lambda a: np.ascontiguousarray(np.asarray(a, dtype=np.float32))
    shared = {
        "norm_mix_w": np.ascontiguousarray(f(inputs["norm_mix_w"]).reshape(8, 128).T),
        "w_in": f(inputs["w_in"]).reshape(1024, 2568),
        "conv_w": f(inputs["conv_w"]).reshape(4, 1536),
        "a_log": f(inputs["a_log"]).reshape(1, 4),
        "dt_bias": f(inputs["dt_bias"]).reshape(1, 4),
        "dn_norm_w": f(inputs["dn_norm_w"]).reshape(1, 128),
        "w_pool": f(inputs["w_pool"]).reshape(4, 128, 128),
        "pool_scale": f(inputs["pool_scale"]).reshape(1, 512),
        "w_out": f(inputs["w_out"]).reshape(1024, 1024),
        "norm_ffn_w": f(inputs["norm_ffn_w"]).reshape(1, 1024),
        "w_query": f(inputs["w_query"]).reshape(1024, 2048),
        "sub_keys": f(inputs["sub_keys"]).reshape(16, 128, 128),
        "expert_cat": np.concatenate([f(inputs["expert_down"]).reshape(16384, 1024),
                                      f(inputs["expert_up"]).reshape(16384, 1024)], axis=1),
        "norm_final_w": f(inputs["norm_final_w"]).reshape(1, 1024),
    }
    shared.update(consts)
    x = f(inputs["x"])
    maps = []
    for c in range(ncores):
        m = dict(shared)
        m["x"] = np.ascontiguousarray(x[c, :NT * 128, :])
        maps.append(m)
    return maps


def kernel(**inputs):
    NT = 64
    ncores = 8
    if "nc" not in _CACHE:
        _CACHE["nc"] = build(NT)
    nc = _CACHE["nc"]
    maps = make_in_maps(inputs, ncores, NT)
    res = run_bass_kernel_spmd(nc, maps, core_ids=list(range(ncores)))
    out = np.stack([np.asarray(r["out"]) for r in res.results], axis=0)
    return out.astype(np.float32)
```
